# Optimizing a Trainium2 kernel written in Bass

```python
import jax
import jax.numpy as jnp
from jax import lax
import numpy as np

D_MODEL = 1024
BATCH = 2
SEQ = 16384
DEPTH = 2

CTX_LEN = 256
GRID_W = 64
BLOCK = 128
NORM_EPS = 1e-6
ROPE_BASE = 10000.0
NEG_INF = -1e30
F32 = jnp.float32

MLA_HEADS = 8
MLA_NOPE = 64
MLA_ROPE = 32
MLA_V = 64
Q_LORA = 256
KV_LORA = 128
RET_HEADS = 4
RET_DK = 128
RET_DV = 128
RET_CHUNK = 128
RET_DECAY_EXP_FWD = 5.0
RET_DECAY_EXP_BWD = 5.5
RET_GN_EPS = 1e-5
RWKV_HEADS = 8
RWKV_HD = 64
RWKV_DIM = RWKV_HEADS * RWKV_HD
DECAY_LORA = 64
AAA_LORA = 64
GATE_LORA = 128
RWKV_LN_EPS = 64e-5
WIN_HEADS = 8
WIN_KV_HEADS = 2
WIN_GROUP = WIN_HEADS // WIN_KV_HEADS
WIN_HD = 64
WINDOW = 128
N_EXPERTS = 16
EXPERT_FF = 1536
EC_FACTOR = 2

MIX_WIDTH = MLA_HEADS * MLA_V + RET_HEADS * RET_DV
EVEN_SPLITS = (Q_LORA, KV_LORA, MLA_ROPE, RET_HEADS * RET_DK, RET_HEADS * RET_DK, RET_HEADS * RET_DV, RET_HEADS * RET_DV)
RWKV_SPLITS = (RWKV_DIM, RWKV_DIM, RWKV_DIM, DECAY_LORA, DECAY_LORA, AAA_LORA, AAA_LORA, GATE_LORA)
WIN_SPLITS = (WIN_HEADS * WIN_HD, WIN_KV_HEADS * WIN_HD, WIN_KV_HEADS * WIN_HD)
EVEN_IN = sum(EVEN_SPLITS)
RWKV_IN = sum(RWKV_SPLITS)
ODD_IN = RWKV_IN + sum(WIN_SPLITS)
N_EVEN = (DEPTH + 1) // 2
N_ODD = DEPTH // 2

kernel_name = 'hybrid_mla_retention_rwkv7_swa_ec_moe_diffusion'


def split_cols(p, sizes):
    return jnp.split(p, np.cumsum(sizes)[:-1].tolist(), axis=-1)


def maybe_flip(t, rev):
    return jnp.flip(t, axis=1) if rev else t


def rmsnorm(x, g):
    xf = x.astype(F32)
    y = xf * lax.rsqrt(jnp.mean(xf * xf, axis=-1, keepdims=True) + NORM_EPS)
    return (y * g.astype(F32)).astype(x.dtype)


def head_layernorm(y, eps):
    yc = y - jnp.mean(y, axis=-1, keepdims=True)
    return yc * lax.rsqrt(jnp.mean(yc * yc, axis=-1, keepdims=True) + eps)


def modulation(cvec, w, b):
    m = jax.nn.silu(cvec) @ w + b
    if m.ndim == 2:
        m = m[:, None, :]
    return jnp.split(m, 6, axis=-1)


def modulate(h, shift, scale):
    return h * (1.0 + scale) + shift


def rope_rotate(x, pos):
    half = x.shape[-1] // 2
    inv_freq = ROPE_BASE ** (-jnp.arange(half, dtype=F32) / half)
    ang = pos.astype(F32)[:, None] * inv_freq[None, :]
    cos = jnp.cos(ang)[None, :, None, :].astype(x.dtype)
    sin = jnp.sin(ang)[None, :, None, :].astype(x.dtype)
    x1, x2 = x[..., :half], x[..., half:]
    return jnp.concatenate([x1 * cos - x2 * sin, x1 * sin + x2 * cos], axis=-1)


def axial_rope(x):
    t = jnp.arange(x.shape[1])
    half = x.shape[-1] // 2
    return jnp.concatenate([rope_rotate(x[..., :half], t // GRID_W),
                            rope_rotate(x[..., half:], t % GRID_W)], axis=-1)


def centred_shift(p):
    z = jnp.zeros_like(p[:, :1])
    return 0.5 * (jnp.concatenate([z, p[:, :-1]], axis=1) + jnp.concatenate([p[:, 1:], z], axis=1))


def mla_keys_values(ckv, kr, kv_norm_g, w_ukv, rotary):
    B, N, _ = ckv.shape
    kv = (rmsnorm(ckv, kv_norm_g) @ w_ukv).reshape(B, N, MLA_HEADS, MLA_NOPE + MLA_V)
    k_nope, v = kv[..., :MLA_NOPE], kv[..., MLA_NOPE:]
    kr = kr[:, :, None, :]
    if rotary:
        kr = axial_rope(kr)
    k = jnp.concatenate([k_nope, jnp.broadcast_to(kr, (B, N, MLA_HEADS, MLA_ROPE))], axis=-1)
    return k, v


def mla_queries(cq, q_norm_g, w_uq, rotary):
    B, N, _ = cq.shape
    q = (rmsnorm(cq, q_norm_g) @ w_uq).reshape(B, N, MLA_HEADS, MLA_NOPE + MLA_ROPE)
    if rotary:
        q = jnp.concatenate([q[..., :MLA_NOPE], axial_rope(q[..., MLA_NOPE:])], axis=-1)
    return q


def dense_block_attention(q, k, v):
    B, N, H, d = q.shape
    nb = N // BLOCK
    scale = d ** -0.5
    qb = jnp.moveaxis(q.reshape(B, nb, BLOCK, H, d), 1, 0)

    def attend(qblk):
        s = jnp.einsum('bqhd,bkhd->bhqk', qblk, k).astype(F32) * scale
        p = jax.nn.softmax(s, axis=-1).astype(v.dtype)
        return jnp.einsum('bhqk,bkhv->bqhv', p, v)

    o = lax.map(attend, qb)
    return jnp.moveaxis(o, 0, 1).reshape(B, N, H * v.shape[-1])


def retention_state_update(S, kd, v, chunk_decay):
    return S * chunk_decay[None, :, None, None] + jnp.einsum('bjhd,bjhv->bhdv', kd, v)


def retention_chunks(q, k, v, log_gamma, state0):
    B, N, H, _ = k.shape
    L = RET_CHUNK
    nc = N // L
    pos = jnp.arange(L, dtype=F32)
    diff = pos[:, None] - pos[None, :]
    d_in = jnp.where(diff >= 0, jnp.exp(log_gamma[:, None, None] * jnp.maximum(diff, 0.0)), 0.0)
    q_dec = jnp.exp(log_gamma[None, :] * (pos[:, None] + 1.0))
    k_dec = jnp.exp(log_gamma[None, :] * (L - 1.0 - pos[:, None]))
    c_dec = jnp.exp(log_gamma * L)
    to_chunks = lambda t: jnp.moveaxis(t.astype(F32).reshape(B, nc, L, H, t.shape[-1]), 1, 0)
    kc, vc = to_chunks(k), to_chunks(v)
    kdc = kc * k_dec[None, None, :, :, None]
    if q is None:
        def state_step(S, xs):
            return retention_state_update(S, xs[0], xs[1], c_dec), None
        S, _ = lax.scan(state_step, state0, (kdc, vc))
        return None, S
    qc = to_chunks(q)

    def step(S, xs):
        qi, ki, kdi, vi = xs
        s = jnp.einsum('bihd,bjhd->bhij', qi, ki) * d_in
        o = (jnp.einsum('bhij,bjhv->bihv', s, vi)
             + jnp.einsum('bihd,bhdv->bihv', qi * q_dec[None, :, :, None], S))
        return retention_state_update(S, kdi, vi, c_dec), o

    S, o = lax.scan(step, state0, (qc, kc, kdc, vc))
    return jnp.moveaxis(o, 0, 1).reshape(B, N, H, v.shape[-1]), S


def bidirectional_retention(q_l, k_l, v_l, q_c, k_c, v_c):
    S0 = jnp.zeros((k_l.shape[0], RET_HEADS, RET_DK, RET_DV), F32)
    outs_l, outs_c = [], []
    for exp0, rev in ((RET_DECAY_EXP_FWD, False), (RET_DECAY_EXP_BWD, True)):
        log_gamma = jnp.log1p(-2.0 ** (-exp0 - jnp.arange(RET_HEADS, dtype=F32)))
        oc, S = retention_chunks(None if q_c is None else maybe_flip(q_c, rev),
                                 maybe_flip(k_c, rev), maybe_flip(v_c, rev), log_gamma, S0)
        ol, _ = retention_chunks(maybe_flip(q_l, rev), maybe_flip(k_l, rev), maybe_flip(v_l, rev), log_gamma, S)
        outs_l.append(maybe_flip(ol, rev))
        if q_c is not None:
            outs_c.append(maybe_flip(oc, rev))
    out_c = outs_c[0] + outs_c[1] if q_c is not None else None
    return outs_l[0] + outs_l[1], out_c


def retention_output(o, g, gn_g):
    B, N = o.shape[:2]
    y = head_layernorm(o, RET_GN_EPS).reshape(B, N, RET_HEADS * RET_DV) * gn_g
    return jax.nn.silu(g) * y.astype(g.dtype)


def even_mixer(a_lat, a_ctx, w_in, w_out, q_norm_g, w_uq, kv_norm_g, w_ukv, ret_norm_g, need_ctx):
    n_lat, n_ctx = a_lat.shape[1], a_ctx.shape[1]
    cq_l, ckv_l, kr_l, rq_l, rk_l, rv_l, rg_l = split_cols(a_lat @ w_in, EVEN_SPLITS)
    cq_c, ckv_c, kr_c, rq_c, rk_c, rv_c, rg_c = split_cols(a_ctx @ w_in, EVEN_SPLITS)
    k_l, v_l = mla_keys_values(ckv_l, kr_l, kv_norm_g, w_ukv, True)
    k_c, v_c = mla_keys_values(ckv_c, kr_c, kv_norm_g, w_ukv, False)
    q_l = mla_queries(cq_l, q_norm_g, w_uq, True)
    att_l = dense_block_attention(q_l, jnp.concatenate([k_c, k_l], axis=1), jnp.concatenate([v_c, v_l], axis=1))
    heads = lambda t: t.reshape(t.shape[0], t.shape[1], RET_HEADS, -1)
    pos_c = jnp.arange(n_ctx)
    pos_l = n_ctx + jnp.arange(n_lat)
    k_scale = RET_DK ** -0.5
    ret_q_l = rope_rotate(heads(rq_l), pos_l)
    ret_k_l = rope_rotate(heads(rk_l), pos_l) * k_scale
    ret_k_c = rope_rotate(heads(rk_c), pos_c) * k_scale
    ret_q_c = rope_rotate(heads(rq_c), pos_c) if need_ctx else None
    ret_l, ret_c = bidirectional_retention(ret_q_l, ret_k_l, heads(rv_l), ret_q_c, ret_k_c, heads(rv_c))
    y_lat = jnp.concatenate([att_l, retention_output(ret_l, rg_l, ret_norm_g)], axis=-1) @ w_out
    if not need_ctx:
        return y_lat, None
    att_c = dense_block_attention(mla_queries(cq_c, q_norm_g, w_uq, False), k_c, v_c)
    y_ctx = jnp.concatenate([att_c, retention_output(ret_c, rg_c, ret_norm_g)], axis=-1) @ w_out
    return y_lat, y_ctx


def rwkv_features(p, mu, w0, w2, a0, a2, k_k, k_a):
    B, N, _ = p.shape
    p = (p + mu * (centred_shift(p) - p)).astype(F32)
    r, k, v, wl_f, wl_b, al_f, al_b, gl = split_cols(p, RWKV_SPLITS)
    heads = lambda t: t.reshape(B, N, RWKV_HEADS, RWKV_HD)
    kk = heads(k * k_k)
    kk = kk * lax.rsqrt(jnp.sum(kk * kk, axis=-1, keepdims=True) + 1e-12)
    decays, rates, keys = [], [], []
    for d, (wl, al) in enumerate(((wl_f, al_f), (wl_b, al_b))):
        w = -jax.nn.softplus(-(w0[d] + jnp.tanh(wl) @ w2[d])) - 0.5
        a = jax.nn.sigmoid(a0[d] + al @ a2[d])
        decays.append(heads(jnp.exp(-jnp.exp(w))))
        rates.append(heads(a))
        keys.append(heads(k * (1.0 + (a - 1.0) * k_a)))
    return heads(r), heads(k), heads(v), kk, decays, rates, keys, gl


def rwkv_update(S, dec, k, v, kk, a):
    sa = jnp.einsum('bhvk,bhk->bhv', S, -kk)
    return (S * dec[:, :, None, :] + sa[..., None] * (kk * a)[:, :, None, :]
            + v[..., None] * k[:, :, None, :])


def rwkv_scan(r, dec, k, v, kk, a, S0):
    seq = lambda t: jnp.moveaxis(t, 1, 0)
    rest = tuple(seq(t) for t in (dec, k, v, kk, a))
    if r is None:
        def state_step(S, xs):
            return rwkv_update(S, *xs), None
        S, _ = lax.scan(state_step, S0, rest)
        return None, S

    def step(S, xs):
        S = rwkv_update(S, *xs[1:])
        return S, jnp.einsum('bhvk,bhk->bhv', S, xs[0])

    S, y = lax.scan(step, S0, (seq(r),) + rest)
    return jnp.moveaxis(y, 0, 1), S


def bidirectional_rwkv(f_l, f_c, need_ctx):
    r_l, _, v_l, kk_l, dec_l, a_l, key_l, _ = f_l
    r_c, _, v_c, kk_c, dec_c, a_c, key_c, _ = f_c
    S0 = jnp.zeros((r_l.shape[0], RWKV_HEADS, RWKV_HD, RWKV_HD), F32)
    ys_l, ys_c = [], []
    for d, rev in ((0, False), (1, True)):
        f = lambda t: maybe_flip(t, rev)
        yc, S = rwkv_scan(f(r_c) if need_ctx else None, f(dec_c[d]), f(key_c[d]), f(v_c), f(kk_c), f(a_c[d]), S0)
        yl, _ = rwkv_scan(f(r_l), f(dec_l[d]), f(key_l[d]), f(v_l), f(kk_l), f(a_l[d]), S)
        ys_l.append(f(yl))
        if need_ctx:
            ys_c.append(f(yc))
    y_c = ys_c[0] + ys_c[1] if need_ctx else None
    return ys_l[0] + ys_l[1], y_c


def rwkv_output(y, feats, g2, r_k, ln_g, ln_b):
    r, k, v, gl = feats[0], feats[1], feats[2], feats[7]
    B, N = y.shape[:2]
    yn = head_layernorm(y, RWKV_LN_EPS).reshape(B, N, RWKV_DIM) * ln_g + ln_b
    bonus = (jnp.sum(r * k * r_k, axis=-1, keepdims=True) * v).reshape(B, N, RWKV_DIM)
    return (yn + bonus) * (jax.nn.sigmoid(gl) @ g2)


def sink_attend(q, k, v, sink, mask):
    s = jnp.einsum('bqkgd,bjkd->bkgqj', q, k).astype(F32) * (WIN_HD ** -0.5)
    if mask is not None:
        s = jnp.where(mask, s, NEG_INF)
    sk = sink.astype(F32).reshape(1, WIN_KV_HEADS, WIN_GROUP, 1, 1)
    m = jnp.maximum(jnp.max(s, axis=-1, keepdims=True), sk)
    e = jnp.exp(s - m)
    p = (e / (jnp.sum(e, axis=-1, keepdims=True) + jnp.exp(sk - m))).astype(v.dtype)
    return jnp.einsum('bkgqj,bjkd->bqkgd', p, v)


def window_attention(q, k, v, k_ctx, v_ctx, sink):
    B, N = q.shape[:2]
    nb = N // BLOCK
    pad = ((0, 0), (BLOCK, BLOCK), (0, 0), (0, 0))
    kp, vp = jnp.pad(k, pad), jnp.pad(v, pad)
    qb = jnp.moveaxis(q.reshape(B, nb, BLOCK, WIN_KV_HEADS, WIN_GROUP, WIN_HD), 1, 0)
    qi = jnp.arange(BLOCK)[:, None]
    kj = jnp.arange(3 * BLOCK)[None, :]
    in_band = jnp.abs(kj - BLOCK - qi) <= WINDOW
    ctx_ok = jnp.ones((BLOCK, k_ctx.shape[1]), dtype=bool)

    def attend(args):
        blk, qblk = args
        start = blk * BLOCK
        kw = lax.dynamic_slice_in_dim(kp, start, 3 * BLOCK, axis=1)
        vw = lax.dynamic_slice_in_dim(vp, start, 3 * BLOCK, axis=1)
        kpos = start - BLOCK + kj
        mask = jnp.concatenate([in_band & (kpos >= 0) & (kpos < N), ctx_ok], axis=-1)
        return sink_attend(qblk, jnp.concatenate([kw, k_ctx], axis=1), jnp.concatenate([vw, v_ctx], axis=1), sink, mask)

    o = lax.map(attend, (jnp.arange(nb), qb))
    return jnp.moveaxis(o, 0, 1).reshape(B, N, WIN_HEADS * WIN_HD)


def odd_mixer(a_lat, a_ctx, w_in, w_out, mu, w0, w2, a0, a2, g2, k_k, k_a, r_k, ln_g, ln_b, sink, need_ctx):
    B, n_lat = a_lat.shape[:2]
    n_ctx = a_ctx.shape[1]
    rw_l, wq_l, wk_l, wv_l = split_cols(a_lat @ w_in, (RWKV_IN,) + WIN_SPLITS)
    rw_c, wq_c, wk_c, wv_c = split_cols(a_ctx @ w_in, (RWKV_IN,) + WIN_SPLITS)
    f_l = rwkv_features(rw_l, mu, w0, w2, a0, a2, k_k, k_a)
    f_c = rwkv_features(rw_c, mu, w0, w2, a0, a2, k_k, k_a)
    y_l, y_c = bidirectional_rwkv(f_l, f_c, need_ctx)
    rwkv_l = rwkv_output(y_l, f_l, g2, r_k, ln_g, ln_b).astype(a_lat.dtype)
    kvh = lambda t, n: t.reshape(B, n, WIN_KV_HEADS, WIN_HD)
    q_l = axial_rope(wq_l.reshape(B, n_lat, WIN_HEADS, WIN_HD)).reshape(B, n_lat, WIN_KV_HEADS, WIN_GROUP, WIN_HD)
    k_l = axial_rope(kvh(wk_l, n_lat))
    k_c, v_c = kvh(wk_c, n_ctx), kvh(wv_c, n_ctx)
    win_l = window_attention(q_l, k_l, kvh(wv_l, n_lat), k_c, v_c, sink)
    y_lat = jnp.concatenate([rwkv_l, win_l], axis=-1) @ w_out
    if not need_ctx:
        return y_lat, None
    rwkv_c = rwkv_output(y_c, f_c, g2, r_k, ln_g, ln_b).astype(a_ctx.dtype)
    q_c = wq_c.reshape(B, n_ctx, WIN_KV_HEADS, WIN_GROUP, WIN_HD)
    win_c = sink_attend(q_c, k_c, v_c, sink, None).reshape(B, n_ctx, WIN_HEADS * WIN_HD)
    y_ctx = jnp.concatenate([rwkv_c, win_c], axis=-1) @ w_out
    return y_lat, y_ctx


def expert_choice_ffn(h, w_router, w_gate, w_up, w_down):
    B, N, D = h.shape
    cap = EC_FACTOR * N // N_EXPERTS
    aff = jax.nn.softmax(jnp.einsum('bnd,de->bne', h, w_router).astype(F32), axis=-1)
    gate, idx = lax.top_k(jnp.swapaxes(aff, 1, 2), cap)
    xs = jax.vmap(lambda hb, ib: hb[ib])(h, idx)
    u = jax.nn.silu(jnp.einsum('becd,edf->becf', xs, w_gate)) * jnp.einsum('becd,edf->becf', xs, w_up)
    y = jnp.einsum('becf,efd->becd', u, w_down) * gate[..., None].astype(h.dtype)
    return jax.vmap(lambda yb, ib: jnp.zeros((N, D), h.dtype).at[ib.reshape(-1)].add(yb.reshape(-1, D)))(y, idx)


def setup_inputs(seed: int = 0) -> dict:
    key = jax.random.key(seed)
    ks = iter(jax.random.split(key, 48))
    D = D_MODEL
    nrm = lambda shape, scale: jax.random.normal(next(ks), shape, F32) * scale
    gain = lambda shape: 1.0 + nrm(shape, 0.02)
    return {
        'x': nrm((BATCH, SEQ, D), 1.0),
        'c': nrm((BATCH, D), 1.0),
        'ctx': nrm((BATCH, CTX_LEN, D), 1.0),
        'c_ctx': nrm((D,), 1.0),
        'ada_w': nrm((DEPTH, D, 6 * D), 0.5 * D ** -0.5),
        'ada_b': nrm((DEPTH, 6 * D), 0.02),
        'norm_mix_g': gain((DEPTH, D)),
        'norm_ffn_g': gain((DEPTH, D)),
        'final_g': gain((D,)),
        'ev_w_in': nrm((N_EVEN, D, EVEN_IN), D ** -0.5),
        'ev_w_out': nrm((N_EVEN, MIX_WIDTH, D), MIX_WIDTH ** -0.5),
        'mla_q_norm_g': gain((N_EVEN, Q_LORA)),
        'mla_w_uq': nrm((N_EVEN, Q_LORA, MLA_HEADS * (MLA_NOPE + MLA_ROPE)), Q_LORA ** -0.5),
        'mla_kv_norm_g': gain((N_EVEN, KV_LORA)),
        'mla_w_ukv': nrm((N_EVEN, KV_LORA, MLA_HEADS * (MLA_NOPE + MLA_V)), KV_LORA ** -0.5),
        'ret_norm_g': gain((N_EVEN, RET_HEADS * RET_DV)),
        'od_w_in': nrm((N_ODD, D, ODD_IN), D ** -0.5),
        'od_w_out': nrm((N_ODD, MIX_WIDTH, D), MIX_WIDTH ** -0.5),
        'rwkv_mu': jax.random.uniform(next(ks), (N_ODD, RWKV_IN), F32),
        'rwkv_w0': jax.random.uniform(next(ks), (N_ODD, 2, RWKV_DIM), F32, -6.0, -1.0),
        'rwkv_w2': nrm((N_ODD, 2, DECAY_LORA, RWKV_DIM), 0.1 * DECAY_LORA ** -0.5),
        'rwkv_a0': nrm((N_ODD, 2, RWKV_DIM), 0.5),
        'rwkv_a2': nrm((N_ODD, 2, AAA_LORA, RWKV_DIM), 0.1 * AAA_LORA ** -0.5),
        'rwkv_g2': nrm((N_ODD, GATE_LORA, RWKV_DIM), GATE_LORA ** -0.5),
        'rwkv_k_k': 0.85 + nrm((N_ODD, RWKV_DIM), 0.02),
        'rwkv_k_a': gain((N_ODD, RWKV_DIM)),
        'rwkv_r_k': nrm((N_ODD, RWKV_HEADS, RWKV_HD), 0.1),
        'rwkv_ln_g': gain((N_ODD, RWKV_DIM)),
        'rwkv_ln_b': nrm((N_ODD, RWKV_DIM), 0.02),
        'win_sink': nrm((N_ODD, WIN_HEADS), 1.0),
        'moe_router': nrm((DEPTH, D, N_EXPERTS), D ** -0.5),
        'moe_w_gate': nrm((DEPTH, N_EXPERTS, D, EXPERT_FF), D ** -0.5),
        'moe_w_up': nrm((DEPTH, N_EXPERTS, D, EXPERT_FF), D ** -0.5),
        'moe_w_down': nrm((DEPTH, N_EXPERTS, EXPERT_FF, D), EXPERT_FF ** -0.5),
    }


def reference(x, c, ctx, c_ctx, ada_w, ada_b, norm_mix_g, norm_ffn_g, final_g,
              ev_w_in, ev_w_out, mla_q_norm_g, mla_w_uq, mla_kv_norm_g, mla_w_ukv, ret_norm_g,
              od_w_in, od_w_out, rwkv_mu, rwkv_w0, rwkv_w2, rwkv_a0, rwkv_a2, rwkv_g2,
              rwkv_k_k, rwkv_k_a, rwkv_r_k, rwkv_ln_g, rwkv_ln_b, win_sink,
              moe_router, moe_w_gate, moe_w_up, moe_w_down):
    x_lat, x_ctx = x, ctx
    for layer in range(DEPTH):
        need_ctx = layer < DEPTH - 1
        sh1, sc1, g1, sh2, sc2, g2 = modulation(c, ada_w[layer], ada_b[layer])
        csh1, csc1, cg1, csh2, csc2, cg2 = modulation(c_ctx, ada_w[layer], ada_b[layer])
        a_lat = modulate(rmsnorm(x_lat, norm_mix_g[layer]), sh1, sc1)
        a_ctx = modulate(rmsnorm(x_ctx, norm_mix_g[layer]), csh1, csc1)
        if layer % 2 == 0:
            e = layer // 2
            y_lat, y_ctx = even_mixer(a_lat, a_ctx, ev_w_in[e], ev_w_out[e], mla_q_norm_g[e], mla_w_uq[e],
                                      mla_kv_norm_g[e], mla_w_ukv[e], ret_norm_g[e], need_ctx)
        else:
            o = layer // 2
            y_lat, y_ctx = odd_mixer(a_lat, a_ctx, od_w_in[o], od_w_out[o], rwkv_mu[o], rwkv_w0[o], rwkv_w2[o],
                                     rwkv_a0[o], rwkv_a2[o], rwkv_g2[o], rwkv_k_k[o], rwkv_k_a[o], rwkv_r_k[o],
                                     rwkv_ln_g[o], rwkv_ln_b[o], win_sink[o], need_ctx)
        x_lat = x_lat + g1 * y_lat
        h_lat = modulate(rmsnorm(x_lat, norm_ffn_g[layer]), sh2, sc2)
        x_lat = x_lat + g2 * expert_choice_ffn(h_lat, moe_router[layer], moe_w_gate[layer], moe_w_up[layer], moe_w_down[layer])
        if need_ctx:
            x_ctx = x_ctx + cg1 * y_ctx
            h_ctx = modulate(rmsnorm(x_ctx, norm_ffn_g[layer]), csh2, csc2)
            x_ctx = x_ctx + cg2 * expert_choice_ffn(h_ctx, moe_router[layer], moe_w_gate[layer], moe_w_up[layer], moe_w_down[layer])
    return rmsnorm(x_lat, final_g)
```

```python
import numpy as np

from contextlib import ExitStack
import concourse.bass as bass
import concourse.mybir as mybir
from concourse.bass_utils import run_bass_kernel_spmd

F32 = mybir.dt.float32
BF16 = mybir.dt.bfloat16
I32 = mybir.dt.int32
U32 = mybir.dt.uint32
AF = mybir.ActivationFunctionType
ALU = mybir.AluOpType
AX = mybir.AxisListType


class V:
    __slots__ = ("key", "ap")

    def __init__(self, key, ap):
        self.key = key
        self.ap = ap

    def __getitem__(self, idx):
        return V(self.key, self.ap[idx])

    def sub(self, subkey, idx=None):
        return V((self.key, subkey), self.ap if idx is None else self.ap[idx])

    def re(self, pattern, **kw):
        return V(self.key, self.ap.rearrange(pattern, **kw))


class _Op:
    __slots__ = ("eng", "fn", "kind", "deps", "sig", "dsem", "dval", "idx", "selfwait")


class Prog:
    ENGS = ("pe", "act", "dve", "pool", "sp")
    NDMA = 8

    def __init__(self, name="k"):
        self.nc = bass.Bass("TRN2", target_bir_lowering=False)
        self.es = ExitStack()
        self.ops = {e: [] for e in self.ENGS}
        self.state = {}
        self.ndma = {e: 0 for e in self.ENGS}
        self.uid = 0
        self.outs = []

    def dram(self, name, shape, dt, kind):
        t = self.nc.dram_tensor(name, list(shape), dt, kind=kind)
        return V(("d", name), t.ap())

    def inp(self, name, shape, dt=F32):
        return self.dram(name, shape, dt, "ExternalInput")

    def out(self, name, shape, dt=F32):
        v = self.dram(name, shape, dt, "ExternalOutput")
        self.outs.append(name)
        return v

    def scratch(self, name, shape, dt=F32):
        return self.dram(name, shape, dt, "Internal")

    def sb(self, name, shape, dt=F32):
        t = self.es.enter_context(self.nc.sbuf_tensor(name, list(shape), dt))
        return V(("s", name), t.ap() if hasattr(t, "ap") and callable(t.ap) else t[:])

    def ps(self, name, shape, dt=F32):
        t = self.es.enter_context(self.nc.psum_tensor(name, list(shape), dt))
        return V(("p", name), t.ap() if hasattr(t, "ap") and callable(t.ap) else t[:])

    def op(self, eng, fn, reads=(), writes=(), kind="c"):
        o = _Op()
        o.eng, o.fn, o.kind = eng, fn, kind
        o.sig = None
        o.idx = len(self.ops[eng])
        deps = set()
        for v in reads:
            st = self.state.get(v.key)
            if st is not None and st[0] is not None:
                deps.add(st[0])
        for v in writes:
            st = self.state.get(v.key)
            if st is not None:
                if st[0] is not None:
                    deps.add(st[0])
                for r in st[1].values():
                    deps.add(r)
        o.deps = [d for d in deps if not (d.eng == eng and d.kind == "c" and kind == "c" and eng == "pe")]
        if kind == "d":
            j = self.ndma[eng]
            self.ndma[eng] += 1
            o.dsem = j % self.NDMA
            o.dval = 16 * (j // self.NDMA + 1)
        self.ops[eng].append(o)
        for v in reads:
            st = self.state.setdefault(v.key, [None, {}])
            if kind == "d":
                st[1][(eng, kind, o.idx)] = o
            else:
                st[1][(eng, kind)] = o
        for v in writes:
            self.state[v.key] = [o, {}]
        return o

    def dma(self, out, in_, eng="sp", **kw):
        return self.op(eng, lambda e: e.dma_start(out=out.ap, in_=in_.ap, **kw), [in_], [out], kind="d")

    def mm(self, out, lhsT, rhs, start=True, stop=True, extra_reads=()):
        return self.op("pe", lambda e: e.matmul(out.ap, lhsT.ap, rhs.ap, start=start, stop=stop),
                       [lhsT, rhs] + ([] if start else [out]) + list(extra_reads), [out])

    def tr(self, out, in_, ident):
        return self.op("pe", lambda e: e.transpose(out.ap, in_.ap, ident.ap), [in_, ident], [out])

    def act(self, out, in_, func, bias=None, scale=None, accum=None, eng="act"):
        kw = {}
        rd = [in_]
        wr = [out]
        if bias is not None:
            if isinstance(bias, V):
                kw["bias"] = bias.ap
                rd.append(bias)
            else:
                kw["bias"] = bias
        if scale is not None:
            if isinstance(scale, V):
                kw["scale"] = scale.ap
                rd.append(scale)
            else:
                kw["scale"] = scale
        if accum is not None:
            kw["accum_out"] = accum.ap
            wr.append(accum)
        return self.op(eng, lambda e: e.activation(out.ap, in_.ap, func, **kw), rd, wr)

    def tt(self, out, a, b, op, eng="dve"):
        return self.op(eng, lambda e: e.tensor_tensor(out=out.ap, in0=a.ap, in1=b.ap, op=op), [a, b], [out])

    def ts(self, out, a, s1, op0, s2=None, op1=None, accum=None, eng="dve"):
        rd = [a]
        wr = [out]
        kw = {}
        a1 = s1
        a2 = s2
        if isinstance(s1, V):
            rd.append(s1)
            a1 = s1.ap
        if isinstance(s2, V):
            rd.append(s2)
            a2 = s2.ap
        if op1 is not None:
            kw["op1"] = op1
        if accum is not None:
            kw["accum_out"] = accum.ap
            wr.append(accum)
        return self.op(eng, lambda e: e.tensor_scalar(out=out.ap, in0=a.ap, scalar1=a1, scalar2=a2, op0=op0, **kw), rd, wr)

    def stt(self, out, a, s, b, op0, op1, accum=None, eng="dve"):
        rd = [a, b]
        wr = [out]
        sa = s
        kw = {}
        if isinstance(s, V):
            rd.append(s)
            sa = s.ap
        if accum is not None:
            kw["accum_out"] = accum.ap
            wr.append(accum)
        return self.op(eng, lambda e: e.scalar_tensor_tensor(out=out.ap, in0=a.ap, scalar=sa, in1=b.ap, op0=op0, op1=op1, **kw), rd, wr)

    def copy(self, out, in_, eng="dve"):
        if eng == "act":
            return self.op("act", lambda e: e.copy(out.ap, in_.ap), [in_], [out])
        return self.op(eng, lambda e: e.tensor_copy(out=out.ap, in_=in_.ap), [in_], [out])

    def memset(self, out, val, eng="dve"):
        return self.op(eng, lambda e: e.memset(out.ap, val), [], [out])

    def recip(self, out, in_, eng="dve"):
        return self.op(eng, lambda e: e.reciprocal(out=out.ap, in_=in_.ap), [in_], [out])

    def reduce(self, out, in_, op, axis=AX.X, eng="dve"):
        return self.op(eng, lambda e: e.tensor_reduce(out=out.ap, in_=in_.ap, axis=axis, op=op), [in_], [out])

    def scan(self, out, d0, d1, init, op0, op1):
        rd = [d0, d1]
        ia = init
        if isinstance(init, V):
            rd.append(init)
            ia = init.ap
        return self.op("dve", lambda e: e.tensor_tensor_scan(out=out.ap, data0=d0.ap, data1=d1.ap, initial=ia, op0=op0, op1=op1), rd, [out])

    def bcreg(self, g, val):
        d = self.__dict__.setdefault("_bcregs", {})
        if val not in d:
            d[val] = g.alloc_register(f"bc{val}")
            g.reg_mov(d[val], val)
        return d[val]

    def build(self):
        nc = self.nc
        cnt = {e: 0 for e in self.ENGS}
        for e in self.ENGS:
            for o in self.ops[e]:
                for d in o.deps:
                    if d.kind == "c":
                        d.sig = True
        for e in self.ENGS:
            for o in self.ops[e]:
                if o.kind == "c" and o.sig:
                    cnt[e] += 1
                    o.sig = cnt[e]
        sems = {}
        for e in self.ENGS:
            sems[("c", e)] = self.es.enter_context(nc.semaphore(f"c_{e}"))
            if self.ndma[e]:
                for i in range(min(self.NDMA, self.ndma[e])):
                    sems[("d", e, i)] = self.es.enter_context(nc.semaphore(f"d_{e}_{i}"))
        final = {e: {} for e in self.ENGS}

        def emit(eng_name, eng):
            waited = {}
            for o in self.ops[eng_name]:
                ws = {}
                for d in o.deps:
                    if d.kind == "c":
                        k, v = ("c", d.eng), d.sig
                    else:
                        k, v = ("d", d.eng, d.dsem), d.dval
                    if ws.get(k, 0) < v:
                        ws[k] = v
                if o.kind == "d" and o.dval > 16:
                    k = ("d", eng_name, o.dsem)
                    if ws.get(k, 0) < o.dval - 16:
                        ws[k] = o.dval - 16
                for k, v in ws.items():
                    if waited.get(k, 0) < v:
                        eng.wait_ge(sems[k], v)
                        waited[k] = v
                inst = o.fn(eng)
                if o.kind == "d":
                    inst.then_inc(sems[("d", eng_name, o.dsem)], 16)
                    final[eng_name][("d", eng_name, o.dsem)] = o.dval
                elif o.sig:
                    inst.then_inc(sems[("c", eng_name)], 1)
            for k, v in final[eng_name].items():
                if waited.get(k, 0) < v:
                    eng.wait_ge(sems[k], v)

        with nc.Block() as block:
            if self.ops["sp"]:
                @block.sync
                def _(e):
                    emit("sp", e)
            if self.ops["pe"]:
                @block.tensor
                def _(e):
                    emit("pe", e)
            if self.ops["act"]:
                @block.scalar
                def _(e):
                    emit("act", e)
            if self.ops["dve"]:
                @block.vector
                def _(e):
                    emit("dve", e)
            if self.ops["pool"]:
                @block.gpsimd
                def _(e):
                    emit("pool", e)
        self.es.close()
        return nc

    def run(self, in_maps, n=8, trace=False):
        nc = self.build()
        res = run_bass_kernel_spmd(nc, in_maps, core_ids=list(range(n)), trace=trace)
        return res


def build_mod():
    P = Prog()
    cT = P.inp("cT", [128, 8, 3]); w = P.inp("w", [128, 8, 1536]); b = P.inp("b", [3, 1536]); o = P.out("o", [3, 1536])
    cs = P.sb("cs", [128, 8, 3]); ws = P.sb("ws", [128, 8, 1536]); bs = P.sb("bs", [3, 1536]); os_ = P.sb("os", [3, 1536]); sg = P.sb("sg", [128, 8, 3])
    P.dma(cs, cT); P.dma(ws, w); P.dma(bs, b)
    P.act(sg, cs, AF.Sigmoid)
    P.tt(cs, cs, sg, ALU.mult)
    pp = [P.ps(f"pp{i}", [128, 512]) for i in range(3)]
    for j in range(3):
        for k in range(8):
            P.mm(pp[j][0:3, :], cs[:, k, :], ws[:, k, j*512:(j+1)*512], start=(k==0), stop=(k==7))
        P.tt(os_[:, j*512:(j+1)*512], pp[j][0:3, :], bs[:, j*512:(j+1)*512], ALU.add)
    P.dma(o, os_)
    return P


EPS = 1e-6
SQ = 96 ** -0.25
IN_CH = [(0, 512), (512, 1024), (1024, 1536), (1536, 2048), (2048, 2464)]


def load_w_bf16(P, dst, src, kchunks, ncols, stage):
    for k in range(kchunks):
        P.dma(stage[:, :ncols], src[:, k, :])
        P.copy(dst[:, k, :], stage[:, :ncols], eng=("act" if k % 2 else "dve"))


def rms_rstd(P, rs, junk, xin, n, eps=EPS):
    P.tt(junk, xin, xin, ALU.mult)
    P.reduce(rs, junk, ALU.add)
    P.ts(rs, rs, 1.0 / n, ALU.mult, eps, ALU.add)
    P.act(rs, rs, AF.Sqrt)
    P.recip(rs, rs)


def k1_build(T):
    P = Prog()
    x = P.inp("x", [T * 128, 1024])
    gB = P.inp("gB", [128, 1024])
    scB = P.inp("scB", [2, 128, 1024])
    shB = P.inp("shB", [2, 128, 1024])
    w_in = P.inp("w_in", [128, 8, 2464])
    w_uq = P.inp("w_uq", [128, 2, 768])
    w_ukv = P.inp("w_ukv", [128, 1, 1024])
    gq = P.inp("gq", [128, 256])
    gkv = P.inp("gkv", [128, 128])
    Cq = P.inp("Cq", [T * 128, 32])
    Sq_ = P.inp("Sq", [T * 128, 32])
    Cr = P.inp("Cr", [T * 128, 64])
    Sr = P.inp("Sr", [T * 128, 64])
    Crk = P.inp("Crk", [T * 128, 64])
    Srk = P.inp("Srk", [T * 128, 64])
    identd = P.inp("ident", [128, 128], BF16)
    Qa_o = P.out("Qa", [T * 128, 8 * 98], BF16)
    Ka_o = P.out("Ka", [T * 128, 8 * 98], BF16)
    Va_o = P.out("Va", [T * 128, 8 * 65], BF16)
    RQ_o = P.out("RQ", [T * 128, 512], BF16)
    RK_o = P.out("RK", [T * 128, 512], BF16)
    RV_o = P.out("RV", [T * 128, 512], BF16)
    RG_o = P.out("RG", [T * 128, 512], F32)

    ident = P.sb("ident_s", [128, 128], BF16)
    P.dma(ident, identd)
    stage = P.sb("stage", [128, 2464])
    w_in_b = P.sb("w_in_b", [128, 8, 2464], BF16)
    w_uq_b = P.sb("w_uq_b", [128, 2, 768], BF16)
    w_ukv_b = P.sb("w_ukv_b", [128, 1, 1024], BF16)
    load_w_bf16(P, w_in_b, w_in, 8, 2464, stage)
    load_w_bf16(P, w_uq_b, w_uq, 2, 768, stage)
    load_w_bf16(P, w_ukv_b, w_ukv, 1, 1024, stage)
    gqs = P.sb("gqs", [128, 256])
    gkvs = P.sb("gkvs", [128, 128])
    P.dma(gqs, gq)
    P.dma(gkvs, gkv)
    gs = P.sb("gs", [128, 1024])
    P.dma(gs, gB)
    Gp = P.sb("Gp", [128, 2, 1024])
    SH = P.sb("SHt", [128, 2, 1024])
    for c in range(2):
        P.dma(stage[:, :1024], scB[c])
        P.stt(Gp[:, c, :], stage[:, :1024], 1.0, gs, ALU.add, ALU.mult)
        P.dma(SH[:, c, :], shB[c])

    xs = [P.sb(f"xs{i}", [128, 1024]) for i in range(2)]
    junk = P.sb("junk", [128, 1024])
    tmp = P.sb("tmp", [128, 1024])
    a_bf = P.sb("a_bf", [128, 1024], BF16)
    aT = P.sb("aT", [128, 1024], BF16)
    Pm = P.sb("Pm", [128, 2464])
    rs = P.sb("rs", [128, 1])
    rs2 = P.sb("rs2", [128, 1])
    rs3 = P.sb("rs3", [128, 1])
    cqn = P.sb("cqn", [128, 256], BF16)
    cqnT = P.sb("cqnT", [128, 256], BF16)
    ckvn = P.sb("ckvn", [128, 128], BF16)
    ckvnT = P.sb("ckvnT", [128, 128], BF16)
    qf = P.sb("qf", [128, 768])
    kvf = P.sb("kvf", [128, 1024])
    t1 = P.sb("t1", [128, 512])
    t2 = P.sb("t2", [128, 512])
    krr = P.sb("krr", [128, 32])
    nq = P.sb("nq", [128, 8])
    nk = P.sb("nk", [128, 8])
    ek = P.sb("ek", [128, 8])
    krs = P.sb("krs", [128, 1])
    tabs = [[P.sb(f"tab{i}_{j}", [128, 64]) for j in range(6)] for i in range(2)]
    Qa = [P.sb(f"Qa{i}", [128, 8, 98], BF16) for i in range(2)]
    Ka = [P.sb(f"Ka{i}", [128, 8, 98], BF16) for i in range(2)]
    Va = [P.sb(f"Va{i}", [128, 8, 65], BF16) for i in range(2)]
    RQ = [P.sb(f"RQ{i}", [128, 512], BF16) for i in range(2)]
    RK = [P.sb(f"RK{i}", [128, 512], BF16) for i in range(2)]
    RV = [P.sb(f"RV{i}", [128, 512], BF16) for i in range(2)]
    RG = [P.sb(f"RG{i}", [128, 512], F32) for i in range(2)]
    pT = P.ps("pT", [128, 1024], BF16)
    pp = [P.ps(f"pp{i}", [128, 512]) for i in range(6)]

    for t in range(T):
        cls = 1 if t == 0 else 0
        i2 = t % 2
        rows = slice(t * 128, (t + 1) * 128)
        xt = xs[i2]
        P.dma(xt, x[rows, :])
        tb = tabs[i2]
        P.dma(tb[0][:, :32], Cq[rows, :])
        P.dma(tb[1][:, :32], Sq_[rows, :])
        P.dma(tb[2], Cr[rows, :])
        P.dma(tb[3], Sr[rows, :])
        P.dma(tb[4], Crk[rows, :])
        P.dma(tb[5], Srk[rows, :])
        rms_rstd(P, rs, junk, xt, 1024)
        P.stt(tmp, xt, rs, Gp[:, cls, :], ALU.mult, ALU.mult)
        P.tt(a_bf, tmp, SH[:, cls, :], ALU.add)
        for k in range(8):
            P.tr(pT[:, k * 128:(k + 1) * 128], a_bf[:, k * 128:(k + 1) * 128], ident)
        P.copy(aT, pT)
        for j, (c0, c1) in enumerate(IN_CH):
            for k in range(8):
                P.mm(pp[j][:, :c1 - c0], aT[:, k * 128:(k + 1) * 128], w_in_b[:, k, c0:c1], start=(k == 0), stop=(k == 7))
            P.copy(Pm[:, c0:c1], pp[j][:, :c1 - c0], eng=("act" if j % 2 else "dve"))
        rms_rstd(P, rs2, junk[:, :256], Pm[:, 0:256], 256)
        P.stt(cqn, Pm[:, 0:256], rs2, gqs, ALU.mult, ALU.mult)
        for k in range(2):
            P.tr(pT[:, k * 128:(k + 1) * 128], cqn[:, k * 128:(k + 1) * 128], ident)
        P.copy(cqnT, pT[:, :256])
        for j, (c0, c1) in enumerate([(0, 512), (512, 768)]):
            for k in range(2):
                P.mm(pp[j][:, :c1 - c0], cqnT[:, k * 128:(k + 1) * 128], w_uq_b[:, k, c0:c1], start=(k == 0), stop=(k == 1))
            P.act(qf[:, c0:c1], pp[j][:, :c1 - c0], AF.Copy, scale=SQ)
        qv = qf.re("p (h d) -> p h d", d=96)
        Qt = Qa[i2]
        P.tt(junk[:, :768], qf, qf, ALU.mult)
        P.reduce(nq, junk[:, :768].re("p (h d) -> p h d", d=96), ALU.add)
        P.ts(Qt[:, :, 96], nq, -0.5, ALU.mult)
        P.memset(Qt[:, :, 97], 1.0)
        P.copy(Qt[:, :, 0:64], qv[:, :, 0:64], eng="act")
        Cb = V(tb[0].key, tb[0].ap[:, :32].unsqueeze(1).to_broadcast([128, 8, 32]))
        t1v = t1[:, :256].re("p (h d) -> p h d", d=32)
        t2v = t2[:, :256].re("p (h d) -> p h d", d=32)
        P.tt(t1v, qv[:, :, 64:96], Cb, ALU.mult)
        q5 = qv[:, :, 64:96].re("p h (b f e) -> p h b f e", b=2, f=2)
        t25 = t2v.re("p h (b f e) -> p h b f e", b=2, f=2)
        S5 = tb[1].ap[:, :32].rearrange("p (b f e) -> p b f e", b=2, f=2)
        for f in range(2):
            Sb = V(tb[1].key, S5[:, :, f, :].unsqueeze(1).to_broadcast([128, 8, 2, 8]))
            P.tt(t25[:, :, :, f, :], q5[:, :, :, 1 - f, :], Sb, ALU.mult)
        P.tt(Qt[:, :, 64:96], t1v, t2v, ALU.add)
        P.dma(Qa_o[rows, :], Qt.re("p h d -> p (h d)"))
        rms_rstd(P, rs3, junk[:, :128], Pm[:, 256:384], 128)
        P.stt(ckvn, Pm[:, 256:384], rs3, gkvs, ALU.mult, ALU.mult)
        P.tr(pT[:, 0:128], ckvn, ident)
        P.copy(ckvnT, pT[:, :128])
        for j in range(2):
            P.mm(pp[2 + j], ckvnT, w_ukv_b[:, 0, j * 512:(j + 1) * 512], start=True, stop=True)
            P.copy(kvf[:, j * 512:(j + 1) * 512], pp[2 + j], eng=("act" if j % 2 else "dve"))
        kvv = kvf.re("p (h d) -> p h d", d=128)
        Kt = Ka[i2]
        Vt = Va[i2]
        kr = Pm[:, 384:416]
        P.tt(t1[:, :32], kr, tb[0][:, :32], ALU.mult)
        kr4 = kr.re("p (b f e) -> p b f e", b=2, f=2)
        t24 = t2[:, :32].re("p (b f e) -> p b f e", b=2, f=2)
        S4 = V(tb[1].key, S5)
        for f in range(2):
            P.tt(t24[:, :, f, :], kr4[:, :, 1 - f, :], S4[:, :, f, :], ALU.mult)
        P.tt(t1[:, :32], t1[:, :32], t2[:, :32], ALU.add)
        P.ts(krr, t1[:, :32], SQ, ALU.mult)
        P.act(Kt[:, :, 0:64], kvv[:, :, 0:64], AF.Copy, scale=SQ)
        P.copy(Kt[:, :, 64:96], V(krr.key, krr.ap.unsqueeze(1).to_broadcast([128, 8, 32])))
        P.memset(Kt[:, :, 96], 1.0)
        kn2 = junk[:, :512].re("p (h d) -> p h d", d=64)
        P.tt(kn2, kvv[:, :, 0:64], kvv[:, :, 0:64], ALU.mult)
        P.reduce(nk, kn2, ALU.add)
        P.tt(junk[:, 512:544], krr, krr, ALU.mult)
        P.reduce(krs, junk[:, 512:544], ALU.add)
        P.ts(nk, nk, SQ * SQ, ALU.mult, krs, ALU.add)
        P.ts(Kt[:, :, 97], nk, -0.5, ALU.mult)
        P.act(ek, Kt[:, :, 97], AF.Exp, scale=-1.0)
        P.tt(Vt[:, :, 0:64], kvv[:, :, 64:128], V(ek.key, ek.ap.unsqueeze(2).to_broadcast([128, 8, 64])), ALU.mult)
        P.copy(Vt[:, :, 64], ek)
        P.dma(Ka_o[rows, :], Kt.re("p h d -> p (h d)"))
        P.dma(Va_o[rows, :], Vt.re("p h d -> p (h d)"))
        for (c0, ci, si, dst, dst_o) in ((416, 2, 3, RQ[i2], RQ_o), (928, 4, 5, RK[i2], RK_o)):
            xv = Pm[:, c0:c0 + 512].re("p (h f e) -> p h f e", h=4, f=2)
            t1r = t1.re("p (h f e) -> p h f e", h=4, f=2)
            t2r = t2.re("p (h f e) -> p h f e", h=4, f=2)
            Cb2 = V(tb[ci].key, tb[ci].ap.unsqueeze(1).unsqueeze(1).to_broadcast([128, 4, 2, 64]))
            P.tt(t1r, xv, Cb2, ALU.mult)
            Sb2 = V(tb[si].key, tb[si].ap.unsqueeze(1).to_broadcast([128, 4, 64]))
            P.tt(t2r[:, :, 1, :], xv[:, :, 0, :], Sb2, ALU.mult)
            P.stt(t2r[:, :, 0, :], xv[:, :, 1, :], -1.0, Sb2, ALU.mult, ALU.mult)
            P.tt(dst, t1, t2, ALU.add)
            P.dma(dst_o[rows, :], dst)
        P.copy(RV[i2], Pm[:, 1440:1952], eng="act")
        P.dma(RV_o[rows, :], RV[i2])
        P.copy(RG[i2], Pm[:, 1952:2464], eng="act")
        P.dma(RG_o[rows, :], RG[i2])
    return P


def k2_build(NQ=16384, NK=16640, NC=256, nb=2):
    P = Prog()
    QT = P.inp("QT", [nb, 98, NQ], BF16)
    QTc = P.inp("QTc", [nb, 98, NC], BF16)
    KT = P.inp("KT", [nb, 98, NK], BF16)
    Vv = P.inp("Vv", [nb, 128, NK // 128, 65], BF16)
    OT = P.out("OT", [nb, 65, NQ])
    OTc = P.out("OTc", [nb, 65, NC])
    nkt = NK // 128
    kT = P.sb("kT", [98, NK], BF16)
    Vs = P.sb("Vs", [128, nkt, 65], BF16)
    qs = [P.sb(f"qs{i}", [98, 512], BF16) for i in range(2)]
    pTs = [P.sb(f"pT{i}", [128, 512], BF16) for i in range(4)]
    osb = [P.sb(f"osb{i}", [65, 512]) for i in range(2)]
    psS = [P.ps(f"psS{i}", [128, 512]) for i in range(4)]
    psO = [P.ps(f"psO{i}", [128, 512]) for i in range(2)]
    blk = 0
    cnt = 0
    for b in range(nb):
        P.dma(kT, KT[b])
        P.dma(Vs, Vv[b])
        jobs = [(QTc[b], OTc[b], 0, NC, NC // 128)] + [(QT[b][:, q0:q0 + 512], OT[b][:, q0:q0 + 512], q0, 512, nkt) for q0 in range(0, NQ, 512)]
        for (qsrc, odst, q0, w, nk) in jobs:
            q = qs[blk % 2]
            P.dma(q[:, :w], qsrc)
            po = psO[blk % 2]
            for kt in range(nk):
                s = psS[cnt % 4]
                pt = pTs[cnt % 4]
                cnt += 1
                P.mm(s[:, :w], kT[:, kt * 128:(kt + 1) * 128], q[:, :w])
                P.act(pt[:, :w], s[:, :w], AF.Exp)
                P.mm(po[0:65, :w], Vs[:, kt, :], pt[:, :w], start=(kt == 0), stop=(kt == nk - 1))
            o = osb[blk % 2]
            P.copy(o[:, :w], po[0:65, :w])
            P.dma(odst, o[:, :w])
            blk += 1
    return P


def ret_consts(h):
    L = 128
    pos = np.arange(L, dtype=np.float64)
    out = {}
    DinT = np.zeros((2, L, L), np.float32); qdec = np.zeros((2, 128, L), np.float32); kdec = np.zeros((128, 2), np.float32); cdec = np.zeros((128, 2), np.float32)
    for d, exp0 in enumerate((5.0, 5.5)):
        lg = np.log1p(-2.0 ** (-exp0 - h))
        j = pos[:, None]; i = pos[None, :]
        if d == 0:
            DinT[d] = np.where(i >= j, np.exp(lg * np.maximum(i - j, 0)), 0.0)
            qdec[d] = np.exp(lg * (pos + 1.0))[None, :]
            kdec[:, d] = np.exp(lg * (L - 1.0 - pos))
        else:
            DinT[d] = np.where(j >= i, np.exp(lg * np.maximum(j - i, 0)), 0.0)
            qdec[d] = np.exp(lg * (L - pos))[None, :]
            kdec[:, d] = np.exp(lg * pos)
        cdec[:, d] = np.exp(lg * L)
    return {"DinT": DinT, "qdec": qdec, "kdec": kdec, "cdec": cdec}

def k3_build(NCH=130, NCTX=2):
    P = Prog()
    N = NCH * 128
    qT = P.inp("qT", [128, N], BF16)
    kT = P.inp("kT", [128, N], BF16)
    kt = P.inp("kt", [128, NCH, 128], BF16)
    vt = P.inp("vt", [128, NCH, 128], BF16)
    DinT = P.inp("DinT", [2, 128, 128]); qdec = P.inp("qdec", [2, 128, 128]); kdec = P.inp("kdec", [128, 2]); cdec = P.inp("cdec", [128, 2])
    o = P.out("o", [2, NCH, 128, 128])
    qTs = P.sb("qTs", [128, N], BF16); kTs = P.sb("kTs", [128, N], BF16)
    kts = P.sb("kts", [128, NCH, 128], BF16); vts = P.sb("vts", [128, NCH, 128], BF16)
    P.dma(qTs, qT); P.dma(kTs, kT); P.dma(kts, kt); P.dma(vts, vt)
    Dm = P.sb("Dm", [128, 2, 128]); qd_ = P.sb("qd_", [128, 2, 128]); kd_ = P.sb("kd_", [128, 2]); cd_ = P.sb("cd_", [128, 2])
    for d in range(2):
        P.dma(Dm[:, d, :], DinT[d]); P.dma(qd_[:, d, :], qdec[d])
    P.dma(kd_, kdec); P.dma(cd_, cdec)
    S = P.sb("S", [128, 128]); Sb = P.sb("Sb", [128, 128], BF16)
    sTm = [P.sb(f"sTm{i}", [128, 128], BF16) for i in range(2)]
    qdv = [P.sb(f"qdv{i}", [128, 128], BF16) for i in range(2)]
    kdv = [P.sb(f"kdv{i}", [128, 128], BF16) for i in range(2)]
    ob = [P.sb(f"ob{i}", [128, 128]) for i in range(2)]
    psA = [P.ps(f"psA{i}", [128, 128]) for i in range(2)]
    psB = [P.ps(f"psB{i}", [128, 128]) for i in range(2)]
    psC = [P.ps(f"psC{i}", [128, 128]) for i in range(2)]
    n = 0
    for d in range(2):
        order = list(range(NCTX)) + list(range(NCTX, NCH))
        if d == 1:
            order = list(range(NCTX))[::-1] + list(range(NCTX, NCH))[::-1]
        P.memset(S, 0.0); P.memset(Sb, 0.0)
        for c in order:
            i2 = n % 2; n += 1
            cols = slice(c * 128, (c + 1) * 128)
            P.mm(psA[i2], kTs[:, cols], qTs[:, cols])
            P.tt(sTm[i2], psA[i2], Dm[:, d, :], ALU.mult)
            P.tt(qdv[i2], qTs[:, cols], qd_[:, d, :], ALU.mult, eng="pool")
            P.mm(psB[i2], sTm[i2], vts[:, c, :], start=True, stop=False)
            P.mm(psB[i2], qdv[i2], Sb, start=False, stop=True)
            P.copy(ob[i2], psB[i2], eng="act")
            P.dma(o[d, c], ob[i2])
            P.ts(kdv[i2], kts[:, c, :], kd_[:, d:d + 1], ALU.mult, eng="pool")
            P.mm(psC[i2], kdv[i2], vts[:, c, :])
            P.stt(S, S, cd_[:, d:d + 1], psC[i2], ALU.mult, ALU.add)
            P.copy(Sb, S)
    return P


def k4_build(T, layer_kind="even", has_ctx=True):
    P = Prog()
    x = P.inp("x", [T * 128, 1024])
    if layer_kind == "even":
        Oatt = P.inp("Oatt", [T * 128, 8, 65])
        Oret = P.inp("Oret", [T * 128, 2, 512])
        RG = P.inp("RG", [T * 128, 512])
        gn = P.inp("gn", [128, 512])
    else:
        Yr = P.inp("Yr", [T * 128, 2, 512]); Gi = P.inp("G", [T * 128, 512]); BGi = P.inp("BG", [T * 128, 512])
        Ow = P.inp("Ow", [T * 128, 8, 65]); NQi = P.inp("NQ", [T * 128, 8])
        sinkB = P.inp("sinkB", [128, 8]); lngB = P.inp("lngB", [128, 512]); lnbB = P.inp("lnbB", [128, 512])
    w_out = P.inp("w_out", [128, 8, 1024])
    g1B = P.inp("g1B", [2, 128, 1024])
    gfB = P.inp("gfB", [128, 1024]); sc2B = P.inp("sc2B", [2, 128, 1024]); sh2B = P.inp("sh2B", [2, 128, 1024])
    w_r = P.inp("w_r", [128, 8, 16])
    identd = P.inp("ident", [128, 128], BF16); identfd = P.inp("identf", [128, 128])
    xmid_o = P.out("xmid", [T * 128, 1024])
    h_o = P.out("h", [T * 128, 1024], BF16)
    aff_o = P.out("aff", [T * 128, 16])
    ident = P.sb("ident_s", [128, 128], BF16); P.dma(ident, identd)
    identf = P.sb("identf_s", [128, 128]); P.dma(identf, identfd)
    stage = P.sb("stage", [128, 1024])
    w_out_b = P.sb("w_out_b", [128, 8, 1024], BF16)
    load_w_bf16(P, w_out_b, w_out, 8, 1024, stage)
    wr = P.sb("wr", [128, 8, 16]); P.dma(wr, w_r)
    G1 = P.sb("G1", [128, 2, 1024]); Gp = P.sb("Gp", [128, 2, 1024]); SH = P.sb("SHt", [128, 2, 1024]); gs = P.sb("gs", [128, 1024])
    P.dma(gs, gfB)
    for c in range(2):
        P.dma(G1[:, c, :], g1B[c])
        P.dma(stage, sc2B[c])
        P.stt(Gp[:, c, :], stage, 1.0, gs, ALU.add, ALU.mult)
        P.dma(SH[:, c, :], sh2B[c])
    if layer_kind == "even":
        gns = P.sb("gns", [128, 512]); P.dma(gns, gn)
    else:
        sks = P.sb("sks", [128, 8]); P.dma(sks, sinkB); lngs = P.sb("lngs", [128, 512]); P.dma(lngs, lngB); lnbs = P.sb("lnbs", [128, 512]); P.dma(lnbs, lnbB)
        gis = [P.sb(f"gis{i}", [128, 512]) for i in range(2)]; bgs = [P.sb(f"bgs{i}", [128, 512]) for i in range(2)]
        nqs = [P.sb(f"nqs{i}", [128, 8]) for i in range(2)]; mu8 = P.sb("mu8", [128, 8]); var8 = P.sb("var8", [128, 8]); den = P.sb("den", [128, 8])
    xs = [P.sb(f"xs{i}", [128, 1024]) for i in range(2)]
    oa = [P.sb(f"oa{i}", [128, 8, 65]) for i in range(2)]
    orr = [P.sb(f"orr{i}", [128, 2, 512]) for i in range(2)]
    rgs = [P.sb(f"rgs{i}", [128, 512]) for i in range(2)]
    cat = P.sb("cat", [128, 1024], BF16); catT = P.sb("catT", [128, 1024], BF16)
    rc = P.sb("rc", [128, 8]); osum = P.sb("osum", [128, 512]); mu = P.sb("mu", [128, 4]); var = P.sb("var", [128, 4])
    junk = P.sb("junk", [128, 1024]); tmp = P.sb("tmp", [128, 1024]); sg = P.sb("sg", [128, 512])
    xm = [P.sb(f"xm{i}", [128, 1024]) for i in range(2)]
    hf = P.sb("hf", [128, 1024]); hb = [P.sb(f"hb{i}", [128, 1024], BF16) for i in range(2)]
    hT = P.sb("hT", [128, 1024]); rs = P.sb("rs", [128, 1])
    lg = P.sb("lg", [128, 16]); mx = P.sb("mx", [128, 1]); sm = P.sb("sm", [128, 1]); af = [P.sb(f"af{i}", [128, 16]) for i in range(2)]
    pT = P.ps("pT", [128, 1024], BF16)
    pTf = [P.ps(f"pTf{i}", [128, 512]) for i in range(2)]
    pp = [P.ps(f"pp{i}", [128, 512]) for i in range(2)]
    pl = P.ps("pl", [128, 16])
    for t in range(T):
        cls = 1 if (t == 0 and has_ctx) else 0
        i2 = t % 2
        rows = slice(t * 128, (t + 1) * 128)
        P.dma(xs[i2], x[rows, :])
        if layer_kind == "even":
            P.dma(oa[i2], Oatt[rows]); P.dma(orr[i2], Oret[rows]); P.dma(rgs[i2], RG[rows, :])
            P.recip(rc, oa[i2][:, :, 64])
            P.tt(cat[:, 0:512].re("p (h d) -> p h d", d=64), oa[i2][:, :, 0:64], V(rc.key, rc.ap.unsqueeze(2).to_broadcast([128, 8, 64])), ALU.mult)
            P.tt(osum, orr[i2][:, 0, :], orr[i2][:, 1, :], ALU.add)
            ov = osum.re("p (h d) -> p h d", d=128)
            P.reduce(mu, ov, ALU.add)
            P.ts(mu, mu, 1.0 / 128, ALU.mult)
            P.tt(ov, ov, V(mu.key, mu.ap.unsqueeze(2).to_broadcast([128, 4, 128])), ALU.subtract)
            P.tt(junk[:, :512], osum, osum, ALU.mult)
            P.reduce(var, junk[:, :512].re("p (h d) -> p h d", d=128), ALU.add)
            P.ts(var, var, 1.0 / 128, ALU.mult, 1e-5, ALU.add)
            P.act(var, var, AF.Sqrt)
            P.recip(var, var)
            P.tt(ov, ov, V(var.key, var.ap.unsqueeze(2).to_broadcast([128, 4, 128])), ALU.mult)
            P.tt(osum, osum, gns, ALU.mult)
            P.act(sg, rgs[i2], AF.Sigmoid)
            P.tt(sg, sg, rgs[i2], ALU.mult)
            P.tt(cat[:, 512:1024], osum, sg, ALU.mult)
        else:
            P.dma(orr[i2], Yr[rows]); P.dma(gis[i2], Gi[rows, :]); P.dma(bgs[i2], BGi[rows, :]); P.dma(oa[i2], Ow[rows]); P.dma(nqs[i2], NQi[rows, :])
            P.tt(osum, orr[i2][:, 0, :], orr[i2][:, 1, :], ALU.add)
            ov = osum.re("p (h d) -> p h d", d=64)
            P.reduce(mu8, ov, ALU.add)
            P.ts(mu8, mu8, 1.0 / 64, ALU.mult)
            P.tt(ov, ov, V(mu8.key, mu8.ap.unsqueeze(2).to_broadcast([128, 8, 64])), ALU.subtract)
            P.tt(junk[:, :512], osum, osum, ALU.mult)
            P.reduce(var8, junk[:, :512].re("p (h d) -> p h d", d=64), ALU.add)
            P.ts(var8, var8, 1.0 / 64, ALU.mult, 64e-5, ALU.add)
            P.act(var8, var8, AF.Sqrt)
            P.recip(var8, var8)
            P.tt(ov, ov, V(var8.key, var8.ap.unsqueeze(2).to_broadcast([128, 8, 64])), ALU.mult)
            P.tt(osum, osum, lngs, ALU.mult)
            P.tt(osum, osum, lnbs, ALU.add)
            P.tt(osum, osum, gis[i2], ALU.mult)
            P.tt(cat[:, 0:512], osum, bgs[i2], ALU.add)
            P.tt(den, nqs[i2], sks, ALU.add)
            P.act(den, den, AF.Exp)
            P.tt(den, den, oa[i2][:, :, 64], ALU.add)
            P.recip(rc, den)
            P.tt(cat[:, 512:1024].re("p (h d) -> p h d", d=64), oa[i2][:, :, 0:64], V(rc.key, rc.ap.unsqueeze(2).to_broadcast([128, 8, 64])), ALU.mult)
        for k in range(8):
            P.tr(pT[:, k * 128:(k + 1) * 128], cat[:, k * 128:(k + 1) * 128], ident)
        P.copy(catT, pT)
        for j in range(2):
            for k in range(8):
                P.mm(pp[j], catT[:, k * 128:(k + 1) * 128], w_out_b[:, k, j * 512:(j + 1) * 512], start=(k == 0), stop=(k == 7))
            P.tt(tmp[:, j * 512:(j + 1) * 512], pp[j], G1[:, cls, j * 512:(j + 1) * 512], ALU.mult)
        P.tt(xm[i2], tmp, xs[i2], ALU.add)
        P.dma(xmid_o[rows, :], xm[i2])
        rms_rstd(P, rs, junk, xm[i2], 1024)
        P.stt(tmp, xm[i2], rs, Gp[:, cls, :], ALU.mult, ALU.mult)
        P.tt(hf, tmp, SH[:, cls, :], ALU.add)
        P.copy(hb[i2], hf, eng="act")
        P.dma(h_o[rows, :], hb[i2])
        for k in range(8):
            P.tr(pTf[k // 4][:, (k % 4) * 128:(k % 4 + 1) * 128], hf[:, k * 128:(k + 1) * 128], identf)
        for j in range(2):
            P.copy(hT[:, j * 512:(j + 1) * 512], pTf[j], eng=("act" if j else "dve"))
        for k in range(8):
            P.mm(pl, hT[:, k * 128:(k + 1) * 128], wr[:, k, :], start=(k == 0), stop=(k == 7))
        P.copy(lg, pl)
        P.reduce(mx, lg, ALU.max)
        P.ts(lg, lg, mx, ALU.subtract)
        P.act(lg, lg, AF.Exp)
        P.reduce(sm, lg, ALU.add)
        P.recip(sm, sm)
        P.ts(af[i2], lg, sm, ALU.mult)
        P.dma(aff_o[rows, :], af[i2])
    return P


BIG = 4000000.0

def k5_build(probs=((128, 2048), (2, 32)), NE=4, NIT=34):
    P = Prog()
    nc = P.nc
    ins = []
    for pi, (J, cap) in enumerate(probs):
        ins.append((P.inp(f"aff{pi}", [128, J, NE]), P.inp(f"h{pi}", [128, J, 1024], BF16),
                    P.out(f"ys{pi}", [NE, cap, 1024], BF16), P.out(f"dest{pi}", [128, J, NE], I32), P.out(f"gsel{pi}", [128, J, NE]),
                    [P.scratch(f"xs{pi}_{e}", [cap, 1024], BF16) for e in range(NE)]))
    wg = P.inp("wg", [NE, 128, 8, 1536]); wu = P.inp("wu", [NE, 128, 8, 1536]); wd = P.inp("wd", [NE, 128, 12, 1024])
    trid = P.inp("tri", [128, 128]); onesd = P.inp("ones", [128, 128]); identd = P.inp("ident", [128, 128], BF16)
    tri = P.sb("tri_s", [128, 128]); ones = P.sb("ones_s", [128, 128]); ident = P.sb("ident_s", [128, 128], BF16)
    P.dma(tri, trid); P.dma(ones, onesd); P.dma(ident, identd)
    JM = max(j for j, _ in probs)
    onesJ = P.sb("onesJ", [128, JM]); P.memset(onesJ, 1.0)
    af = P.sb("af", [128, JM, NE]); cmpb = P.sb("cmpb", [128, JM, NE]); cs = P.sb("cs", [128, JM, NE])
    lo = P.sb("lo", [128, NE]); hi = P.sb("hi", [128, NE]); mid = P.sb("mid", [128, NE]); cntp = P.sb("cntp", [128, NE])
    pred = P.sb("pred", [128, NE]); d1 = P.sb("d1", [128, NE]); d2 = P.sb("d2", [128, NE]); offs = P.sb("offs", [128, NE])
    desti = [P.sb(f"desti{pi}", [128, J, NE], I32) for pi, (J, cap) in enumerate(probs)]
    pt = P.ps("pt", [128, NE])
    hrow = [P.sb(f"hrow{i}", [128, 1024], BF16) for i in range(3)]
    for pi, (J, cap) in enumerate(probs):
        aff_i, h_i, ys_o, dest_o, gsel_o, xs = ins[pi]
        a = af[:, :J, :]; cb = cmpb[:, :J, :]; c_ = cs[:, :J, :]
        P.dma(a, aff_i)
        P.memset(lo, 0.0); P.memset(hi, 1.5)
        def bcj(v): return V(v.key, v.ap.unsqueeze(1).to_broadcast([128, J, NE]))
        for it in range(NIT):
            P.tt(mid, lo, hi, ALU.add)
            P.ts(mid, mid, 0.5, ALU.mult)
            P.tt(cb, a, bcj(mid), ALU.is_ge)
            P.reduce(cntp, cb.re("p j e -> p e j"), ALU.add)
            P.mm(pt, ones, cntp)
            P.ts(pred, pt, cap - 0.5, ALU.is_ge)
            P.tt(d1, mid, lo, ALU.subtract)
            P.tt(d1, d1, pred, ALU.mult)
            P.tt(d2, hi, mid, ALU.subtract)
            P.tt(d2, d2, pred, ALU.mult)
            P.tt(lo, lo, d1, ALU.add)
            P.tt(hi, mid, d2, ALU.add)
        P.tt(cb, a, bcj(lo), ALU.is_ge)
        P.reduce(cntp, cb.re("p j e -> p e j"), ALU.add)
        P.mm(pt, tri, cntp)
        P.ts(offs, pt, -(1.0 + BIG), ALU.add)
        for e in range(NE):
            P.scan(c_[:, :, e], onesJ[:, :J], cb[:, :, e], 0.0, ALU.mult, ALU.add)
        P.tt(c_, c_, bcj(offs), ALU.add)
        P.tt(c_, c_, cb, ALU.mult)
        P.ts(c_, c_, BIG, ALU.add)
        P.copy(desti[pi], c_)
        P.dma(dest_o, desti[pi])
        P.tt(cb, cb, a, ALU.mult)
        P.dma(gsel_o, cb)
        for j in range(J):
            hr = hrow[j % 3]
            P.dma(hr, h_i[:, j, :])
            for e in range(NE):
                idx = desti[pi][:, j, e:e + 1]
                P.op("pool", (lambda g, e=e, idx=idx, hr=hr, xs=xs, cap=cap: g.indirect_dma_start(
                    out=xs[e].ap, out_offset=bass.IndirectOffsetOnAxis(ap=idx.ap, axis=0), in_=hr.ap, in_offset=None,
                    bounds_check=P.bcreg(g, cap - 1), oob_is_err=False)), [hr, idx], [xs[e]], kind="d")
    stage = P.sb("stage", [128, 1536])
    wgb = P.sb("wgb", [128, 8, 1536], BF16); wub = P.sb("wub", [128, 8, 1536], BF16); wdb = P.sb("wdb", [128, 12, 1024], BF16)
    xr = [P.sb(f"xr{i}", [128, 1024], BF16) for i in range(2)]
    xT = P.sb("xT", [128, 8, 512], BF16)
    uT = P.sb("uT", [128, 12, 512], BF16)
    sgt = [P.sb(f"sgt{i}", [128, 512]) for i in range(2)]
    yb = [P.sb(f"yb{i}", [128, 1024], BF16) for i in range(2)]
    pT = P.ps("pT", [128, 1024], BF16)
    pg = [P.ps(f"pg{i}", [128, 512]) for i in range(2)]
    pu = [P.ps(f"pu{i}", [128, 512]) for i in range(2)]
    py = [P.ps(f"py{i}", [128, 512]) for i in range(2)]
    n = 0
    for e in range(NE):
        load_w_bf16(P, wgb, wg[e], 8, 1536, stage)
        load_w_bf16(P, wub, wu[e], 8, 1536, stage)
        load_w_bf16(P, wdb, wd[e], 12, 1024, stage)
        for pi, (J, cap) in enumerate(probs):
            aff_i, h_i, ys_o, dest_o, gsel_o, xs = ins[pi]
            for g0 in range(0, cap, 512):
                gw = min(512, cap - g0)
                nblk = (gw + 127) // 128
                for bi in range(nblk):
                    r0 = g0 + bi * 128; rw = min(128, cap - r0)
                    x_ = xr[bi % 2]
                    P.dma(x_[:rw, :], xs[e][r0:r0 + rw, :])
                    for k in range(8):
                        P.tr(pT[:, k * 128:k * 128 + rw], x_[:rw, k * 128:(k + 1) * 128], ident[:rw, :rw])
                    P.copy(xT[:, :, bi * 128:bi * 128 + rw], pT.re("p (k t) -> p k t", k=8)[:, :, :rw], eng=("act" if bi % 2 else "dve"))
                for fc in range(12):
                    i2 = n % 2; n += 1
                    for k in range(8):
                        P.mm(pg[i2][:, :gw], wgb[:, k, fc * 128:(fc + 1) * 128], xT[:, k, :gw], start=(k == 0), stop=(k == 7))
                    for k in range(8):
                        P.mm(pu[i2][:, :gw], wub[:, k, fc * 128:(fc + 1) * 128], xT[:, k, :gw], start=(k == 0), stop=(k == 7))
                    P.act(sgt[i2][:, :gw], pg[i2][:, :gw], AF.Sigmoid)
                    P.tt(sgt[i2][:, :gw], sgt[i2][:, :gw], pg[i2][:, :gw], ALU.mult)
                    P.tt(uT[:, fc, :gw], sgt[i2][:, :gw], pu[i2][:, :gw], ALU.mult)
                for bi in range(nblk):
                    r0 = g0 + bi * 128; rw = min(128, cap - r0)
                    y_ = yb[bi % 2]
                    for j in range(2):
                        for fc in range(12):
                            P.mm(py[j][:rw, :], uT[:, fc, bi * 128:bi * 128 + rw], wdb[:, fc, j * 512:(j + 1) * 512], start=(fc == 0), stop=(fc == 11))
                        P.copy(y_[:rw, j * 512:(j + 1) * 512], py[j][:rw, :], eng=("act" if j else "dve"))
                    P.dma(ys_o[e, r0:r0 + rw, :], y_[:rw, :])
    return P


def k6_build(T, caps=(2048, 32), final=False, NE=16):
    P = Prog()
    xmid = P.inp("xmid", [T * 128, 1024])
    gsel = P.inp("gsel", [T * 128, NE])
    dest = P.inp("dest", [T * 128, NE], I32)
    ys = [[P.inp(f"ys{c}_{e}", [caps[c], 1024], BF16) for e in range(NE)] for c in range(len(caps))]
    g2B = P.inp("g2B", [2, 128, 1024])
    xo = P.out("xo", [T * 128, 1024])
    G2 = P.sb("G2", [128, 2, 1024])
    for c in range(2):
        P.dma(G2[:, c, :], g2B[c])
    if final:
        fgB = P.inp("fgB", [128, 1024]); fg = P.sb("fg", [128, 1024]); P.dma(fg, fgB)
        fo = P.out("fo", [T * 128, 1024])
        junk = P.sb("junk", [128, 1024]); rs = P.sb("rs", [128, 1])
    xs = [P.sb(f"xs{i}", [128, 1024]) for i in range(2)]
    gs = [P.sb(f"gs{i}", [128, NE]) for i in range(2)]
    ds = [P.sb(f"ds{i}", [128, NE], I32) for i in range(2)]
    gt = [P.sb(f"gt{i}", [128, 1024], BF16) for i in range(4)]
    for g in gt:
        P.memset(g, 0.0)
    acc = P.sb("acc", [128, 1024])
    ob = [P.sb(f"ob{i}", [128, 1024]) for i in range(2)]
    fb = [P.sb(f"fb{i}", [128, 1024]) for i in range(2)]
    n = 0
    for t in range(T):
        cls = 1 if (t == 0 and len(caps) > 1) else 0
        i2 = t % 2
        rows = slice(t * 128, (t + 1) * 128)
        P.dma(xs[i2], xmid[rows, :]); P.dma(gs[i2], gsel[rows, :]); P.dma(ds[i2], dest[rows, :])
        P.memset(acc, 0.0)
        for e in range(NE):
            g = gt[n % 4]; n += 1
            idx = ds[i2][:, e:e + 1]
            src = ys[cls][e]
            P.op("pool", (lambda q, e=e, idx=idx, g=g, src=src, cap=caps[cls]: q.indirect_dma_start(
                out=g.ap, out_offset=None, in_=src.ap, in_offset=bass.IndirectOffsetOnAxis(ap=idx.ap, axis=0),
                bounds_check=P.bcreg(q, cap - 1), oob_is_err=False)), [src, idx], [g], kind="d")
            P.stt(acc, g, gs[i2][:, e:e + 1], acc, ALU.mult, ALU.add)
        P.tt(acc, acc, G2[:, cls, :], ALU.mult)
        P.tt(ob[i2], acc, xs[i2], ALU.add)
        P.dma(xo[rows, :], ob[i2])
        if final:
            rms_rstd(P, rs, junk, ob[i2], 1024)
            P.stt(fb[i2], ob[i2], rs, fg, ALU.mult, ALU.mult)
            P.dma(fo[rows, :], fb[i2])
    return P


SQW = 64 ** -0.25
RW_CH = [(0, 512), (512, 1024), (1024, 1536), (1536, 1920)]
WIN_CH = [(1920, 2432), (2432, 2688)]

def k7_build(T):
    P = Prog()
    x = P.inp("x", [T * 128, 1024]); xp = P.inp("xp", [T * 128, 1024]); xn = P.inp("xn", [T * 128, 1024]); fl = P.inp("fl", [T * 128, 2])
    gB = P.inp("gB", [128, 1024]); scB = P.inp("scB", [2, 128, 1024]); shB = P.inp("shB", [2, 128, 1024])
    w_in = P.inp("w_in", [128, 8, 2688]); muB = P.inp("muB", [128, 1920])
    kkB = P.inp("kkB", [128, 512]); kaB = P.inp("kaB", [128, 512]); rkB = P.inp("rkB", [128, 512])
    w0B = P.inp("w0B", [2, 128, 512]); a0B = P.inp("a0B", [2, 128, 512])
    w2s = P.inp("w2s", [128, 1, 512]); a2s = P.inp("a2s", [128, 1, 512]); g2s = P.inp("g2s", [128, 1, 512])
    Cw = P.inp("Cw", [T * 128, 64]); Sw = P.inp("Sw", [T * 128, 64])
    identd = P.inp("ident", [128, 128], BF16)
    onames = ["R", "Vv", "KK", "LWf", "LWb", "Af", "Ab", "Kf", "Kb", "G", "BG"]
    outs = {n: P.out(n, [T * 128, 512]) for n in onames}
    NQ_o = P.out("NQ", [T * 128, 8])
    Qw_o = P.out("Qw", [T * 128, 8 * 66], BF16); Kw_o = P.out("Kw", [T * 128, 2 * 66], BF16); Vw_o = P.out("Vw", [T * 128, 2 * 65], BF16)
    ident = P.sb("ident_s", [128, 128], BF16); P.dma(ident, identd)
    stage = P.sb("stage", [128, 2688])
    Pm = P.sb("Pm", [128, 2688]); tmpw = Pm[:, :1920]
    mus = P.sb("mus", [128, 1920]); P.dma(mus, muB)
    W1 = P.sb("W1", [128, 8, 1920], BF16); W2 = P.sb("W2", [128, 8, 1920], BF16); Ww = P.sb("Ww", [128, 8, 768], BF16)
    for k in range(8):
        P.dma(stage, w_in[:, k, :])
        P.tt(tmpw, stage[:, :1920], mus, ALU.mult)
        P.copy(W2[:, k, :], tmpw, eng="act")
        P.tt(W1[:, k, :], stage[:, :1920], tmpw, ALU.subtract)
        P.copy(Ww[:, k, :], stage[:, 1920:2688], eng="act")
    w2b = P.sb("w2b", [128, 1, 512], BF16); a2b = P.sb("a2b", [128, 1, 512], BF16); g2b = P.sb("g2b", [128, 1, 512], BF16)
    load_w_bf16(P, w2b, w2s, 1, 512, stage); load_w_bf16(P, a2b, a2s, 1, 512, stage); load_w_bf16(P, g2b, g2s, 1, 512, stage)
    cB = {}
    for n, d in (("kk", kkB), ("ka", kaB), ("rk", rkB)):
        cB[n] = P.sb(n + "_s", [128, 512]); P.dma(cB[n], d)
    w0s = P.sb("w0s", [128, 2, 512]); a0s = P.sb("a0s", [128, 2, 512])
    for d in range(2):
        P.dma(w0s[:, d, :], w0B[d]); P.dma(a0s[:, d, :], a0B[d])
    gs = P.sb("gs", [128, 1024]); P.dma(gs, gB)
    Gp = P.sb("Gp", [128, 2, 1024]); SH = P.sb("SHt", [128, 2, 1024])
    for c in range(2):
        P.dma(stage[:, :1024], scB[c])
        P.stt(Gp[:, c, :], stage[:, :1024], 1.0, gs, ALU.add, ALU.mult)
        P.dma(SH[:, c, :], shB[c])
    xs = [[P.sb(f"xs{i}_{j}", [128, 1024]) for j in range(3)] for i in range(1)] * 2
    fls = [P.sb(f"fls{i}", [128, 2]) for i in range(2)]
    tabs = [[P.sb(f"tab{i}_{j}", [128, 64]) for j in range(2)] for i in range(2)]
    junk = P.sb("junk", [128, 1024]); tmp = P.sb("tmp", [128, 1024]); rs = P.sb("rs", [128, 1])
    a_bf = P.sb("a_bf", [128, 1024], BF16); af32 = [P.sb(f"af32_{j}", [128, 1024]) for j in range(2)]
    ash = P.sb("ash", [128, 1024], BF16)
    aT = P.sb("aT", [128, 1024], BF16); ashT = P.sb("ashT", [128, 1024], BF16)
    sm = {n: P.sb("sm_" + n, [128, 8]) for n in ("ss", "srk", "nq")}
    nk2 = P.sb("nk2", [128, 2]); ek2 = P.sb("ek2", [128, 2])
    bft = {n: P.sb("bf_" + n, [128, 128], BF16) for n in ("th", "al", "sg")}
    bfT = {n: P.sb("bfT_" + n, [128, 128], BF16) for n in ("th", "al", "sg")}
    ob = {n: [P.sb(f"ob_{n}{i}", [128, 512]) for i in range(1)] * 2 for n in onames if n not in ("R", "Vv")}
    t5 = P.sb("t5", [128, 512]); t6 = P.sb("t6", [128, 512]); qf = P.sb("qf", [128, 512]); kf = P.sb("kf", [128, 128])
    NQb = [P.sb(f"NQb{i}", [128, 8]) for i in range(2)]
    Qw = [P.sb(f"Qw{i}", [128, 8, 66], BF16) for i in range(2)]
    Kw = [P.sb(f"Kw{i}", [128, 2, 66], BF16) for i in range(2)]
    Vw = [P.sb(f"Vw{i}", [128, 2, 65], BF16) for i in range(2)]
    pT = P.ps("pT", [128, 1024], BF16)
    pp = [P.ps(f"pp{i}", [128, 512]) for i in range(6)]
    pcnt = [0]
    def npp():
        p = pp[pcnt[0] % 6]; pcnt[0] += 1
        return p

    def norm_mod(dst, xt, cls):
        rms_rstd(P, rs, junk, xt, 1024)
        P.stt(tmp, xt, rs, Gp[:, cls, :], ALU.mult, ALU.mult)
        P.tt(dst, tmp, SH[:, cls, :], ALU.add)

    def rope(dst, src, nh, tb, scale):
        w = nh * 64
        Cb = V(tb[0].key, tb[0].ap.unsqueeze(1).to_broadcast([128, nh, 64]))
        t1v = t5[:, :w].re("p (h d) -> p h d", d=64); t2v = t6[:, :w].re("p (h d) -> p h d", d=64)
        P.tt(t1v, src, Cb, ALU.mult)
        s5 = src.re("p h (b f e) -> p h b f e", b=2, f=2); t25 = t2v.re("p h (b f e) -> p h b f e", b=2, f=2)
        S5 = tb[1].ap.rearrange("p (b f e) -> p b f e", b=2, f=2)
        for f in range(2):
            Sb = V(tb[1].key, S5[:, :, f, :].unsqueeze(1).to_broadcast([128, nh, 2, 16]))
            P.tt(t25[:, :, :, f, :], s5[:, :, :, 1 - f, :], Sb, ALU.mult)
        P.tt(t1v, t1v, t2v, ALU.add)
        P.ts(dst, t1v, scale, ALU.mult)

    for t in range(T):
        cls = 1 if t == 0 else 0
        i2 = t % 2
        rows = slice(t * 128, (t + 1) * 128)
        xt, xpt, xnt = xs[i2]
        P.dma(xt, x[rows, :]); P.dma(xpt, xp[rows, :]); P.dma(xnt, xn[rows, :]); P.dma(fls[i2], fl[rows, :])
        tb = tabs[i2]
        P.dma(tb[0], Cw[rows, :]); P.dma(tb[1], Sw[rows, :])
        norm_mod(a_bf, xt, cls)
        norm_mod(af32[0], xpt, cls)
        norm_mod(af32[1], xnt, cls)
        P.ts(af32[0], af32[0], fls[i2][:, 0:1], ALU.mult, 0.5, ALU.mult)
        P.ts(af32[1], af32[1], fls[i2][:, 1:2], ALU.mult, 0.5, ALU.mult)
        P.tt(ash, af32[0], af32[1], ALU.add)
        for k in range(8):
            P.tr(pT[:, k * 128:(k + 1) * 128], a_bf[:, k * 128:(k + 1) * 128], ident)
        P.copy(aT, pT)
        for k in range(8):
            P.tr(pT[:, k * 128:(k + 1) * 128], ash[:, k * 128:(k + 1) * 128], ident)
        P.copy(ashT, pT)
        for j, (c0, c1) in enumerate(RW_CH):
            p = npp()
            for k in range(8):
                P.mm(p[:, :c1 - c0], aT[:, k * 128:(k + 1) * 128], W1[:, k, c0:c1], start=(k == 0), stop=False)
            for k in range(8):
                P.mm(p[:, :c1 - c0], ashT[:, k * 128:(k + 1) * 128], W2[:, k, c0:c1], start=False, stop=(k == 7))
            P.copy(Pm[:, c0:c1], p[:, :c1 - c0], eng=("act" if j % 2 else "dve"))
        for j, (c0, c1) in enumerate(WIN_CH):
            p = npp()
            for k in range(8):
                P.mm(p[:, :c1 - c0], aT[:, k * 128:(k + 1) * 128], Ww[:, k, c0 - 1920:c1 - 1920], start=(k == 0), stop=(k == 7))
            P.copy(Pm[:, c0:c1], p[:, :c1 - c0], eng=("act" if j % 2 else "dve"))
        O = {n: ob[n][i2] for n in onames if n not in ("R", "Vv")}
        r_, k_, v_ = Pm[:, 0:512], Pm[:, 512:1024], Pm[:, 1024:1536]
        O["R"] = r_; O["Vv"] = v_
        P.tt(t5, k_, cB["kk"], ALU.mult)
        P.tt(t6, t5, t5, ALU.mult)
        P.reduce(sm["ss"], t6.re("p (h d) -> p h d", d=64), ALU.add)
        P.ts(sm["ss"], sm["ss"], 1e-12, ALU.add)
        P.act(sm["ss"], sm["ss"], AF.Sqrt)
        P.recip(sm["ss"], sm["ss"])
        P.tt(O["KK"].re("p (h d) -> p h d", d=64), t5.re("p (h d) -> p h d", d=64), V(sm["ss"].key, sm["ss"].ap.unsqueeze(2).to_broadcast([128, 8, 64])), ALU.mult)
        P.act(bft["th"], Pm[:, 1536:1664], AF.Tanh)
        P.copy(bft["al"], Pm[:, 1664:1792])
        P.act(bft["sg"], Pm[:, 1792:1920], AF.Sigmoid)
        for i, n in enumerate(("th", "al", "sg")):
            P.tr(pT[:, i * 128:(i + 1) * 128], bft[n], ident)
            P.copy(bfT[n], pT[:, i * 128:(i + 1) * 128], eng=("act" if i % 2 else "dve"))
        for d, (lw_n, a_n, k_n) in enumerate((("LWf", "Af", "Kf"), ("LWb", "Ab", "Kb"))):
            ps_ = slice(d * 64, (d + 1) * 64)
            p = npp(); P.mm(p, bfT["th"][ps_, :], w2b[ps_, 0, :])
            P.tt(t5, p, w0s[:, d, :], ALU.add)
            P.act(t5, t5, AF.Sigmoid)
            P.ts(O[lw_n], t5, -0.6065306597126334, ALU.mult)
            p = npp(); P.mm(p, bfT["al"][ps_, :], a2b[ps_, 0, :])
            P.tt(t5, p, a0s[:, d, :], ALU.add)
            P.act(O[a_n], t5, AF.Sigmoid)
            P.stt(t6, O[a_n], -1.0, cB["ka"], ALU.add, ALU.mult)
            P.stt(O[k_n], t6, 1.0, k_, ALU.add, ALU.mult)
        p = npp(); P.mm(p, bfT["sg"], g2b[:, 0, :])
        P.copy(O["G"], p, eng="act")
        P.tt(t5, r_, k_, ALU.mult)
        P.tt(t5, t5, cB["rk"], ALU.mult)
        P.reduce(sm["srk"], t5.re("p (h d) -> p h d", d=64), ALU.add)
        P.tt(t6.re("p (h d) -> p h d", d=64), v_.re("p (h d) -> p h d", d=64), V(sm["srk"].key, sm["srk"].ap.unsqueeze(2).to_broadcast([128, 8, 64])), ALU.mult)
        P.tt(O["BG"], t6, O["G"], ALU.mult)
        for n in onames:
            P.dma(outs[n][rows, :], O[n])
        qv = qf.re("p (h d) -> p h d", d=64)
        rope(qv, Pm[:, 1920:2432].re("p (h d) -> p h d", d=64), 8, tb, SQW)
        Qt = Qw[i2]
        P.copy(Qt[:, :, 0:64], qv, eng="act")
        P.tt(t5, qf, qf, ALU.mult)
        P.reduce(sm["nq"], t5.re("p (h d) -> p h d", d=64), ALU.add)
        P.ts(NQb[i2], sm["nq"], -0.5, ALU.mult)
        P.copy(Qt[:, :, 64], NQb[i2])
        P.memset(Qt[:, :, 65], 1.0)
        P.dma(NQ_o[rows, :], NQb[i2])
        P.dma(Qw_o[rows, :], Qt.re("p h d -> p (h d)"))
        kv = kf.re("p (h d) -> p h d", d=64)
        rope(kv, Pm[:, 2432:2560].re("p (h d) -> p h d", d=64), 2, tb, SQW)
        Kt = Kw[i2]; Vt = Vw[i2]
        P.copy(Kt[:, :, 0:64], kv, eng="act")
        P.memset(Kt[:, :, 64], 1.0)
        P.tt(t5[:, :128], kf, kf, ALU.mult)
        P.reduce(nk2, t5[:, :128].re("p (h d) -> p h d", d=64), ALU.add)
        P.ts(Kt[:, :, 65], nk2, -0.5, ALU.mult)
        P.act(ek2, Kt[:, :, 65], AF.Exp, scale=-1.0)
        P.tt(Vt[:, :, 0:64], Pm[:, 2560:2688].re("p (h d) -> p h d", d=64), V(ek2.key, ek2.ap.unsqueeze(2).to_broadcast([128, 2, 64])), ALU.mult)
        P.copy(Vt[:, :, 64], ek2)
        P.dma(Kw_o[rows, :], Kt.re("p h d -> p (h d)"))
        P.dma(Vw_o[rows, :], Vt.re("p h d -> p (h d)"))
    return P


L = 64

def k8_consts():
    s = np.arange(L)[:, None]; t = np.arange(L)[None, :]
    t4 = lambda m: np.ascontiguousarray(np.tile(m.astype(np.float32), (1, 4)))
    return {"triI": (s <= t).astype(np.float32), "ones": np.ones((L, L), np.float32), "mS": t4(s < t), "mST": t4(s > t), "mI": t4(s <= t),
            "I4": t4(np.eye(L)), "identf": np.eye(L, dtype=np.float32)}

def k8_build(NCH, NCTX):
    P = Prog()
    names = ["R", "K", "Vv", "KK", "A", "LW"]
    din = {n: P.inp(n, [NCH, L, 256]) for n in names}
    yo = P.out("y", [NCH - NCTX, L, 256])
    cst = {}
    for n, w in (("triI", 64), ("ones", 64), ("mS", 256), ("mST", 256), ("mI", 256), ("I4", 256), ("identf", 64)):
        d = P.inp(n, [L, w]); cst[n] = P.sb(n + "_s", [L, w]); P.dma(cst[n], d)
    tl = {}
    def T_(n, w=256, nb=1):
        if n not in tl:
            tl[n] = [P.sb(f"{n}{i}", [L, w]) for i in range(nb)]
        return tl[n]
    pss = [P.ps(f"ps{i}", [L, 512]) for i in range(8)]
    pc = [0]
    def nps():
        p = pss[pc[0] % 8]; pc[0] += 1
        return p[:, :256]
    H = [slice(h * 64, (h + 1) * 64) for h in range(4)]
    def mm4(ps, A_, B_, start=True, stop=True):
        for h in range(4):
            P.mm(ps[:, H[h]], A_[:, H[h]], B_[:, H[h]], start=start, stop=stop)
    ST = P.sb("ST", [L, 256]); P.memset(ST, 0.0)
    for c in range(NCH):
        i2 = c % 2
        X = {}
        for n in names:
            X[n] = T_("in_" + n, nb=2)[i2]
            P.dma(X[n], din[n][c])
        R, K, Vv, KK, A, LW = (X[n] for n in names)
        pA = nps(); P.mm(pA, cst["triI"], LW)
        pB = nps(); P.mm(pB, cst["ones"], LW)
        cum = T_("cum")[0]; P.copy(cum, pA)
        eP = T_("eP")[0]; P.act(eP, cum, AF.Exp)
        eN = T_("eN")[0]; P.act(eN, cum, AF.Exp, scale=-1.0)
        t1 = T_("t1")[0]; P.tt(t1, cum, LW, ALU.subtract)
        ePx = T_("ePx")[0]; P.act(ePx, t1, AF.Exp)
        t2 = T_("t2")[0]; P.tt(t2, pB, cum, ALU.subtract)
        eT = T_("eT")[0]; P.act(eT, t2, AF.Exp)
        al = T_("al")[0]; P.stt(al, KK, -1.0, ePx, ALU.mult, ALU.mult)
        be = T_("be")[0]; P.tt(be, KK, A, ALU.mult)
        bcn = T_("bcn")[0]; P.tt(bcn, be, eN, ALU.mult)
        kc = T_("kc")[0]; P.tt(kc, K, eN, ALU.mult)
        rt = T_("rt")[0]; P.tt(rt, R, eP, ALU.mult)
        bh = T_("bh")[0]; P.tt(bh, be, eT, ALU.mult)
        kh = T_("kh")[0]; P.tt(kh, K, eT, ALU.mult)
        pPL = nps()
        for h in range(4):
            P.mm(pPL[:, h:h + 1], LW[:, H[h]], cst["ones"][:, 0:1])
        PL = T_("PL", 4)[0]; P.act(PL, pPL[:, 0:4], AF.Exp)
        TT = {}
        for n, src in (("alT", al), ("bcT", bcn), ("kcT", kc), ("rtT", rt)):
            p = nps()
            for h in range(4):
                P.tr(p[:, H[h]], src[:, H[h]], cst["identf"])
            TT[n] = T_(n)[0]; P.copy(TT[n], p, eng="act")
        def gram(name, a, b, mask):
            p = nps(); mm4(p, TT[a], TT[b])
            o = T_(name)[0]; P.tt(o, p, cst[mask], ALU.mult)
            return o
        M = gram("M0", "bcT", "alT", "mS")
        N_ = gram("N0", "alT", "bcT", "mST")
        AakT = gram("AakT", "kcT", "alT", "mS")
        ArbT = gram("ArbT", "bcT", "rtT", "mI")
        ArkT = gram("ArkT", "kcT", "rtT", "mI")
        Tt = T_("Tt")[0]; P.tt(Tt, M, cst["I4"], ALU.add)
        for i in range(5):
            pM = nps(); mm4(pM, N_, M)
            pN = nps(); mm4(pN, M, N_)
            Mn = T_(f"Mn{i % 2}")[0]; Nn = T_(f"Nn{i % 2}")[0]
            P.copy(Mn, pM, eng="act"); P.copy(Nn, pN)
            pX = nps(); mm4(pX, Nn, Tt)
            P.tt(Tt, Tt, pX, ALU.add)
            M, N_ = Mn, Nn
        p = nps(); mm4(p, AakT, Vv); AV = T_("AV")[0]; P.copy(AV, p, eng="act")
        p = nps(); mm4(p, Tt, AV); U0 = T_("U0")[0]; P.copy(U0, p)
        p = nps(); mm4(p, Tt, al); Ah = T_("Ah")[0]; P.copy(Ah, p, eng="act")
        p = nps(); mm4(p, Ah, bh); GT = T_("GT")[0]; P.copy(GT, p)
        p = nps(); mm4(p, Ah, ArbT); RhT = T_("RhT")[0]; P.tt(RhT, p, TT["rtT"], ALU.add)
        if c >= NCTX:
            p = nps()
            for h in range(4):
                P.mm(p[:, H[h]], ArbT[:, H[h]], U0[:, H[h]], start=True, stop=False)
                P.mm(p[:, H[h]], ArkT[:, H[h]], Vv[:, H[h]], start=False, stop=False)
                P.mm(p[:, H[h]], RhT[:, H[h]], ST[:, H[h]], start=False, stop=True)
            yb = T_("yb", nb=2)[i2]; P.copy(yb, p, eng="act")
            P.dma(yo[c - NCTX], yb)
        p = nps()
        for h in range(4):
            P.mm(p[:, H[h]], bh[:, H[h]], U0[:, H[h]], start=True, stop=False)
            P.mm(p[:, H[h]], kh[:, H[h]], Vv[:, H[h]], start=False, stop=False)
            P.mm(p[:, H[h]], GT[:, H[h]], ST[:, H[h]], start=False, stop=True)
        for h in range(4):
            P.stt(ST[:, H[h]], ST[:, H[h]], PL[:, h:h + 1], p[:, H[h]], ALU.mult, ALU.add)
    return P


def k9_consts():
    import numpy as np
    kj = np.arange(128)[:, None]; qi = np.arange(128)[None, :]
    mL = (kj >= qi).astype(np.float32)
    mR = (kj <= qi).astype(np.float32)
    t4 = lambda m: np.ascontiguousarray(np.tile(m, (1, 4)))
    return {"mL": t4(mL), "mR": t4(mR)}

def k9_build(NB=32):
    P = Prog()
    NKB = NB + 2
    QT = P.inp("QT", [2, 66, NB * 512], BF16)
    KT = P.inp("KT", [2, 66, NKB * 128], BF16)
    Vv = P.inp("Vv", [2, 128, NKB, 65], BF16)
    KTc = P.inp("KTc", [2, 66, 256], BF16); Vc = P.inp("Vc", [2, 128, 2, 65], BF16)
    mLd = P.inp("mL", [128, 512]); mRd = P.inp("mR", [128, 512])
    OT = P.out("OT", [2, 65, NB * 512])
    mL = P.sb("mL_s", [128, 512]); mR = P.sb("mR_s", [128, 512]); P.dma(mL, mLd); P.dma(mR, mRd)
    kT = P.sb("kT", [66, NKB * 128], BF16); vs = P.sb("vs", [128, NKB, 65], BF16)
    kTc = P.sb("kTc", [66, 256], BF16); vc = P.sb("vc", [128, 2, 65], BF16)
    qs = [P.sb(f"qs{i}", [66, 512], BF16) for i in range(2)]
    pTs = [P.sb(f"pT{i}", [128, 512], BF16) for i in range(4)]
    ef = [P.sb(f"ef{i}", [128, 512]) for i in range(2)]
    osb = [P.sb(f"osb{i}", [65, 512]) for i in range(2)]
    psS = [P.ps(f"psS{i}", [128, 512]) for i in range(4)]
    psO = [P.ps(f"psO{i}", [128, 512]) for i in range(2)]
    cnt = 0; blk = 0
    for kv in range(2):
        P.dma(kT, KT[kv]); P.dma(vs, Vv[kv]); P.dma(kTc, KTc[kv]); P.dma(vc, Vc[kv])
        for n in range(NB):
            q = qs[blk % 2]
            P.dma(q, QT[kv][:, n * 512:(n + 1) * 512])
            po = psO[blk % 2]
            tiles = [("c", 0), ("c", 1), ("L", n), ("C", n + 1), ("R", n + 2)]
            for ti, (kind, kb) in enumerate(tiles):
                s = psS[cnt % 4]; pt = pTs[cnt % 4]; cnt += 1
                if kind == "c":
                    P.mm(s, kTc[:, kb * 128:(kb + 1) * 128], q)
                else:
                    P.mm(s, kT[:, kb * 128:(kb + 1) * 128], q)
                if kind in ("L", "R"):
                    e = ef[ti % 2]
                    P.act(e, s, AF.Exp)
                    P.tt(pt, e, mL if kind == "L" else mR, ALU.mult)
                else:
                    P.act(pt, s, AF.Exp)
                vv = vc[:, kb, :] if kind == "c" else vs[:, kb, :]
                P.mm(po[0:65, :], vv, pt, start=(ti == 0), stop=(ti == len(tiles) - 1))
            o = osb[blk % 2]
            P.copy(o, po[0:65, :])
            P.dma(OT[kv][:, n * 512:(n + 1) * 512], o)
            blk += 1
    return P

import time, os, sys, numpy as np, ml_dtypes

bf = ml_dtypes.bfloat16
B, N, D, NCTX = 2, 16384, 1024, 256
T = 33
def bc(v): return np.ascontiguousarray(np.broadcast_to(np.asarray(v, np.float32), (128, len(v))))
def wl(w, k): return np.ascontiguousarray(w.reshape(k, 128, -1).transpose(1, 0, 2))
def pack(i, lat, ctx):
    b, q = i // 4, i % 4
    return np.ascontiguousarray(np.concatenate([ctx[b][(q % 2) * 128:(q % 2 + 1) * 128], lat[b][q * 4096:(q + 1) * 4096]], 0))
def unpack(outs, name):
    lat = np.stack([np.concatenate([outs[b * 4 + q][name][128:] for q in range(4)], 0) for b in range(B)])
    ctx = np.stack([np.concatenate([outs[b * 4 + q][name][:128] for q in range(2)], 0) for b in range(B)])
    return lat, ctx
def run(P, in_maps, tag=""):
    t = time.time(); nc = P.build(); tb = time.time() - t
    t = time.time(); res = run_bass_kernel_spmd(nc, in_maps, core_ids=list(range(8))); print(f"[{tag}] build {tb:.1f}s run {time.time()-t:.1f}s", flush=True)
    return res.results
def rope_tabs_mla(tpos):
    n = len(tpos); C = np.ones((n, 32), np.float32); S = np.zeros((n, 32), np.float32)
    half = 8
    inv = (10000.0 ** (-np.arange(half, dtype=np.float32) / half)).astype(np.float32)
    for blk, pos in enumerate((tpos // 64, tpos % 64)):
        ang = pos.astype(np.float32)[:, None] * inv[None, :]
        c = np.cos(ang).astype(np.float32); s = np.sin(ang).astype(np.float32)
        C[:, blk*16:blk*16+8] = c; C[:, blk*16+8:blk*16+16] = c
        S[:, blk*16:blk*16+8] = -s; S[:, blk*16+8:blk*16+16] = s
    return C, S
def rope_tabs_ret(pos):
    half = 64
    inv = (10000.0 ** (-np.arange(half, dtype=np.float32) / half)).astype(np.float32)
    ang = pos.astype(np.float32)[:, None] * inv[None, :]
    return np.cos(ang).astype(np.float32), np.sin(ang).astype(np.float32)
IDB = np.eye(128, dtype=np.float32).astype(bf); IDF = np.eye(128, dtype=np.float32)

def stage_mod(I):
    pass

def modulation_dev(I):

    cv = np.concatenate([I['c'], I['c_ctx'][None]], 0)
    cT = np.ascontiguousarray(cv.reshape(3, 8, 128).transpose(2, 1, 0))
    ims = []
    for i in range(8):
        l = i // 4; cs = (i % 4) * 1536
        ims.append({"cT": cT, "w": wl(np.ascontiguousarray(I['ada_w'][l][:, cs:cs+1536]), 8), "b": np.ascontiguousarray(np.broadcast_to(I['ada_b'][l][cs:cs+1536], (3, 1536)))})
    r = run(build_mod(), ims, "mod")
    out = np.zeros((2, 3, 6144), np.float32)
    for i in range(8): out[i//4][:, (i%4)*1536:(i%4+1)*1536] = r[i]["o"]
    return out

def layer0_pre(I, mod, x_lat, x_ctx):

    ims = []
    for i in range(8):
        b, q = i // 4, i % 4
        tl = q * 4096 + np.arange(4096); tc = (q % 2) * 128 + np.arange(128)
        Cl, Sl = rope_tabs_mla(tl); Cc, Sc = np.ones((128, 32), np.float32), np.zeros((128, 32), np.float32)
        Crl, Srl = rope_tabs_ret(256 + tl); Crc, Src = rope_tabs_ret(tc)
        Cr = np.concatenate([Crc, Crl]); Sr = np.concatenate([Src, Srl]); ks = np.float32(128 ** -0.5)
        m = mod[0]
        ims.append({"x": pack(i, x_lat, x_ctx), "gB": bc(I['norm_mix_g'][0]),
                    "scB": np.stack([bc(m[b][1024:2048]), bc(m[2][1024:2048])]), "shB": np.stack([bc(m[b][0:1024]), bc(m[2][0:1024])]),
                    "w_in": wl(I['ev_w_in'][0], 8), "w_uq": wl(I['mla_w_uq'][0], 2), "w_ukv": wl(I['mla_w_ukv'][0], 1),
                    "gq": bc(I['mla_q_norm_g'][0]), "gkv": bc(I['mla_kv_norm_g'][0]),
                    "Cq": np.concatenate([Cc, Cl]), "Sq": np.concatenate([Sc, Sl]), "Cr": Cr, "Sr": Sr, "Crk": Cr * ks, "Srk": Sr * ks, "ident": IDB})
    r = run(k1_build(T), ims, "k1")
    return {n: unpack(r, n) for n in ("Qa", "Ka", "Va", "RQ", "RK", "RV", "RG")}

def layer0_mla(o1):

    Qa_l, Qa_c = o1["Qa"]; Ka_l, Ka_c = o1["Ka"]; Va_l, Va_c = o1["Va"]
    Ka = np.concatenate([Ka_c, Ka_l], 1).reshape(B, 16640, 8, 98); Va = np.concatenate([Va_c, Va_l], 1).reshape(B, 16640, 8, 65)
    Ql = Qa_l.reshape(B, N, 8, 98); Qc = Qa_c.reshape(B, NCTX, 8, 98)
    ims = []
    for h in range(8):
        ims.append({"QT": np.ascontiguousarray(Ql[:, :, h, :].transpose(0, 2, 1)), "QTc": np.ascontiguousarray(Qc[:, :, h, :].transpose(0, 2, 1)),
                    "KT": np.ascontiguousarray(Ka[:, :, h, :].transpose(0, 2, 1)),
                    "Vv": np.ascontiguousarray(Va[:, :, h, :].reshape(B, 130, 128, 65).transpose(0, 2, 1, 3))})
    r = run(k2_build(), ims, "k2")
    Ol = np.stack([r[h]["OT"].transpose(0, 2, 1) for h in range(8)], 2)
    Oc = np.stack([r[h]["OTc"].transpose(0, 2, 1) for h in range(8)], 2)
    return np.ascontiguousarray(Ol), np.ascontiguousarray(Oc)

def layer0_ret(o1):

    RQ = np.concatenate([o1["RQ"][1], o1["RQ"][0]], 1); RK = np.concatenate([o1["RK"][1], o1["RK"][0]], 1); RV = np.concatenate([o1["RV"][1], o1["RV"][0]], 1)
    ims = []
    for i in range(8):
        b, h = i // 4, i % 4
        sl = slice(h * 128, (h + 1) * 128)
        d = {"qT": np.ascontiguousarray(RQ[b][:, sl].T), "kT": np.ascontiguousarray(RK[b][:, sl].T),
             "kt": np.ascontiguousarray(RK[b][:, sl].reshape(130, 128, 128).transpose(1, 0, 2)),
             "vt": np.ascontiguousarray(RV[b][:, sl].reshape(130, 128, 128).transpose(1, 0, 2))}
        d.update(ret_consts(h)); ims.append(d)
    r = run(k3_build(), ims, "k3")
    O = np.zeros((B, 16640, 2, 512), np.float32)
    for i in range(8):
        b, h = i // 4, i % 4
        O[b][:, :, h * 128:(h + 1) * 128] = r[i]["o"].reshape(2, 16640, 128).transpose(1, 0, 2)
    return np.ascontiguousarray(O[:, 256:]), np.ascontiguousarray(O[:, :256])

def mid_common(I, mod, layer, b):
    m = mod[layer]
    return {"g1B": np.stack([bc(m[b][2048:3072]), bc(m[2][2048:3072])]), "gfB": bc(I['norm_ffn_g'][layer]),
            "sc2B": np.stack([bc(m[b][4096:5120]), bc(m[2][4096:5120])]), "sh2B": np.stack([bc(m[b][3072:4096]), bc(m[2][3072:4096])]),
            "w_r": wl(I['moe_router'][layer], 8), "ident": IDB, "identf": IDF}

def layer0_mid(I, mod, x_lat, x_ctx, o1, Oatt, Oret):

    ims = []
    for i in range(8):
        b = i // 4
        d = {"x": pack(i, x_lat, x_ctx), "Oatt": pack(i, Oatt[0], Oatt[1]), "Oret": pack(i, Oret[0], Oret[1]), "RG": pack(i, o1["RG"][0], o1["RG"][1]),
             "gn": bc(I['ret_norm_g'][0]), "w_out": wl(I['ev_w_out'][0], 8)}
        d.update(mid_common(I, mod, 0, b)); ims.append(d)
    r = run(k4_build(T, "even"), ims, "k4")
    return {n: unpack(r, n) for n in ("xmid", "h", "aff")}

def relerr(a, b): return float(np.sqrt(((a.astype(np.float64) - b) ** 2).mean() / (b.astype(np.float64) ** 2).mean()))

def moe_dev(I, layer, h_lat, h_ctx, aff_lat, aff_ctx, with_ctx=True):
    probs = ((128, 2048), (2, 32)) if with_ctx else ((128, 2048),)
    tri = np.triu(np.ones((128, 128), np.float32), 1)
    ims = []
    for i in range(8):
        b, eg = i // 4, i % 4
        es = slice(eg * 4, eg * 4 + 4)
        d = {"aff0": np.ascontiguousarray(aff_lat[b][:, es].reshape(128, 128, 4)), "h0": np.ascontiguousarray(h_lat[b].reshape(128, 128, 1024)),
             "wg": np.ascontiguousarray(I['moe_w_gate'][layer][es].reshape(4, 8, 128, 1536).transpose(0, 2, 1, 3)),
             "wu": np.ascontiguousarray(I['moe_w_up'][layer][es].reshape(4, 8, 128, 1536).transpose(0, 2, 1, 3)),
             "wd": np.ascontiguousarray(I['moe_w_down'][layer][es].reshape(4, 12, 128, 1024).transpose(0, 2, 1, 3)),
             "tri": tri, "ones": np.ones((128, 128), np.float32), "ident": IDB}
        if with_ctx:
            d["aff1"] = np.ascontiguousarray(aff_ctx[b][:, es].reshape(128, 2, 4)); d["h1"] = np.ascontiguousarray(h_ctx[b].reshape(128, 2, 1024))
        ims.append(d)
    r = run(k5_build(probs), ims, "k5")
    out = {}
    for pi, (J, cap) in enumerate(probs):
        n = 128 * J
        out[pi] = {"ys": [np.concatenate([r[b * 4 + eg][f"ys{pi}"] for eg in range(4)], 0) for b in range(B)],
                   "dest": np.stack([np.concatenate([r[b * 4 + eg][f"dest{pi}"].reshape(n, 4) for eg in range(4)], 1) for b in range(B)]),
                   "gsel": np.stack([np.concatenate([r[b * 4 + eg][f"gsel{pi}"].reshape(n, 4) for eg in range(4)], 1) for b in range(B)])}
    return out

def combine_dev(I, mod, layer, xmid_lat, xmid_ctx, mo, final=False):

    with_ctx = 1 in mo
    ims = []
    for i in range(8):
        b = i // 4
        m = mod[layer]
        if with_ctx:
            d = {"xmid": pack(i, xmid_lat, xmid_ctx), "gsel": pack(i, mo[0]["gsel"], mo[1]["gsel"]), "dest": pack(i, mo[0]["dest"], mo[1]["dest"]),
                 }
            for e in range(16):
                d[f"ys0_{e}"] = np.ascontiguousarray(mo[0]["ys"][b][e]); d[f"ys1_{e}"] = np.ascontiguousarray(mo[1]["ys"][b][e])
        else:
            q = i % 4; sl = slice(q * 4096, (q + 1) * 4096)
            d = {"xmid": np.ascontiguousarray(xmid_lat[b][sl]), "gsel": np.ascontiguousarray(mo[0]["gsel"][b][sl]), "dest": np.ascontiguousarray(mo[0]["dest"][b][sl])}
            for e in range(16):
                d[f"ys0_{e}"] = np.ascontiguousarray(mo[0]["ys"][b][e])
        d["g2B"] = np.stack([bc(m[b][5120:6144]), bc(m[2][5120:6144])])
        if final: d["fgB"] = bc(I['final_g'])
        ims.append(d)
    Tn = T if with_ctx else 32
    r = run(k6_build(Tn, (2048, 32) if with_ctx else (2048,), final=final), ims, "k6")
    if with_ctx:
        return unpack(r, "xo")
    xo = np.stack([np.concatenate([r[b * 4 + q]["xo"] for q in range(4)], 0) for b in range(B)])
    fo = np.stack([np.concatenate([r[b * 4 + q]["fo"] for q in range(4)], 0) for b in range(B)]) if final else None
    return xo, fo

def rope_tabs_win(tpos, is_ctx):
    n = len(tpos); C = np.ones((n, 64), np.float32); S = np.zeros((n, 64), np.float32)
    if not is_ctx:
        half = 16
        inv = (10000.0 ** (-np.arange(half, dtype=np.float32) / half)).astype(np.float32)
        for blk, pos in enumerate((tpos // 64, tpos % 64)):
            ang = pos.astype(np.float32)[:, None] * inv[None, :]
            c = np.cos(ang).astype(np.float32); s = np.sin(ang).astype(np.float32)
            C[:, blk*32:blk*32+16] = c; C[:, blk*32+16:blk*32+32] = c
            S[:, blk*32:blk*32+16] = -s; S[:, blk*32+16:blk*32+32] = s
    return C, S

def shifted_streams(xs):
    z = np.zeros_like(xs[:, :1])
    xp = np.concatenate([z, xs[:, :-1]], 1); xn = np.concatenate([xs[:, 1:], z], 1)
    fl = np.ones(xs.shape[:2] + (2,), np.float32); fl[:, 0, 0] = 0; fl[:, -1, 1] = 0
    return xp, xn, fl

def layer1_pre(I, mod, x_lat, x_ctx):

    xpl, xnl, fll = shifted_streams(x_lat); xpc, xnc, flc = shifted_streams(x_ctx)
    m = mod[1]; o = 0
    ims = []
    for i in range(8):
        b, q = i // 4, i % 4
        Cl, Sl = rope_tabs_win(q * 4096 + np.arange(4096), False); Cc, Sc = rope_tabs_win(np.arange(128), True)
        ims.append({"x": pack(i, x_lat, x_ctx), "xp": pack(i, xpl, xpc), "xn": pack(i, xnl, xnc), "fl": pack(i, fll, flc), "gB": bc(I['norm_mix_g'][1]),
            "scB": np.stack([bc(m[b][1024:2048]), bc(m[2][1024:2048])]), "shB": np.stack([bc(m[b][0:1024]), bc(m[2][0:1024])]),
            "w_in": wl(I['od_w_in'][o], 8), "muB": bc(I['rwkv_mu'][o]), "kkB": bc(I['rwkv_k_k'][o]), "kaB": bc(I['rwkv_k_a'][o]), "rkB": bc(I['rwkv_r_k'][o].reshape(-1)),
            "w0B": np.stack([bc(I['rwkv_w0'][o][d]) for d in range(2)]), "a0B": np.stack([bc(I['rwkv_a0'][o][d]) for d in range(2)]),
            "w2s": np.ascontiguousarray(I['rwkv_w2'][o].reshape(128, 1, 512)), "a2s": np.ascontiguousarray(I['rwkv_a2'][o].reshape(128, 1, 512)),
            "g2s": np.ascontiguousarray(I['rwkv_g2'][o].reshape(128, 1, 512)), "Cw": np.concatenate([Cc, Cl]), "Sw": np.concatenate([Sc, Sl]), "ident": IDB})
    r = run(k7_build(T), ims, "k7")
    names = ["R", "Vv", "KK", "LWf", "LWb", "Af", "Ab", "Kf", "Kb", "G", "BG", "NQ", "Qw", "Kw", "Vw"]
    return {n: unpack(r, n) for n in names}

def layer1_rwkv(o7):

    cst = k8_consts()
    ims = []
    def seq(name, b, dirn, ch):
        lat, ctx = o7[name]
        a = np.concatenate([ctx[b][::-1] if dirn else ctx[b], lat[b][::-1] if dirn else lat[b]], 0)[:, ch]
        return np.ascontiguousarray(a.reshape(260, 64, 256))
    for i in range(8):
        b, dirn, hg = i // 4, (i % 4) // 2, i % 2
        ch = slice(hg * 256, (hg + 1) * 256)
        d = {"R": seq("R", b, dirn, ch), "K": seq("Kb" if dirn else "Kf", b, dirn, ch), "Vv": seq("Vv", b, dirn, ch), "KK": seq("KK", b, dirn, ch),
             "A": seq("Ab" if dirn else "Af", b, dirn, ch), "LW": seq("LWb" if dirn else "LWf", b, dirn, ch)}
        d.update(cst); ims.append(d)
    r = run(k8_build(260, 4), ims, "k8")
    Y = np.zeros((B, N, 2, 512), np.float32)
    for i in range(8):
        b, dirn, hg = i // 4, (i % 4) // 2, i % 2
        y = r[i]["y"].reshape(N, 256)
        Y[b][:, dirn, hg * 256:(hg + 1) * 256] = y[::-1] if dirn else y
    return Y

def layer1_win(o7):

    Ql = o7["Qw"][0].reshape(B, N, 2, 4, 66)
    Kl = o7["Kw"][0].reshape(B, N, 2, 66); Vl = o7["Vw"][0].reshape(B, N, 2, 65)
    Kc = o7["Kw"][1].reshape(B, NCTX, 2, 66); Vc = o7["Vw"][1].reshape(B, NCTX, 2, 65)
    padK = np.zeros((128, 2, 66), bf); padK[:, :, 64] = 1.0; padK[:, :, 65] = -10000.0
    padV = np.zeros((128, 2, 65), bf)
    cst = k9_consts(); ims = []
    for i in range(8):
        b, q = i // 4, i % 4
        t0 = q * 4096
        lo = Kl[b][t0 - 128:t0] if q > 0 else padK; hi = Kl[b][t0 + 4096:t0 + 4224] if q < 3 else padK
        Kh = np.concatenate([lo, Kl[b][t0:t0 + 4096], hi], 0)
        lo = Vl[b][t0 - 128:t0] if q > 0 else padV; hi = Vl[b][t0 + 4096:t0 + 4224] if q < 3 else padV
        Vh = np.concatenate([lo, Vl[b][t0:t0 + 4096], hi], 0)
        Qb = Ql[b][t0:t0 + 4096].reshape(32, 128, 2, 4, 66)
        d = {"QT": np.ascontiguousarray(Qb.transpose(2, 4, 0, 3, 1).reshape(2, 66, 32 * 512)),
             "KT": np.ascontiguousarray(Kh.transpose(1, 2, 0)), "Vv": np.ascontiguousarray(Vh.reshape(34, 128, 2, 65).transpose(2, 1, 0, 3)),
             "KTc": np.ascontiguousarray(Kc[b].transpose(1, 2, 0)), "Vc": np.ascontiguousarray(Vc[b].reshape(2, 128, 2, 65).transpose(2, 1, 0, 3))}
        d.update(cst); ims.append(d)
    r = run(k9_build(32), ims, "k9")
    O = np.zeros((B, N, 8, 65), np.float32)
    for i in range(8):
        b, q = i // 4, i % 4
        ot = r[i]["OT"].reshape(2, 65, 32, 4, 128)
        O[b][q * 4096:(q + 1) * 4096] = ot.transpose(2, 4, 0, 3, 1).reshape(4096, 8, 65)
    return O

def layer1_mid(I, mod, x_lat, o7, Y, Ow):

    ims = []
    for i in range(8):
        b, q = i // 4, i % 4; sl = slice(q * 4096, (q + 1) * 4096)
        c = lambda a: np.ascontiguousarray(a[b][sl])
        d = {"x": c(x_lat), "Yr": c(Y), "G": c(o7["G"][0]), "BG": c(o7["BG"][0]), "Ow": c(Ow), "NQ": c(o7["NQ"][0]),
             "sinkB": bc(I['win_sink'][0]), "lngB": bc(I['rwkv_ln_g'][0]), "lnbB": bc(I['rwkv_ln_b'][0]), "w_out": wl(I['od_w_out'][0], 8)}
        d.update(mid_common(I, mod, 1, b)); ims.append(d)
    r = run(k4_build(32, "odd", has_ctx=False), ims, "k10")
    cat = lambda n: np.stack([np.concatenate([r[b * 4 + q][n] for q in range(4)], 0) for b in range(B)])
    return {n: cat(n) for n in ("xmid", "h", "aff")}


def kernel(**inputs):
    I = {k: np.asarray(v) for k, v in inputs.items()}
    mod = modulation_dev(I)
    x_lat, x_ctx = I['x'], I['ctx']
    o1 = layer0_pre(I, mod, x_lat, x_ctx)
    Oret = layer0_ret(o1)
    Oatt = layer0_mla(o1)
    o4 = layer0_mid(I, mod, x_lat, x_ctx, o1, Oatt, Oret)
    del o1, Oret, Oatt
    mo = moe_dev(I, 0, o4["h"][0], o4["h"][1], o4["aff"][0], o4["aff"][1], True)
    x_lat, x_ctx = combine_dev(I, mod, 0, o4["xmid"][0], o4["xmid"][1], mo)
    del o4, mo
    o7 = layer1_pre(I, mod, x_lat, x_ctx)
    Y = layer1_rwkv(o7)
    Ow = layer1_win(o7)
    o10 = layer1_mid(I, mod, x_lat, o7, Y, Ow)
    del o7, Y, Ow
    mo = moe_dev(I, 1, o10["h"], None, o10["aff"], None, False)
    xo2, fo = combine_dev(I, mod, 1, o10["xmid"], None, mo, final=True)
    return np.ascontiguousarray(fo.astype(np.float32))
```

```python
import numpy as np

from contextlib import ExitStack
import concourse.bass as bass
import concourse.mybir as mybir
from concourse.bass_utils import run_bass_kernel_spmd

F32 = mybir.dt.float32
BF16 = mybir.dt.bfloat16
I32 = mybir.dt.int32
U32 = mybir.dt.uint32
AF = mybir.ActivationFunctionType
ALU = mybir.AluOpType
AX = mybir.AxisListType


class V:
    __slots__ = ("key", "ap")

    def __init__(self, key, ap):
        self.key = key
        self.ap = ap

    def __getitem__(self, idx):
        return V(self.key, self.ap[idx])

    def sub(self, subkey, idx=None):
        return V((self.key, subkey), self.ap if idx is None else self.ap[idx])

    def re(self, pattern, **kw):
        return V(self.key, self.ap.rearrange(pattern, **kw))


class _Op:
    __slots__ = ("eng", "fn", "kind", "deps", "sig", "dsem", "dval", "idx", "selfwait")


class Prog:
    ENGS = ("pe", "act", "dve", "pool", "sp")
    NDMA = 8

    def __init__(self, name="k"):
        self.nc = bass.Bass("TRN2", target_bir_lowering=False)
        self.es = ExitStack()
        self.ops = {e: [] for e in self.ENGS}
        self.state = {}
        self.ndma = {e: 0 for e in self.ENGS}
        self.uid = 0
        self.outs = []

    def dram(self, name, shape, dt, kind):
        t = self.nc.dram_tensor(name, list(shape), dt, kind=kind)
        return V(("d", name), t.ap())

    def inp(self, name, shape, dt=F32):
        return self.dram(name, shape, dt, "ExternalInput")

    def out(self, name, shape, dt=F32):
        v = self.dram(name, shape, dt, "ExternalOutput")
        self.outs.append(name)
        return v

    def scratch(self, name, shape, dt=F32):
        return self.dram(name, shape, dt, "Internal")

    def sb(self, name, shape, dt=F32):
        t = self.es.enter_context(self.nc.sbuf_tensor(name, list(shape), dt))
        return V(("s", name), t.ap() if hasattr(t, "ap") and callable(t.ap) else t[:])

    def ps(self, name, shape, dt=F32):
        t = self.es.enter_context(self.nc.psum_tensor(name, list(shape), dt))
        return V(("p", name), t.ap() if hasattr(t, "ap") and callable(t.ap) else t[:])

    def op(self, eng, fn, reads=(), writes=(), kind="c"):
        o = _Op()
        o.eng, o.fn, o.kind = eng, fn, kind
        o.sig = None
        o.idx = len(self.ops[eng])
        deps = set()
        for v in reads:
            st = self.state.get(v.key)
            if st is not None and st[0] is not None:
                deps.add(st[0])
        for v in writes:
            st = self.state.get(v.key)
            if st is not None:
                if st[0] is not None:
                    deps.add(st[0])
                for r in st[1].values():
                    deps.add(r)
        o.deps = [d for d in deps if not (d.eng == eng and d.kind == "c" and kind == "c" and eng == "pe")]
        if kind == "d":
            j = self.ndma[eng]
            self.ndma[eng] += 1
            o.dsem = j % self.NDMA
            o.dval = 16 * (j // self.NDMA + 1)
        self.ops[eng].append(o)
        for v in reads:
            st = self.state.setdefault(v.key, [None, {}])
            if kind == "d":
                st[1][(eng, kind, o.idx)] = o
            else:
                st[1][(eng, kind)] = o
        for v in writes:
            self.state[v.key] = [o, {}]
        return o

    def dma(self, out, in_, eng="sp", **kw):
        return self.op(eng, lambda e: e.dma_start(out=out.ap, in_=in_.ap, **kw), [in_], [out], kind="d")

    def mm(self, out, lhsT, rhs, start=True, stop=True, extra_reads=()):
        return self.op("pe", lambda e: e.matmul(out.ap, lhsT.ap, rhs.ap, start=start, stop=stop),
                       [lhsT, rhs] + ([] if start else [out]) + list(extra_reads), [out])

    def tr(self, out, in_, ident):
        return self.op("pe", lambda e: e.transpose(out.ap, in_.ap, ident.ap), [in_, ident], [out])

    def act(self, out, in_, func, bias=None, scale=None, accum=None, eng="act"):
        kw = {}
        rd = [in_]
        wr = [out]
        if bias is not None:
            if isinstance(bias, V):
                kw["bias"] = bias.ap
                rd.append(bias)
            else:
                kw["bias"] = bias
        if scale is not None:
            if isinstance(scale, V):
                kw["scale"] = scale.ap
                rd.append(scale)
            else:
                kw["scale"] = scale
        if accum is not None:
            kw["accum_out"] = accum.ap
            wr.append(accum)
        return self.op(eng, lambda e: e.activation(out.ap, in_.ap, func, **kw), rd, wr)

    def tt(self, out, a, b, op, eng="dve"):
        return self.op(eng, lambda e: e.tensor_tensor(out=out.ap, in0=a.ap, in1=b.ap, op=op), [a, b], [out])

    def ts(self, out, a, s1, op0, s2=None, op1=None, accum=None, eng="dve"):
        rd = [a]
        wr = [out]
        kw = {}
        a1 = s1
        a2 = s2
        if isinstance(s1, V):
            rd.append(s1)
            a1 = s1.ap
        if isinstance(s2, V):
            rd.append(s2)
            a2 = s2.ap
        if op1 is not None:
            kw["op1"] = op1
        if accum is not None:
            kw["accum_out"] = accum.ap
            wr.append(accum)
        return self.op(eng, lambda e: e.tensor_scalar(out=out.ap, in0=a.ap, scalar1=a1, scalar2=a2, op0=op0, **kw), rd, wr)

    def stt(self, out, a, s, b, op0, op1, accum=None, eng="dve"):
        rd = [a, b]
        wr = [out]
        sa = s
        kw = {}
        if isinstance(s, V):
            rd.append(s)
            sa = s.ap
        if accum is not None:
            kw["accum_out"] = accum.ap
            wr.append(accum)
        return self.op(eng, lambda e: e.scalar_tensor_tensor(out=out.ap, in0=a.ap, scalar=sa, in1=b.ap, op0=op0, op1=op1, **kw), rd, wr)

    def copy(self, out, in_, eng="dve"):
        if eng == "act":
            return self.op("act", lambda e: e.copy(out.ap, in_.ap), [in_], [out])
        return self.op(eng, lambda e: e.tensor_copy(out=out.ap, in_=in_.ap), [in_], [out])

    def memset(self, out, val, eng="dve"):
        return self.op(eng, lambda e: e.memset(out.ap, val), [], [out])

    def recip(self, out, in_, eng="dve"):
        return self.op(eng, lambda e: e.reciprocal(out=out.ap, in_=in_.ap), [in_], [out])

    def reduce(self, out, in_, op, axis=AX.X, eng="dve"):
        return self.op(eng, lambda e: e.tensor_reduce(out=out.ap, in_=in_.ap, axis=axis, op=op), [in_], [out])

    def scan(self, out, d0, d1, init, op0, op1):
        rd = [d0, d1]
        ia = init
        if isinstance(init, V):
            rd.append(init)
            ia = init.ap
        return self.op("dve", lambda e: e.tensor_tensor_scan(out=out.ap, data0=d0.ap, data1=d1.ap, initial=ia, op0=op0, op1=op1), rd, [out])

    def bcreg(self, g, val):
        d = self.__dict__.setdefault("_bcregs", {})
        if val not in d:
            d[val] = g.alloc_register(f"bc{val}")
            g.reg_mov(d[val], val)
        return d[val]

    def build(self):
        nc = self.nc
        cnt = {e: 0 for e in self.ENGS}
        for e in self.ENGS:
            for o in self.ops[e]:
                for d in o.deps:
                    if d.kind == "c":
                        d.sig = True
        for e in self.ENGS:
            for o in self.ops[e]:
                if o.kind == "c" and o.sig:
                    cnt[e] += 1
                    o.sig = cnt[e]
        sems = {}
        for e in self.ENGS:
            sems[("c", e)] = self.es.enter_context(nc.semaphore(f"c_{e}"))
            if self.ndma[e]:
                for i in range(min(self.NDMA, self.ndma[e])):
                    sems[("d", e, i)] = self.es.enter_context(nc.semaphore(f"d_{e}_{i}"))
        final = {e: {} for e in self.ENGS}

        def emit(eng_name, eng):
            waited = {}
            for o in self.ops[eng_name]:
                ws = {}
                for d in o.deps:
                    if d.kind == "c":
                        k, v = ("c", d.eng), d.sig
                    else:
                        k, v = ("d", d.eng, d.dsem), d.dval
                    if ws.get(k, 0) < v:
                        ws[k] = v
                if o.kind == "d" and o.dval > 16:
                    k = ("d", eng_name, o.dsem)
                    if ws.get(k, 0) < o.dval - 16:
                        ws[k] = o.dval - 16
                for k, v in ws.items():
                    if waited.get(k, 0) < v:
                        eng.wait_ge(sems[k], v)
                        waited[k] = v
                inst = o.fn(eng)
                if o.kind == "d":
                    inst.then_inc(sems[("d", eng_name, o.dsem)], 16)
                    final[eng_name][("d", eng_name, o.dsem)] = o.dval
                elif o.sig:
                    inst.then_inc(sems[("c", eng_name)], 1)
            for k, v in final[eng_name].items():
                if waited.get(k, 0) < v:
                    eng.wait_ge(sems[k], v)

        with nc.Block() as block:
            if self.ops["sp"]:
                @block.sync
                def _(e):
                    emit("sp", e)
            if self.ops["pe"]:
                @block.tensor
                def _(e):
                    emit("pe", e)
            if self.ops["act"]:
                @block.scalar
                def _(e):
                    emit("act", e)
            if self.ops["dve"]:
                @block.vector
                def _(e):
                    emit("dve", e)
            if self.ops["pool"]:
                @block.gpsimd
                def _(e):
                    emit("pool", e)
        self.es.close()
        return nc

    def run(self, in_maps, n=8, trace=False):
        nc = self.build()
        res = run_bass_kernel_spmd(nc, in_maps, core_ids=list(range(n)), trace=trace)
        return res


def build_mod():
    P = Prog()
    cT = P.inp("cT", [128, 8, 3]); w = P.inp("w", [128, 8, 1536]); b = P.inp("b", [3, 1536]); o = P.out("o", [3, 1536])
    cs = P.sb("cs", [128, 8, 3]); ws = P.sb("ws", [128, 8, 1536]); bs = P.sb("bs", [3, 1536]); os_ = P.sb("os", [3, 1536]); sg = P.sb("sg", [128, 8, 3])
    P.dma(cs, cT); P.dma(ws, w); P.dma(bs, b)
    P.act(sg, cs, AF.Sigmoid)
    P.tt(cs, cs, sg, ALU.mult)
    pp = [P.ps(f"pp{i}", [128, 512]) for i in range(3)]
    for j in range(3):
        for k in range(8):
            P.mm(pp[j][0:3, :], cs[:, k, :], ws[:, k, j*512:(j+1)*512], start=(k==0), stop=(k==7))
        P.tt(os_[:, j*512:(j+1)*512], pp[j][0:3, :], bs[:, j*512:(j+1)*512], ALU.add)
    P.dma(o, os_)
    return P


EPS = 1e-6
SQ = 96 ** -0.25
IN_CH = [(0, 512), (512, 1024), (1024, 1536), (1536, 2048), (2048, 2464)]


def load_w_bf16(P, dst, src, kchunks, ncols, stage):
    for k in range(kchunks):
        P.dma(stage[:, :ncols], src[:, k, :])
        P.copy(dst[:, k, :], stage[:, :ncols], eng=("act" if k % 2 else "dve"))


def rms_rstd(P, rs, junk, xin, n, eps=EPS):
    P.tt(junk, xin, xin, ALU.mult)
    P.reduce(rs, junk, ALU.add)
    P.ts(rs, rs, 1.0 / n, ALU.mult, eps, ALU.add)
    P.act(rs, rs, AF.Sqrt)
    P.recip(rs, rs)


def k1_build(T):
    P = Prog()
    x = P.inp("x", [T * 128, 1024])
    gB = P.inp("gB", [128, 1024])
    scB = P.inp("scB", [2, 128, 1024])
    shB = P.inp("shB", [2, 128, 1024])
    w_in = P.inp("w_in", [128, 8, 2464])
    w_uq = P.inp("w_uq", [128, 2, 768])
    w_ukv = P.inp("w_ukv", [128, 1, 1024])
    gq = P.inp("gq", [128, 256])
    gkv = P.inp("gkv", [128, 128])
    Cq = P.inp("Cq", [T * 128, 32])
    Sq_ = P.inp("Sq", [T * 128, 32])
    Cr = P.inp("Cr", [T * 128, 64])
    Sr = P.inp("Sr", [T * 128, 64])
    Crk = P.inp("Crk", [T * 128, 64])
    Srk = P.inp("Srk", [T * 128, 64])
    identd = P.inp("ident", [128, 128], BF16)
    Qa_o = P.out("Qa", [T * 128, 8 * 98], BF16)
    Ka_o = P.out("Ka", [T * 128, 8 * 98], BF16)
    Va_o = P.out("Va", [T * 128, 8 * 65], BF16)
    RQ_o = P.out("RQ", [T * 128, 512], BF16)
    RK_o = P.out("RK", [T * 128, 512], BF16)
    RV_o = P.out("RV", [T * 128, 512], BF16)
    RG_o = P.out("RG", [T * 128, 512], F32)

    ident = P.sb("ident_s", [128, 128], BF16)
    P.dma(ident, identd)
    stage = P.sb("stage", [128, 2464])
    w_in_b = P.sb("w_in_b", [128, 8, 2464], BF16)
    w_uq_b = P.sb("w_uq_b", [128, 2, 768], BF16)
    w_ukv_b = P.sb("w_ukv_b", [128, 1, 1024], BF16)
    load_w_bf16(P, w_in_b, w_in, 8, 2464, stage)
    load_w_bf16(P, w_uq_b, w_uq, 2, 768, stage)
    load_w_bf16(P, w_ukv_b, w_ukv, 1, 1024, stage)
    gqs = P.sb("gqs", [128, 256])
    gkvs = P.sb("gkvs", [128, 128])
    P.dma(gqs, gq)
    P.dma(gkvs, gkv)
    gs = P.sb("gs", [128, 1024])
    P.dma(gs, gB)
    Gp = P.sb("Gp", [128, 2, 1024])
    SH = P.sb("SHt", [128, 2, 1024])
    for c in range(2):
        P.dma(stage[:, :1024], scB[c])
        P.stt(Gp[:, c, :], stage[:, :1024], 1.0, gs, ALU.add, ALU.mult)
        P.dma(SH[:, c, :], shB[c])

    xs = [P.sb(f"xs{i}", [128, 1024]) for i in range(2)]
    junk = P.sb("junk", [128, 1024])
    tmp = P.sb("tmp", [128, 1024])
    a_bf = P.sb("a_bf", [128, 1024], BF16)
    aT = P.sb("aT", [128, 1024], BF16)
    Pm = P.sb("Pm", [128, 2464])
    rs = P.sb("rs", [128, 1])
    rs2 = P.sb("rs2", [128, 1])
    rs3 = P.sb("rs3", [128, 1])
    cqn = P.sb("cqn", [128, 256], BF16)
    cqnT = P.sb("cqnT", [128, 256], BF16)
    ckvn = P.sb("ckvn", [128, 128], BF16)
    ckvnT = P.sb("ckvnT", [128, 128], BF16)
    qf = P.sb("qf", [128, 768])
    kvf = P.sb("kvf", [128, 1024])
    t1 = P.sb("t1", [128, 512])
    t2 = P.sb("t2", [128, 512])
    krr = P.sb("krr", [128, 32])
    nq = P.sb("nq", [128, 8])
    nk = P.sb("nk", [128, 8])
    ek = P.sb("ek", [128, 8])
    krs = P.sb("krs", [128, 1])
    tabs = [[P.sb(f"tab{i}_{j}", [128, 64]) for j in range(6)] for i in range(2)]
    Qa = [P.sb(f"Qa{i}", [128, 8, 98], BF16) for i in range(2)]
    Ka = [P.sb(f"Ka{i}", [128, 8, 98], BF16) for i in range(2)]
    Va = [P.sb(f"Va{i}", [128, 8, 65], BF16) for i in range(2)]
    RQ = [P.sb(f"RQ{i}", [128, 512], BF16) for i in range(2)]
    RK = [P.sb(f"RK{i}", [128, 512], BF16) for i in range(2)]
    RV = [P.sb(f"RV{i}", [128, 512], BF16) for i in range(2)]
    RG = [P.sb(f"RG{i}", [128, 512], F32) for i in range(2)]
    pT = P.ps("pT", [128, 1024], BF16)
    pp = [P.ps(f"pp{i}", [128, 512]) for i in range(6)]

    for t in range(T):
        cls = 1 if t == 0 else 0
        i2 = t % 2
        rows = slice(t * 128, (t + 1) * 128)
        xt = xs[i2]
        P.dma(xt, x[rows, :])
        tb = tabs[i2]
        P.dma(tb[0][:, :32], Cq[rows, :])
        P.dma(tb[1][:, :32], Sq_[rows, :])
        P.dma(tb[2], Cr[rows, :])
        P.dma(tb[3], Sr[rows, :])
        P.dma(tb[4], Crk[rows, :])
        P.dma(tb[5], Srk[rows, :])
        rms_rstd(P, rs, junk, xt, 1024)
        P.stt(tmp, xt, rs, Gp[:, cls, :], ALU.mult, ALU.mult)
        P.tt(a_bf, tmp, SH[:, cls, :], ALU.add)
        for k in range(8):
            P.tr(pT[:, k * 128:(k + 1) * 128], a_bf[:, k * 128:(k + 1) * 128], ident)
        P.copy(aT, pT)
        for j, (c0, c1) in enumerate(IN_CH):
            for k in range(8):
                P.mm(pp[j][:, :c1 - c0], aT[:, k * 128:(k + 1) * 128], w_in_b[:, k, c0:c1], start=(k == 0), stop=(k == 7))
            P.copy(Pm[:, c0:c1], pp[j][:, :c1 - c0], eng=("act" if j % 2 else "dve"))
        rms_rstd(P, rs2, junk[:, :256], Pm[:, 0:256], 256)
        P.stt(cqn, Pm[:, 0:256], rs2, gqs, ALU.mult, ALU.mult)
        for k in range(2):
            P.tr(pT[:, k * 128:(k + 1) * 128], cqn[:, k * 128:(k + 1) * 128], ident)
        P.copy(cqnT, pT[:, :256])
        for j, (c0, c1) in enumerate([(0, 512), (512, 768)]):
            for k in range(2):
                P.mm(pp[j][:, :c1 - c0], cqnT[:, k * 128:(k + 1) * 128], w_uq_b[:, k, c0:c1], start=(k == 0), stop=(k == 1))
            P.act(qf[:, c0:c1], pp[j][:, :c1 - c0], AF.Copy, scale=SQ)
        qv = qf.re("p (h d) -> p h d", d=96)
        Qt = Qa[i2]
        P.tt(junk[:, :768], qf, qf, ALU.mult)
        P.reduce(nq, junk[:, :768].re("p (h d) -> p h d", d=96), ALU.add)
        P.ts(Qt[:, :, 96], nq, -0.5, ALU.mult)
        P.memset(Qt[:, :, 97], 1.0)
        P.copy(Qt[:, :, 0:64], qv[:, :, 0:64], eng="act")
        Cb = V(tb[0].key, tb[0].ap[:, :32].unsqueeze(1).to_broadcast([128, 8, 32]))
        t1v = t1[:, :256].re("p (h d) -> p h d", d=32)
        t2v = t2[:, :256].re("p (h d) -> p h d", d=32)
        P.tt(t1v, qv[:, :, 64:96], Cb, ALU.mult)
        q5 = qv[:, :, 64:96].re("p h (b f e) -> p h b f e", b=2, f=2)
        t25 = t2v.re("p h (b f e) -> p h b f e", b=2, f=2)
        S5 = tb[1].ap[:, :32].rearrange("p (b f e) -> p b f e", b=2, f=2)
        for f in range(2):
            Sb = V(tb[1].key, S5[:, :, f, :].unsqueeze(1).to_broadcast([128, 8, 2, 8]))
            P.tt(t25[:, :, :, f, :], q5[:, :, :, 1 - f, :], Sb, ALU.mult)
        P.tt(Qt[:, :, 64:96], t1v, t2v, ALU.add)
        P.dma(Qa_o[rows, :], Qt.re("p h d -> p (h d)"))
        rms_rstd(P, rs3, junk[:, :128], Pm[:, 256:384], 128)
        P.stt(ckvn, Pm[:, 256:384], rs3, gkvs, ALU.mult, ALU.mult)
        P.tr(pT[:, 0:128], ckvn, ident)
        P.copy(ckvnT, pT[:, :128])
        for j in range(2):
            P.mm(pp[2 + j], ckvnT, w_ukv_b[:, 0, j * 512:(j + 1) * 512], start=True, stop=True)
            P.copy(kvf[:, j * 512:(j + 1) * 512], pp[2 + j], eng=("act" if j % 2 else "dve"))
        kvv = kvf.re("p (h d) -> p h d", d=128)
        Kt = Ka[i2]
        Vt = Va[i2]
        kr = Pm[:, 384:416]
        P.tt(t1[:, :32], kr, tb[0][:, :32], ALU.mult)
        kr4 = kr.re("p (b f e) -> p b f e", b=2, f=2)
        t24 = t2[:, :32].re("p (b f e) -> p b f e", b=2, f=2)
        S4 = V(tb[1].key, S5)
        for f in range(2):
            P.tt(t24[:, :, f, :], kr4[:, :, 1 - f, :], S4[:, :, f, :], ALU.mult)
        P.tt(t1[:, :32], t1[:, :32], t2[:, :32], ALU.add)
        P.ts(krr, t1[:, :32], SQ, ALU.mult)
        P.act(Kt[:, :, 0:64], kvv[:, :, 0:64], AF.Copy, scale=SQ)
        P.copy(Kt[:, :, 64:96], V(krr.key, krr.ap.unsqueeze(1).to_broadcast([128, 8, 32])))
        P.memset(Kt[:, :, 96], 1.0)
        kn2 = junk[:, :512].re("p (h d) -> p h d", d=64)
        P.tt(kn2, kvv[:, :, 0:64], kvv[:, :, 0:64], ALU.mult)
        P.reduce(nk, kn2, ALU.add)
        P.tt(junk[:, 512:544], krr, krr, ALU.mult)
        P.reduce(krs, junk[:, 512:544], ALU.add)
        P.ts(nk, nk, SQ * SQ, ALU.mult, krs, ALU.add)
        P.ts(Kt[:, :, 97], nk, -0.5, ALU.mult)
        P.act(ek, Kt[:, :, 97], AF.Exp, scale=-1.0)
        P.tt(Vt[:, :, 0:64], kvv[:, :, 64:128], V(ek.key, ek.ap.unsqueeze(2).to_broadcast([128, 8, 64])), ALU.mult)
        P.copy(Vt[:, :, 64], ek)
        P.dma(Ka_o[rows, :], Kt.re("p h d -> p (h d)"))
        P.dma(Va_o[rows, :], Vt.re("p h d -> p (h d)"))
        for (c0, ci, si, dst, dst_o) in ((416, 2, 3, RQ[i2], RQ_o), (928, 4, 5, RK[i2], RK_o)):
            xv = Pm[:, c0:c0 + 512].re("p (h f e) -> p h f e", h=4, f=2)
            t1r = t1.re("p (h f e) -> p h f e", h=4, f=2)
            t2r = t2.re("p (h f e) -> p h f e", h=4, f=2)
            Cb2 = V(tb[ci].key, tb[ci].ap.unsqueeze(1).unsqueeze(1).to_broadcast([128, 4, 2, 64]))
            P.tt(t1r, xv, Cb2, ALU.mult)
            Sb2 = V(tb[si].key, tb[si].ap.unsqueeze(1).to_broadcast([128, 4, 64]))
            P.tt(t2r[:, :, 1, :], xv[:, :, 0, :], Sb2, ALU.mult)
            P.stt(t2r[:, :, 0, :], xv[:, :, 1, :], -1.0, Sb2, ALU.mult, ALU.mult)
            P.tt(dst, t1, t2, ALU.add)
            P.dma(dst_o[rows, :], dst)
        P.copy(RV[i2], Pm[:, 1440:1952], eng="act")
        P.dma(RV_o[rows, :], RV[i2])
        P.copy(RG[i2], Pm[:, 1952:2464], eng="act")
        P.dma(RG_o[rows, :], RG[i2])
    return P


def k2_build(NQ=16384, NK=16640, NC=256, nb=2):
    P = Prog()
    QT = P.inp("QT", [nb, 98, NQ], BF16)
    QTc = P.inp("QTc", [nb, 98, NC], BF16)
    KT = P.inp("KT", [nb, 98, NK], BF16)
    Vv = P.inp("Vv", [nb, 128, NK // 128, 65], BF16)
    OT = P.out("OT", [nb, 65, NQ])
    OTc = P.out("OTc", [nb, 65, NC])
    nkt = NK // 128
    kT = P.sb("kT", [98, NK], BF16)
    Vs = P.sb("Vs", [128, nkt, 65], BF16)
    qs = [P.sb(f"qs{i}", [98, 512], BF16) for i in range(2)]
    pTs = [P.sb(f"pT{i}", [128, 512], BF16) for i in range(4)]
    osb = [P.sb(f"osb{i}", [65, 512]) for i in range(2)]
    psS = [P.ps(f"psS{i}", [128, 512]) for i in range(4)]
    psO = [P.ps(f"psO{i}", [128, 512]) for i in range(2)]
    LA = 2
    for b in range(nb):
        P.dma(kT, KT[b])
        P.dma(Vs, Vv[b])
        jobs = [(QTc[b], OTc[b], NC, NC // 128)] + [(QT[b][:, q0:q0 + 512], OT[b][:, q0:q0 + 512], 512, nkt) for q0 in range(0, NQ, 512)]
        items = []
        for j, (qsrc, odst, w, nk) in enumerate(jobs):
            for kt in range(nk):
                items.append((j, kt, nk, qsrc, odst, w))
        for idx in range(len(items) + LA):
            if idx < len(items):
                j, kt, nk, qsrc, odst, w = items[idx]
                q = qs[j % 2]
                if kt == 0:
                    P.dma(q[:, :w], qsrc)
                s_ = psS[idx % 4]; pt = pTs[idx % 4]
                P.mm(s_[:, :w], kT[:, kt * 128:(kt + 1) * 128], q[:, :w])
                P.act(pt[:, :w], s_[:, :w], AF.Exp)
            if idx - LA >= 0:
                j, kt, nk, qsrc, odst, w = items[idx - LA]
                po = psO[j % 2]; pt = pTs[(idx - LA) % 4]
                P.mm(po[0:65, :w], Vs[:, kt, :], pt[:, :w], start=(kt == 0), stop=(kt == nk - 1))
                if kt == nk - 1:
                    o = osb[j % 2]
                    P.copy(o[:, :w], po[0:65, :w])
                    P.dma(odst, o[:, :w])
    return P


def ret_consts(h):
    L = 128
    pos = np.arange(L, dtype=np.float64)
    out = {}
    DinT = np.zeros((2, L, L), np.float32); qdec = np.zeros((2, 128, L), np.float32); kdec = np.zeros((128, 2), np.float32); cdec = np.zeros((128, 2), np.float32)
    for d, exp0 in enumerate((5.0, 5.5)):
        lg = np.log1p(-2.0 ** (-exp0 - h))
        j = pos[:, None]; i = pos[None, :]
        if d == 0:
            DinT[d] = np.where(i >= j, np.exp(lg * np.maximum(i - j, 0)), 0.0)
            qdec[d] = np.exp(lg * (pos + 1.0))[None, :]
            kdec[:, d] = np.exp(lg * (L - 1.0 - pos))
        else:
            DinT[d] = np.where(j >= i, np.exp(lg * np.maximum(j - i, 0)), 0.0)
            qdec[d] = np.exp(lg * (L - pos))[None, :]
            kdec[:, d] = np.exp(lg * pos)
        cdec[:, d] = np.exp(lg * L)
    return {"DinT": DinT, "qdec": qdec, "kdec": kdec, "cdec": cdec}

def k3_build(NCH=130, NCTX=2):
    P = Prog()
    N = NCH * 128
    qT = P.inp("qT", [128, N], BF16)
    kT = P.inp("kT", [128, N], BF16)
    kt = P.inp("kt", [128, NCH, 128], BF16)
    vt = P.inp("vt", [128, NCH, 128], BF16)
    DinT = P.inp("DinT", [2, 128, 128]); qdec = P.inp("qdec", [2, 128, 128]); kdec = P.inp("kdec", [128, 2]); cdec = P.inp("cdec", [128, 2])
    o = P.out("o", [2, NCH, 128, 128])
    qTs = P.sb("qTs", [128, N], BF16); kTs = P.sb("kTs", [128, N], BF16)
    kts = P.sb("kts", [128, NCH, 128], BF16); vts = P.sb("vts", [128, NCH, 128], BF16)
    P.dma(qTs, qT); P.dma(kTs, kT); P.dma(kts, kt); P.dma(vts, vt)
    Dm = P.sb("Dm", [128, 2, 128]); qd_ = P.sb("qd_", [128, 2, 128]); kd_ = P.sb("kd_", [128, 2]); cd_ = P.sb("cd_", [128, 2])
    for d in range(2):
        P.dma(Dm[:, d, :], DinT[d]); P.dma(qd_[:, d, :], qdec[d])
    P.dma(kd_, kdec); P.dma(cd_, cdec)
    S = P.sb("S", [128, 128]); Sb = P.sb("Sb", [128, 128], BF16)
    sTm = [P.sb(f"sTm{i}", [128, 128], BF16) for i in range(2)]
    qdv = [P.sb(f"qdv{i}", [128, 128], BF16) for i in range(2)]
    kdv = [P.sb(f"kdv{i}", [128, 128], BF16) for i in range(2)]
    ob = [P.sb(f"ob{i}", [128, 128]) for i in range(2)]
    psA = [P.ps(f"psA{i}", [128, 128]) for i in range(2)]
    psB = [P.ps(f"psB{i}", [128, 128]) for i in range(2)]
    psC = [P.ps(f"psC{i}", [128, 128]) for i in range(2)]
    n = 0
    for d in range(2):
        order = list(range(NCTX)) + list(range(NCTX, NCH))
        if d == 1:
            order = list(range(NCTX))[::-1] + list(range(NCTX, NCH))[::-1]
        P.memset(S, 0.0); P.memset(Sb, 0.0)
        for c in order:
            i2 = n % 2; n += 1
            cols = slice(c * 128, (c + 1) * 128)
            P.mm(psA[i2], kTs[:, cols], qTs[:, cols])
            P.tt(sTm[i2], psA[i2], Dm[:, d, :], ALU.mult)
            P.tt(qdv[i2], qTs[:, cols], qd_[:, d, :], ALU.mult, eng="pool")
            P.mm(psB[i2], sTm[i2], vts[:, c, :], start=True, stop=False)
            P.mm(psB[i2], qdv[i2], Sb, start=False, stop=True)
            P.copy(ob[i2], psB[i2], eng="act")
            P.dma(o[d, c], ob[i2])
            P.ts(kdv[i2], kts[:, c, :], kd_[:, d:d + 1], ALU.mult, eng="pool")
            P.mm(psC[i2], kdv[i2], vts[:, c, :])
            P.stt(S, S, cd_[:, d:d + 1], psC[i2], ALU.mult, ALU.add)
            P.copy(Sb, S)
    return P


def k4_build(T, layer_kind="even", has_ctx=True):
    P = Prog()
    x = P.inp("x", [T * 128, 1024])
    if layer_kind == "even":
        Oatt = P.inp("Oatt", [T * 128, 8, 65])
        Oret = P.inp("Oret", [T * 128, 2, 512])
        RG = P.inp("RG", [T * 128, 512])
        gn = P.inp("gn", [128, 512])
    else:
        Yr = P.inp("Yr", [T * 128, 2, 512]); Gi = P.inp("G", [T * 128, 512]); BGi = P.inp("BG", [T * 128, 512])
        Ow = P.inp("Ow", [T * 128, 8, 65]); NQi = P.inp("NQ", [T * 128, 8])
        sinkB = P.inp("sinkB", [128, 8]); lngB = P.inp("lngB", [128, 512]); lnbB = P.inp("lnbB", [128, 512])
    w_out = P.inp("w_out", [128, 8, 1024])
    g1B = P.inp("g1B", [2, 128, 1024])
    gfB = P.inp("gfB", [128, 1024]); sc2B = P.inp("sc2B", [2, 128, 1024]); sh2B = P.inp("sh2B", [2, 128, 1024])
    w_r = P.inp("w_r", [128, 8, 16])
    identd = P.inp("ident", [128, 128], BF16); identfd = P.inp("identf", [128, 128])
    xmid_o = P.out("xmid", [T * 128, 1024])
    h_o = P.out("h", [T * 128, 1024], BF16)
    aff_o = P.out("aff", [T * 128, 16])
    ident = P.sb("ident_s", [128, 128], BF16); P.dma(ident, identd)
    identf = P.sb("identf_s", [128, 128]); P.dma(identf, identfd)
    stage = P.sb("stage", [128, 1024])
    w_out_b = P.sb("w_out_b", [128, 8, 1024], BF16)
    load_w_bf16(P, w_out_b, w_out, 8, 1024, stage)
    wr = P.sb("wr", [128, 8, 16]); P.dma(wr, w_r)
    G1 = P.sb("G1", [128, 2, 1024]); Gp = P.sb("Gp", [128, 2, 1024]); SH = P.sb("SHt", [128, 2, 1024]); gs = P.sb("gs", [128, 1024])
    P.dma(gs, gfB)
    for c in range(2):
        P.dma(G1[:, c, :], g1B[c])
        P.dma(stage, sc2B[c])
        P.stt(Gp[:, c, :], stage, 1.0, gs, ALU.add, ALU.mult)
        P.dma(SH[:, c, :], sh2B[c])
    if layer_kind == "even":
        gns = P.sb("gns", [128, 512]); P.dma(gns, gn)
    else:
        sks = P.sb("sks", [128, 8]); P.dma(sks, sinkB); lngs = P.sb("lngs", [128, 512]); P.dma(lngs, lngB); lnbs = P.sb("lnbs", [128, 512]); P.dma(lnbs, lnbB)
        gis = [P.sb(f"gis{i}", [128, 512]) for i in range(2)]; bgs = [P.sb(f"bgs{i}", [128, 512]) for i in range(2)]
        nqs = [P.sb(f"nqs{i}", [128, 8]) for i in range(2)]; mu8 = P.sb("mu8", [128, 8]); var8 = P.sb("var8", [128, 8]); den = P.sb("den", [128, 8])
    xs = [P.sb(f"xs{i}", [128, 1024]) for i in range(2)]
    oa = [P.sb(f"oa{i}", [128, 8, 65]) for i in range(2)]
    orr = [P.sb(f"orr{i}", [128, 2, 512]) for i in range(2)]
    rgs = [P.sb(f"rgs{i}", [128, 512]) for i in range(2)]
    cat = P.sb("cat", [128, 1024], BF16); catT = P.sb("catT", [128, 1024], BF16)
    rc = P.sb("rc", [128, 8]); osum = P.sb("osum", [128, 512]); mu = P.sb("mu", [128, 4]); var = P.sb("var", [128, 4])
    junk = P.sb("junk", [128, 1024]); tmp = P.sb("tmp", [128, 1024]); sg = P.sb("sg", [128, 512])
    xm = [P.sb(f"xm{i}", [128, 1024]) for i in range(2)]
    hf = P.sb("hf", [128, 1024]); hb = [P.sb(f"hb{i}", [128, 1024], BF16) for i in range(2)]
    hT = P.sb("hT", [128, 1024]); rs = P.sb("rs", [128, 1])
    lg = P.sb("lg", [128, 16]); mx = P.sb("mx", [128, 1]); sm = P.sb("sm", [128, 1]); af = [P.sb(f"af{i}", [128, 16]) for i in range(2)]
    pT = P.ps("pT", [128, 1024], BF16)
    pTf = [P.ps(f"pTf{i}", [128, 512]) for i in range(2)]
    pp = [P.ps(f"pp{i}", [128, 512]) for i in range(2)]
    pl = P.ps("pl", [128, 16])
    for t in range(T):
        cls = 1 if (t == 0 and has_ctx) else 0
        i2 = t % 2
        rows = slice(t * 128, (t + 1) * 128)
        P.dma(xs[i2], x[rows, :])
        if layer_kind == "even":
            P.dma(oa[i2], Oatt[rows]); P.dma(orr[i2], Oret[rows]); P.dma(rgs[i2], RG[rows, :])
            P.recip(rc, oa[i2][:, :, 64])
            P.tt(cat[:, 0:512].re("p (h d) -> p h d", d=64), oa[i2][:, :, 0:64], V(rc.key, rc.ap.unsqueeze(2).to_broadcast([128, 8, 64])), ALU.mult)
            P.tt(osum, orr[i2][:, 0, :], orr[i2][:, 1, :], ALU.add)
            ov = osum.re("p (h d) -> p h d", d=128)
            P.reduce(mu, ov, ALU.add)
            P.ts(mu, mu, 1.0 / 128, ALU.mult)
            P.tt(ov, ov, V(mu.key, mu.ap.unsqueeze(2).to_broadcast([128, 4, 128])), ALU.subtract)
            P.tt(junk[:, :512], osum, osum, ALU.mult)
            P.reduce(var, junk[:, :512].re("p (h d) -> p h d", d=128), ALU.add)
            P.ts(var, var, 1.0 / 128, ALU.mult, 1e-5, ALU.add)
            P.act(var, var, AF.Sqrt)
            P.recip(var, var)
            P.tt(ov, ov, V(var.key, var.ap.unsqueeze(2).to_broadcast([128, 4, 128])), ALU.mult)
            P.tt(osum, osum, gns, ALU.mult)
            P.act(sg, rgs[i2], AF.Sigmoid)
            P.tt(sg, sg, rgs[i2], ALU.mult)
            P.tt(cat[:, 512:1024], osum, sg, ALU.mult)
        else:
            P.dma(orr[i2], Yr[rows]); P.dma(gis[i2], Gi[rows, :]); P.dma(bgs[i2], BGi[rows, :]); P.dma(oa[i2], Ow[rows]); P.dma(nqs[i2], NQi[rows, :])
            P.tt(osum, orr[i2][:, 0, :], orr[i2][:, 1, :], ALU.add)
            ov = osum.re("p (h d) -> p h d", d=64)
            P.reduce(mu8, ov, ALU.add)
            P.ts(mu8, mu8, 1.0 / 64, ALU.mult)
            P.tt(ov, ov, V(mu8.key, mu8.ap.unsqueeze(2).to_broadcast([128, 8, 64])), ALU.subtract)
            P.tt(junk[:, :512], osum, osum, ALU.mult)
            P.reduce(var8, junk[:, :512].re("p (h d) -> p h d", d=64), ALU.add)
            P.ts(var8, var8, 1.0 / 64, ALU.mult, 64e-5, ALU.add)
            P.act(var8, var8, AF.Sqrt)
            P.recip(var8, var8)
            P.tt(ov, ov, V(var8.key, var8.ap.unsqueeze(2).to_broadcast([128, 8, 64])), ALU.mult)
            P.tt(osum, osum, lngs, ALU.mult)
            P.tt(osum, osum, lnbs, ALU.add)
            P.tt(osum, osum, gis[i2], ALU.mult)
            P.tt(cat[:, 0:512], osum, bgs[i2], ALU.add)
            P.tt(den, nqs[i2], sks, ALU.add)
            P.act(den, den, AF.Exp)
            P.tt(den, den, oa[i2][:, :, 64], ALU.add)
            P.recip(rc, den)
            P.tt(cat[:, 512:1024].re("p (h d) -> p h d", d=64), oa[i2][:, :, 0:64], V(rc.key, rc.ap.unsqueeze(2).to_broadcast([128, 8, 64])), ALU.mult)
        for k in range(8):
            P.tr(pT[:, k * 128:(k + 1) * 128], cat[:, k * 128:(k + 1) * 128], ident)
        P.copy(catT, pT)
        for j in range(2):
            for k in range(8):
                P.mm(pp[j], catT[:, k * 128:(k + 1) * 128], w_out_b[:, k, j * 512:(j + 1) * 512], start=(k == 0), stop=(k == 7))
            P.tt(tmp[:, j * 512:(j + 1) * 512], pp[j], G1[:, cls, j * 512:(j + 1) * 512], ALU.mult)
        P.tt(xm[i2], tmp, xs[i2], ALU.add)
        P.dma(xmid_o[rows, :], xm[i2])
        rms_rstd(P, rs, junk, xm[i2], 1024)
        P.stt(tmp, xm[i2], rs, Gp[:, cls, :], ALU.mult, ALU.mult)
        P.tt(hf, tmp, SH[:, cls, :], ALU.add)
        P.copy(hb[i2], hf, eng="act")
        P.dma(h_o[rows, :], hb[i2])
        for k in range(8):
            P.tr(pTf[k // 4][:, (k % 4) * 128:(k % 4 + 1) * 128], hf[:, k * 128:(k + 1) * 128], identf)
        for j in range(2):
            P.copy(hT[:, j * 512:(j + 1) * 512], pTf[j], eng=("act" if j else "dve"))
        for k in range(8):
            P.mm(pl, hT[:, k * 128:(k + 1) * 128], wr[:, k, :], start=(k == 0), stop=(k == 7))
        P.copy(lg, pl)
        P.reduce(mx, lg, ALU.max)
        P.ts(lg, lg, mx, ALU.subtract)
        P.act(lg, lg, AF.Exp)
        P.reduce(sm, lg, ALU.add)
        P.recip(sm, sm)
        P.ts(af[i2], lg, sm, ALU.mult)
        P.dma(aff_o[rows, :], af[i2])
    return P


BIG = 4000000.0

def k5_build(probs=((128, 2048), (2, 32)), NE=4, NIT=34):
    P = Prog()
    nc = P.nc
    ins = []
    for pi, (J, cap) in enumerate(probs):
        ins.append((P.inp(f"aff{pi}", [128, J, NE]), P.inp(f"h{pi}", [128, J, 1024], BF16),
                    P.out(f"ys{pi}", [NE, cap, 1024], BF16), P.out(f"dest{pi}", [128, J, NE], I32), P.out(f"gsel{pi}", [128, J, NE]),
                    [P.scratch(f"xs{pi}_{e}", [cap, 1024], BF16) for e in range(NE)]))
    wg = P.inp("wg", [NE, 128, 8, 1536]); wu = P.inp("wu", [NE, 128, 8, 1536]); wd = P.inp("wd", [NE, 128, 12, 1024])
    trid = P.inp("tri", [128, 128]); onesd = P.inp("ones", [128, 128]); identd = P.inp("ident", [128, 128], BF16)
    tri = P.sb("tri_s", [128, 128]); ones = P.sb("ones_s", [128, 128]); ident = P.sb("ident_s", [128, 128], BF16)
    P.dma(tri, trid); P.dma(ones, onesd); P.dma(ident, identd)
    JM = max(j for j, _ in probs)
    onesJ = P.sb("onesJ", [128, JM]); P.memset(onesJ, 1.0)
    af = P.sb("af", [128, JM, NE]); cmpb = P.sb("cmpb", [128, JM, NE]); cs = P.sb("cs", [128, JM, NE])
    lo = P.sb("lo", [128, NE]); hi = P.sb("hi", [128, NE]); mid = P.sb("mid", [128, NE]); cntp = P.sb("cntp", [128, NE])
    pred = P.sb("pred", [128, NE]); d1 = P.sb("d1", [128, NE]); d2 = P.sb("d2", [128, NE]); offs = P.sb("offs", [128, NE])
    desti = [P.sb(f"desti{pi}", [128, J, NE], I32) for pi, (J, cap) in enumerate(probs)]
    pt = P.ps("pt", [128, NE])
    hrow = [P.sb(f"hrow{i}", [128, 1024], BF16) for i in range(3)]
    for pi, (J, cap) in enumerate(probs):
        aff_i, h_i, ys_o, dest_o, gsel_o, xs = ins[pi]
        a = af[:, :J, :]; cb = cmpb[:, :J, :]; c_ = cs[:, :J, :]
        P.dma(a, aff_i)
        P.memset(lo, 0.0); P.memset(hi, 1.5)
        def bcj(v): return V(v.key, v.ap.unsqueeze(1).to_broadcast([128, J, NE]))
        for it in range(NIT):
            P.tt(mid, lo, hi, ALU.add)
            P.ts(mid, mid, 0.5, ALU.mult)
            P.tt(cb, a, bcj(mid), ALU.is_ge)
            P.reduce(cntp, cb.re("p j e -> p e j"), ALU.add)
            P.mm(pt, ones, cntp)
            P.ts(pred, pt, cap - 0.5, ALU.is_ge)
            P.tt(d1, mid, lo, ALU.subtract)
            P.tt(d1, d1, pred, ALU.mult)
            P.tt(d2, hi, mid, ALU.subtract)
            P.tt(d2, d2, pred, ALU.mult)
            P.tt(lo, lo, d1, ALU.add)
            P.tt(hi, mid, d2, ALU.add)
        P.tt(cb, a, bcj(lo), ALU.is_ge)
        P.reduce(cntp, cb.re("p j e -> p e j"), ALU.add)
        P.mm(pt, tri, cntp)
        P.ts(offs, pt, -(1.0 + BIG), ALU.add)
        for e in range(NE):
            P.scan(c_[:, :, e], onesJ[:, :J], cb[:, :, e], 0.0, ALU.mult, ALU.add)
        P.tt(c_, c_, bcj(offs), ALU.add)
        P.tt(c_, c_, cb, ALU.mult)
        P.ts(c_, c_, BIG, ALU.add)
        P.copy(desti[pi], c_)
        P.dma(dest_o, desti[pi])
        P.tt(cb, cb, a, ALU.mult)
        P.dma(gsel_o, cb)
        for j in range(J):
            hr = hrow[j % 3]
            P.dma(hr, h_i[:, j, :])
            for e in range(NE):
                idx = desti[pi][:, j, e:e + 1]
                P.op("pool", (lambda g, e=e, idx=idx, hr=hr, xs=xs, cap=cap: g.indirect_dma_start(
                    out=xs[e].ap, out_offset=bass.IndirectOffsetOnAxis(ap=idx.ap, axis=0), in_=hr.ap, in_offset=None,
                    bounds_check=P.bcreg(g, cap - 1), oob_is_err=False)), [hr, idx], [xs[e]], kind="d")
    stage = P.sb("stage", [128, 1536])
    wgb = P.sb("wgb", [128, 8, 1536], BF16); wub = P.sb("wub", [128, 8, 1536], BF16); wdb = P.sb("wdb", [128, 12, 1024], BF16)
    xr = [P.sb(f"xr{i}", [128, 1024], BF16) for i in range(2)]
    xT = P.sb("xT", [128, 8, 512], BF16)
    uT = P.sb("uT", [128, 12, 512], BF16)
    sgt = [P.sb(f"sgt{i}", [128, 512]) for i in range(2)]
    yb = [P.sb(f"yb{i}", [128, 1024], BF16) for i in range(2)]
    pT = P.ps("pT", [128, 1024], BF16)
    pg = [P.ps(f"pg{i}", [128, 512]) for i in range(2)]
    pu = [P.ps(f"pu{i}", [128, 512]) for i in range(2)]
    py = [P.ps(f"py{i}", [128, 512]) for i in range(2)]
    n = 0
    for e in range(NE):
        load_w_bf16(P, wgb, wg[e], 8, 1536, stage)
        load_w_bf16(P, wub, wu[e], 8, 1536, stage)
        load_w_bf16(P, wdb, wd[e], 12, 1024, stage)
        for pi, (J, cap) in enumerate(probs):
            aff_i, h_i, ys_o, dest_o, gsel_o, xs = ins[pi]
            for g0 in range(0, cap, 512):
                gw = min(512, cap - g0)
                nblk = (gw + 127) // 128
                for bi in range(nblk):
                    r0 = g0 + bi * 128; rw = min(128, cap - r0)
                    x_ = xr[bi % 2]
                    P.dma(x_[:rw, :], xs[e][r0:r0 + rw, :])
                    for k in range(8):
                        P.tr(pT[:, k * 128:k * 128 + rw], x_[:rw, k * 128:(k + 1) * 128], ident[:rw, :rw])
                    P.copy(xT[:, :, bi * 128:bi * 128 + rw], pT.re("p (k t) -> p k t", k=8)[:, :, :rw], eng=("act" if bi % 2 else "dve"))
                for fc in range(12):
                    i2 = n % 2; n += 1
                    for k in range(8):
                        P.mm(pg[i2][:, :gw], wgb[:, k, fc * 128:(fc + 1) * 128], xT[:, k, :gw], start=(k == 0), stop=(k == 7))
                    for k in range(8):
                        P.mm(pu[i2][:, :gw], wub[:, k, fc * 128:(fc + 1) * 128], xT[:, k, :gw], start=(k == 0), stop=(k == 7))
                    P.act(sgt[i2][:, :gw], pg[i2][:, :gw], AF.Sigmoid)
                    P.tt(sgt[i2][:, :gw], sgt[i2][:, :gw], pg[i2][:, :gw], ALU.mult)
                    P.tt(uT[:, fc, :gw], sgt[i2][:, :gw], pu[i2][:, :gw], ALU.mult)
                for bi in range(nblk):
                    r0 = g0 + bi * 128; rw = min(128, cap - r0)
                    y_ = yb[bi % 2]
                    for j in range(2):
                        for fc in range(12):
                            P.mm(py[j][:rw, :], uT[:, fc, bi * 128:bi * 128 + rw], wdb[:, fc, j * 512:(j + 1) * 512], start=(fc == 0), stop=(fc == 11))
                        P.copy(y_[:rw, j * 512:(j + 1) * 512], py[j][:rw, :], eng=("act" if j else "dve"))
                    P.dma(ys_o[e, r0:r0 + rw, :], y_[:rw, :])
    return P


def k6_build(T, caps=(2048, 32), final=False, NE=16):
    P = Prog()
    xmid = P.inp("xmid", [T * 128, 1024])
    gsel = P.inp("gsel", [T * 128, NE])
    dest = P.inp("dest", [T * 128, NE], I32)
    ys = [[P.inp(f"ys{c}_{e}", [caps[c], 1024], BF16) for e in range(NE)] for c in range(len(caps))]
    g2B = P.inp("g2B", [2, 128, 1024])
    xo = P.out("xo", [T * 128, 1024])
    G2 = P.sb("G2", [128, 2, 1024])
    for c in range(2):
        P.dma(G2[:, c, :], g2B[c])
    if final:
        fgB = P.inp("fgB", [128, 1024]); fg = P.sb("fg", [128, 1024]); P.dma(fg, fgB)
        fo = P.out("fo", [T * 128, 1024])
        junk = P.sb("junk", [128, 1024]); rs = P.sb("rs", [128, 1])
    xs = [P.sb(f"xs{i}", [128, 1024]) for i in range(2)]
    gs = [P.sb(f"gs{i}", [128, NE]) for i in range(2)]
    ds = [P.sb(f"ds{i}", [128, NE], I32) for i in range(2)]
    gt = [P.sb(f"gt{i}", [128, 1024], BF16) for i in range(4)]
    for g in gt:
        P.memset(g, 0.0)
    acc = P.sb("acc", [128, 1024])
    ob = [P.sb(f"ob{i}", [128, 1024]) for i in range(2)]
    fb = [P.sb(f"fb{i}", [128, 1024]) for i in range(2)]
    n = 0
    for t in range(T):
        cls = 1 if (t == 0 and len(caps) > 1) else 0
        i2 = t % 2
        rows = slice(t * 128, (t + 1) * 128)
        P.dma(xs[i2], xmid[rows, :]); P.dma(gs[i2], gsel[rows, :]); P.dma(ds[i2], dest[rows, :])
        P.memset(acc, 0.0)
        for e in range(NE):
            g = gt[n % 4]; n += 1
            idx = ds[i2][:, e:e + 1]
            src = ys[cls][e]
            P.op("pool", (lambda q, e=e, idx=idx, g=g, src=src, cap=caps[cls]: q.indirect_dma_start(
                out=g.ap, out_offset=None, in_=src.ap, in_offset=bass.IndirectOffsetOnAxis(ap=idx.ap, axis=0),
                bounds_check=P.bcreg(q, cap - 1), oob_is_err=False)), [src, idx], [g], kind="d")
            P.stt(acc, g, gs[i2][:, e:e + 1], acc, ALU.mult, ALU.add)
        P.tt(acc, acc, G2[:, cls, :], ALU.mult)
        P.tt(ob[i2], acc, xs[i2], ALU.add)
        P.dma(xo[rows, :], ob[i2])
        if final:
            rms_rstd(P, rs, junk, ob[i2], 1024)
            P.stt(fb[i2], ob[i2], rs, fg, ALU.mult, ALU.mult)
            P.dma(fo[rows, :], fb[i2])
    return P


SQW = 64 ** -0.25
RW_CH = [(0, 512), (512, 1024), (1024, 1536), (1536, 1920)]
WIN_CH = [(1920, 2432), (2432, 2688)]

def k7_build(T):
    P = Prog()
    x = P.inp("x", [T * 128, 1024]); xp = P.inp("xp", [T * 128, 1024]); xn = P.inp("xn", [T * 128, 1024]); fl = P.inp("fl", [T * 128, 2])
    gB = P.inp("gB", [128, 1024]); scB = P.inp("scB", [2, 128, 1024]); shB = P.inp("shB", [2, 128, 1024])
    w_in = P.inp("w_in", [128, 8, 2688]); muB = P.inp("muB", [128, 1920])
    kkB = P.inp("kkB", [128, 512]); kaB = P.inp("kaB", [128, 512]); rkB = P.inp("rkB", [128, 512])
    w0B = P.inp("w0B", [2, 128, 512]); a0B = P.inp("a0B", [2, 128, 512])
    w2s = P.inp("w2s", [128, 1, 512]); a2s = P.inp("a2s", [128, 1, 512]); g2s = P.inp("g2s", [128, 1, 512])
    Cw = P.inp("Cw", [T * 128, 64]); Sw = P.inp("Sw", [T * 128, 64])
    identd = P.inp("ident", [128, 128], BF16)
    onames = ["R", "Vv", "KK", "LWf", "LWb", "Af", "Ab", "Kf", "Kb", "G", "BG"]
    outs = {n: P.out(n, [T * 128, 512]) for n in onames}
    NQ_o = P.out("NQ", [T * 128, 8])
    Qw_o = P.out("Qw", [T * 128, 8 * 66], BF16); Kw_o = P.out("Kw", [T * 128, 2 * 66], BF16); Vw_o = P.out("Vw", [T * 128, 2 * 65], BF16)
    ident = P.sb("ident_s", [128, 128], BF16); P.dma(ident, identd)
    stage = P.sb("stage", [128, 2688])
    Pm = P.sb("Pm", [128, 2688]); tmpw = Pm[:, :1920]
    mus = P.sb("mus", [128, 1920]); P.dma(mus, muB)
    W1 = P.sb("W1", [128, 8, 1920], BF16); W2 = P.sb("W2", [128, 8, 1920], BF16); Ww = P.sb("Ww", [128, 8, 768], BF16)
    for k in range(8):
        P.dma(stage, w_in[:, k, :])
        P.tt(tmpw, stage[:, :1920], mus, ALU.mult)
        P.copy(W2[:, k, :], tmpw, eng="act")
        P.tt(W1[:, k, :], stage[:, :1920], tmpw, ALU.subtract)
        P.copy(Ww[:, k, :], stage[:, 1920:2688], eng="act")
    w2b = P.sb("w2b", [128, 1, 512], BF16); a2b = P.sb("a2b", [128, 1, 512], BF16); g2b = P.sb("g2b", [128, 1, 512], BF16)
    load_w_bf16(P, w2b, w2s, 1, 512, stage); load_w_bf16(P, a2b, a2s, 1, 512, stage); load_w_bf16(P, g2b, g2s, 1, 512, stage)
    cB = {}
    for n, d in (("kk", kkB), ("ka", kaB), ("rk", rkB)):
        cB[n] = P.sb(n + "_s", [128, 512]); P.dma(cB[n], d)
    w0s = P.sb("w0s", [128, 2, 512]); a0s = P.sb("a0s", [128, 2, 512])
    for d in range(2):
        P.dma(w0s[:, d, :], w0B[d]); P.dma(a0s[:, d, :], a0B[d])
    gs = P.sb("gs", [128, 1024]); P.dma(gs, gB)
    Gp = P.sb("Gp", [128, 2, 1024]); SH = P.sb("SHt", [128, 2, 1024])
    for c in range(2):
        P.dma(stage[:, :1024], scB[c])
        P.stt(Gp[:, c, :], stage[:, :1024], 1.0, gs, ALU.add, ALU.mult)
        P.dma(SH[:, c, :], shB[c])
    xs = [[P.sb(f"xs{i}_{j}", [128, 1024]) for j in range(3)] for i in range(1)] * 2
    fls = [P.sb(f"fls{i}", [128, 2]) for i in range(2)]
    tabs = [[P.sb(f"tab{i}_{j}", [128, 64]) for j in range(2)] for i in range(2)]
    junk = P.sb("junk", [128, 1024]); tmp = P.sb("tmp", [128, 1024]); rs = P.sb("rs", [128, 1])
    a_bf = P.sb("a_bf", [128, 1024], BF16); af32 = [P.sb(f"af32_{j}", [128, 1024]) for j in range(2)]
    ash = P.sb("ash", [128, 1024], BF16)
    aT = P.sb("aT", [128, 1024], BF16); ashT = P.sb("ashT", [128, 1024], BF16)
    sm = {n: P.sb("sm_" + n, [128, 8]) for n in ("ss", "srk", "nq")}
    nk2 = P.sb("nk2", [128, 2]); ek2 = P.sb("ek2", [128, 2])
    bft = {n: P.sb("bf_" + n, [128, 128], BF16) for n in ("th", "al", "sg")}
    bfT = {n: P.sb("bfT_" + n, [128, 128], BF16) for n in ("th", "al", "sg")}
    ob = {n: [P.sb(f"ob_{n}{i}", [128, 512]) for i in range(1)] * 2 for n in onames if n not in ("R", "Vv")}
    t5 = P.sb("t5", [128, 512]); t6 = P.sb("t6", [128, 512]); qf = P.sb("qf", [128, 512]); kf = P.sb("kf", [128, 128])
    NQb = [P.sb(f"NQb{i}", [128, 8]) for i in range(2)]
    Qw = [P.sb(f"Qw{i}", [128, 8, 66], BF16) for i in range(2)]
    Kw = [P.sb(f"Kw{i}", [128, 2, 66], BF16) for i in range(2)]
    Vw = [P.sb(f"Vw{i}", [128, 2, 65], BF16) for i in range(2)]
    pT = P.ps("pT", [128, 1024], BF16)
    pp = [P.ps(f"pp{i}", [128, 512]) for i in range(6)]
    pcnt = [0]
    def npp():
        p = pp[pcnt[0] % 6]; pcnt[0] += 1
        return p

    def norm_mod(dst, xt, cls):
        rms_rstd(P, rs, junk, xt, 1024)
        P.stt(tmp, xt, rs, Gp[:, cls, :], ALU.mult, ALU.mult)
        P.tt(dst, tmp, SH[:, cls, :], ALU.add)

    def rope(dst, src, nh, tb, scale):
        w = nh * 64
        Cb = V(tb[0].key, tb[0].ap.unsqueeze(1).to_broadcast([128, nh, 64]))
        t1v = t5[:, :w].re("p (h d) -> p h d", d=64); t2v = t6[:, :w].re("p (h d) -> p h d", d=64)
        P.tt(t1v, src, Cb, ALU.mult)
        s5 = src.re("p h (b f e) -> p h b f e", b=2, f=2); t25 = t2v.re("p h (b f e) -> p h b f e", b=2, f=2)
        S5 = tb[1].ap.rearrange("p (b f e) -> p b f e", b=2, f=2)
        for f in range(2):
            Sb = V(tb[1].key, S5[:, :, f, :].unsqueeze(1).to_broadcast([128, nh, 2, 16]))
            P.tt(t25[:, :, :, f, :], s5[:, :, :, 1 - f, :], Sb, ALU.mult)
        P.tt(t1v, t1v, t2v, ALU.add)
        P.ts(dst, t1v, scale, ALU.mult)

    for t in range(T):
        cls = 1 if t == 0 else 0
        i2 = t % 2
        rows = slice(t * 128, (t + 1) * 128)
        xt, xpt, xnt = xs[i2]
        P.dma(xt, x[rows, :]); P.dma(xpt, xp[rows, :]); P.dma(xnt, xn[rows, :]); P.dma(fls[i2], fl[rows, :])
        tb = tabs[i2]
        P.dma(tb[0], Cw[rows, :]); P.dma(tb[1], Sw[rows, :])
        norm_mod(a_bf, xt, cls)
        norm_mod(af32[0], xpt, cls)
        norm_mod(af32[1], xnt, cls)
        P.ts(af32[0], af32[0], fls[i2][:, 0:1], ALU.mult, 0.5, ALU.mult)
        P.ts(af32[1], af32[1], fls[i2][:, 1:2], ALU.mult, 0.5, ALU.mult)
        P.tt(ash, af32[0], af32[1], ALU.add)
        for k in range(8):
            P.tr(pT[:, k * 128:(k + 1) * 128], a_bf[:, k * 128:(k + 1) * 128], ident)
        P.copy(aT, pT)
        for k in range(8):
            P.tr(pT[:, k * 128:(k + 1) * 128], ash[:, k * 128:(k + 1) * 128], ident)
        P.copy(ashT, pT)
        for j, (c0, c1) in enumerate(RW_CH):
            p = npp()
            for k in range(8):
                P.mm(p[:, :c1 - c0], aT[:, k * 128:(k + 1) * 128], W1[:, k, c0:c1], start=(k == 0), stop=False)
            for k in range(8):
                P.mm(p[:, :c1 - c0], ashT[:, k * 128:(k + 1) * 128], W2[:, k, c0:c1], start=False, stop=(k == 7))
            P.copy(Pm[:, c0:c1], p[:, :c1 - c0], eng=("act" if j % 2 else "dve"))
        for j, (c0, c1) in enumerate(WIN_CH):
            p = npp()
            for k in range(8):
                P.mm(p[:, :c1 - c0], aT[:, k * 128:(k + 1) * 128], Ww[:, k, c0 - 1920:c1 - 1920], start=(k == 0), stop=(k == 7))
            P.copy(Pm[:, c0:c1], p[:, :c1 - c0], eng=("act" if j % 2 else "dve"))
        O = {n: ob[n][i2] for n in onames if n not in ("R", "Vv")}
        r_, k_, v_ = Pm[:, 0:512], Pm[:, 512:1024], Pm[:, 1024:1536]
        O["R"] = r_; O["Vv"] = v_
        P.tt(t5, k_, cB["kk"], ALU.mult)
        P.tt(t6, t5, t5, ALU.mult)
        P.reduce(sm["ss"], t6.re("p (h d) -> p h d", d=64), ALU.add)
        P.ts(sm["ss"], sm["ss"], 1e-12, ALU.add)
        P.act(sm["ss"], sm["ss"], AF.Sqrt)
        P.recip(sm["ss"], sm["ss"])
        P.tt(O["KK"].re("p (h d) -> p h d", d=64), t5.re("p (h d) -> p h d", d=64), V(sm["ss"].key, sm["ss"].ap.unsqueeze(2).to_broadcast([128, 8, 64])), ALU.mult)
        P.act(bft["th"], Pm[:, 1536:1664], AF.Tanh)
        P.copy(bft["al"], Pm[:, 1664:1792])
        P.act(bft["sg"], Pm[:, 1792:1920], AF.Sigmoid)
        for i, n in enumerate(("th", "al", "sg")):
            P.tr(pT[:, i * 128:(i + 1) * 128], bft[n], ident)
            P.copy(bfT[n], pT[:, i * 128:(i + 1) * 128], eng=("act" if i % 2 else "dve"))
        for d, (lw_n, a_n, k_n) in enumerate((("LWf", "Af", "Kf"), ("LWb", "Ab", "Kb"))):
            ps_ = slice(d * 64, (d + 1) * 64)
            p = npp(); P.mm(p, bfT["th"][ps_, :], w2b[ps_, 0, :])
            P.tt(t5, p, w0s[:, d, :], ALU.add)
            P.act(t5, t5, AF.Sigmoid)
            P.ts(O[lw_n], t5, -0.6065306597126334, ALU.mult)
            p = npp(); P.mm(p, bfT["al"][ps_, :], a2b[ps_, 0, :])
            P.tt(t5, p, a0s[:, d, :], ALU.add)
            P.act(O[a_n], t5, AF.Sigmoid)
            P.stt(t6, O[a_n], -1.0, cB["ka"], ALU.add, ALU.mult)
            P.stt(O[k_n], t6, 1.0, k_, ALU.add, ALU.mult)
        p = npp(); P.mm(p, bfT["sg"], g2b[:, 0, :])
        P.copy(O["G"], p, eng="act")
        P.tt(t5, r_, k_, ALU.mult)
        P.tt(t5, t5, cB["rk"], ALU.mult)
        P.reduce(sm["srk"], t5.re("p (h d) -> p h d", d=64), ALU.add)
        P.tt(t6.re("p (h d) -> p h d", d=64), v_.re("p (h d) -> p h d", d=64), V(sm["srk"].key, sm["srk"].ap.unsqueeze(2).to_broadcast([128, 8, 64])), ALU.mult)
        P.tt(O["BG"], t6, O["G"], ALU.mult)
        for n in onames:
            P.dma(outs[n][rows, :], O[n])
        qv = qf.re("p (h d) -> p h d", d=64)
        rope(qv, Pm[:, 1920:2432].re("p (h d) -> p h d", d=64), 8, tb, SQW)
        Qt = Qw[i2]
        P.copy(Qt[:, :, 0:64], qv, eng="act")
        P.tt(t5, qf, qf, ALU.mult)
        P.reduce(sm["nq"], t5.re("p (h d) -> p h d", d=64), ALU.add)
        P.ts(NQb[i2], sm["nq"], -0.5, ALU.mult)
        P.copy(Qt[:, :, 64], NQb[i2])
        P.memset(Qt[:, :, 65], 1.0)
        P.dma(NQ_o[rows, :], NQb[i2])
        P.dma(Qw_o[rows, :], Qt.re("p h d -> p (h d)"))
        kv = kf.re("p (h d) -> p h d", d=64)
        rope(kv, Pm[:, 2432:2560].re("p (h d) -> p h d", d=64), 2, tb, SQW)
        Kt = Kw[i2]; Vt = Vw[i2]
        P.copy(Kt[:, :, 0:64], kv, eng="act")
        P.memset(Kt[:, :, 64], 1.0)
        P.tt(t5[:, :128], kf, kf, ALU.mult)
        P.reduce(nk2, t5[:, :128].re("p (h d) -> p h d", d=64), ALU.add)
        P.ts(Kt[:, :, 65], nk2, -0.5, ALU.mult)
        P.act(ek2, Kt[:, :, 65], AF.Exp, scale=-1.0)
        P.tt(Vt[:, :, 0:64], Pm[:, 2560:2688].re("p (h d) -> p h d", d=64), V(ek2.key, ek2.ap.unsqueeze(2).to_broadcast([128, 2, 64])), ALU.mult)
        P.copy(Vt[:, :, 64], ek2)
        P.dma(Kw_o[rows, :], Kt.re("p h d -> p (h d)"))
        P.dma(Vw_o[rows, :], Vt.re("p h d -> p (h d)"))
    return P


L = 64

def k8_consts():
    s = np.arange(L)[:, None]; t = np.arange(L)[None, :]
    t4 = lambda m: np.ascontiguousarray(np.tile(m.astype(np.float32), (1, 4)))
    return {"triI": (s <= t).astype(np.float32), "ones": np.ones((L, L), np.float32), "mS": t4(s < t), "mST": t4(s > t), "mI": t4(s <= t),
            "I4": t4(np.eye(L)), "identf": np.eye(L, dtype=np.float32)}

def k8_build(NCH, NCTX, W=3):
    P = Prog()
    names = ["R", "K", "Vv", "KK", "A", "LW"]
    din = {n: P.inp(n, [NCH, L, 256]) for n in names}
    yo = P.out("y", [NCH - NCTX, L, 256])
    cst = {}
    for n, w in (("triI", 64), ("ones", 64), ("mS", 256), ("mST", 256), ("mI", 256), ("I4", 256), ("identf", 64)):
        d = P.inp(n, [L, w]); cst[n] = P.sb(n + "_s", [L, w]); P.dma(cst[n], d)
    tl = {}
    cur = [0]
    def T_(n, w=256, nb=1):
        k = (n, cur[0])
        if k not in tl:
            tl[k] = [P.sb(f"{n}_s{cur[0]}", [L, w])] * 2
        return tl[k]
    pss = [P.ps(f"ps{i}", [L, 512]) for i in range(8)]
    pc = [0]
    def nps():
        p = pss[pc[0] % 8]; pc[0] += 1
        return p[:, :256]
    H = [slice(h * 64, (h + 1) * 64) for h in range(4)]
    def mm4(ps, A_, B_, start=True, stop=True):
        for h in range(4):
            P.mm(ps[:, H[h]], A_[:, H[h]], B_[:, H[h]], start=start, stop=stop)
    ST = P.sb("ST", [L, 256]); P.memset(ST, 0.0)
    def chunk(c):
        i2 = 0
        X = {}
        for n in names:
            X[n] = T_("in_" + n, nb=2)[i2]
            P.dma(X[n], din[n][c])
        R, K, Vv, KK, A, LW = (X[n] for n in names)
        pA = nps(); P.mm(pA, cst["triI"], LW)
        pB = nps(); P.mm(pB, cst["ones"], LW)
        yield
        cum = T_("cum")[0]; P.copy(cum, pA)
        eP = T_("eP")[0]; P.act(eP, cum, AF.Exp)
        eN = T_("eN")[0]; P.act(eN, cum, AF.Exp, scale=-1.0)
        yield
        t1 = T_("t1")[0]; P.tt(t1, cum, LW, ALU.subtract)
        ePx = T_("ePx")[0]; P.act(ePx, t1, AF.Exp)
        t2 = T_("t2")[0]; P.tt(t2, pB, cum, ALU.subtract)
        eT = T_("eT")[0]; P.act(eT, t2, AF.Exp)
        yield
        al = T_("al")[0]; P.stt(al, KK, -1.0, ePx, ALU.mult, ALU.mult)
        be = T_("be")[0]; P.tt(be, KK, A, ALU.mult)
        bcn = T_("bcn")[0]; P.tt(bcn, be, eN, ALU.mult)
        kc = T_("kc")[0]; P.tt(kc, K, eN, ALU.mult)
        rt = T_("rt")[0]; P.tt(rt, R, eP, ALU.mult)
        bh = T_("bh")[0]; P.tt(bh, be, eT, ALU.mult)
        kh = T_("kh")[0]; P.tt(kh, K, eT, ALU.mult)
        yield
        pPL = nps()
        for h in range(4):
            P.mm(pPL[:, h:h + 1], LW[:, H[h]], cst["ones"][:, 0:1])
        PL = T_("PL", 4)[0]; P.act(PL, pPL[:, 0:4], AF.Exp)
        yield
        TT = {}
        for n, src in (("alT", al), ("bcT", bcn), ("kcT", kc), ("rtT", rt)):
            p = nps()
            for h in range(4):
                P.tr(p[:, H[h]], src[:, H[h]], cst["identf"])
            TT[n] = T_(n)[0]; P.copy(TT[n], p, eng="act")
            yield
        def gram(name, a, b, mask):
            p = nps(); mm4(p, TT[a], TT[b])
            o = T_(name)[0]; P.tt(o, p, cst[mask], ALU.mult)
            return o
        M = gram("M0", "bcT", "alT", "mS")
        N_ = gram("N0", "alT", "bcT", "mST")
        AakT = gram("AakT", "kcT", "alT", "mS")
        ArbT = gram("ArbT", "bcT", "rtT", "mI")
        ArkT = gram("ArkT", "kcT", "rtT", "mI")
        yield
        Tt = T_("Tt")[0]; P.tt(Tt, M, cst["I4"], ALU.add)
        yield
        for i in range(5):
            pM = nps(); mm4(pM, N_, M)
            pN = nps(); mm4(pN, M, N_)
            Mn = T_(f"Mn{i % 2}")[0]; Nn = T_(f"Nn{i % 2}")[0]
            P.copy(Mn, pM, eng="act"); P.copy(Nn, pN)
            yield
            pX = nps(); mm4(pX, Nn, Tt)
            P.tt(Tt, Tt, pX, ALU.add)
            yield
            M, N_ = Mn, Nn
        p = nps(); mm4(p, AakT, Vv); AV = T_("AV")[0]; P.copy(AV, p, eng="act")
        yield
        p = nps(); mm4(p, Tt, AV); U0 = T_("U0")[0]; P.copy(U0, p)
        yield
        p = nps(); mm4(p, Tt, al); Ah = T_("Ah")[0]; P.copy(Ah, p, eng="act")
        yield
        p = nps(); mm4(p, Ah, bh); GT = T_("GT")[0]; P.copy(GT, p)
        p = nps(); mm4(p, Ah, ArbT); RhT = T_("RhT")[0]; P.tt(RhT, p, TT["rtT"], ALU.add)
        yield
        if c >= NCTX:
            p = nps()
            for h in range(4):
                P.mm(p[:, H[h]], ArbT[:, H[h]], U0[:, H[h]], start=True, stop=False)
                P.mm(p[:, H[h]], ArkT[:, H[h]], Vv[:, H[h]], start=False, stop=False)
                P.mm(p[:, H[h]], RhT[:, H[h]], ST[:, H[h]], start=False, stop=True)
            yb = T_("yb", nb=2)[i2]; P.copy(yb, p, eng="act")
            P.dma(yo[c - NCTX], yb)
        p = nps()
        for h in range(4):
            P.mm(p[:, H[h]], bh[:, H[h]], U0[:, H[h]], start=True, stop=False)
            P.mm(p[:, H[h]], kh[:, H[h]], Vv[:, H[h]], start=False, stop=False)
            P.mm(p[:, H[h]], GT[:, H[h]], ST[:, H[h]], start=False, stop=True)
        for h in range(4):
            P.stt(ST[:, H[h]], ST[:, H[h]], PL[:, h:h + 1], p[:, H[h]], ALU.mult, ALU.add)

    active = []
    nxt = 0
    while nxt < NCH or active:
        if nxt < NCH and len(active) < W:
            active.append((nxt % W, chunk(nxt))); nxt += 1
        for item in list(active):
            cur[0] = item[0]
            try:
                next(item[1])
            except StopIteration:
                active.remove(item)
    return P


def k9_consts():
    import numpy as np
    kj = np.arange(128)[:, None]; qi = np.arange(128)[None, :]
    mL = (kj >= qi).astype(np.float32)
    mR = (kj <= qi).astype(np.float32)
    t4 = lambda m: np.ascontiguousarray(np.tile(m, (1, 4)))
    return {"mL": t4(mL), "mR": t4(mR)}

def k9_build(NB=32):
    P = Prog()
    NKB = NB + 2
    QT = P.inp("QT", [2, 66, NB * 512], BF16)
    KT = P.inp("KT", [2, 66, NKB * 128], BF16)
    Vv = P.inp("Vv", [2, 128, NKB, 65], BF16)
    KTc = P.inp("KTc", [2, 66, 256], BF16); Vc = P.inp("Vc", [2, 128, 2, 65], BF16)
    mLd = P.inp("mL", [128, 512]); mRd = P.inp("mR", [128, 512])
    OT = P.out("OT", [2, 65, NB * 512])
    mL = P.sb("mL_s", [128, 512]); mR = P.sb("mR_s", [128, 512]); P.dma(mL, mLd); P.dma(mR, mRd)
    kT = P.sb("kT", [66, NKB * 128], BF16); vs = P.sb("vs", [128, NKB, 65], BF16)
    kTc = P.sb("kTc", [66, 256], BF16); vc = P.sb("vc", [128, 2, 65], BF16)
    qs = [P.sb(f"qs{i}", [66, 512], BF16) for i in range(2)]
    pTs = [P.sb(f"pT{i}", [128, 512], BF16) for i in range(4)]
    ef = [P.sb(f"ef{i}", [128, 512]) for i in range(2)]
    osb = [P.sb(f"osb{i}", [65, 512]) for i in range(2)]
    psS = [P.ps(f"psS{i}", [128, 512]) for i in range(4)]
    psO = [P.ps(f"psO{i}", [128, 512]) for i in range(2)]
    cnt = 0; blk = 0
    for kv in range(2):
        P.dma(kT, KT[kv]); P.dma(vs, Vv[kv]); P.dma(kTc, KTc[kv]); P.dma(vc, Vc[kv])
        for n in range(NB):
            q = qs[blk % 2]
            P.dma(q, QT[kv][:, n * 512:(n + 1) * 512])
            po = psO[blk % 2]
            tiles = [("c", 0), ("c", 1), ("L", n), ("C", n + 1), ("R", n + 2)]
            for ti, (kind, kb) in enumerate(tiles):
                s = psS[cnt % 4]; pt = pTs[cnt % 4]; cnt += 1
                if kind == "c":
                    P.mm(s, kTc[:, kb * 128:(kb + 1) * 128], q)
                else:
                    P.mm(s, kT[:, kb * 128:(kb + 1) * 128], q)
                if kind in ("L", "R"):
                    e = ef[ti % 2]
                    P.act(e, s, AF.Exp)
                    P.tt(pt, e, mL if kind == "L" else mR, ALU.mult)
                else:
                    P.act(pt, s, AF.Exp)
                vv = vc[:, kb, :] if kind == "c" else vs[:, kb, :]
                P.mm(po[0:65, :], vv, pt, start=(ti == 0), stop=(ti == len(tiles) - 1))
            o = osb[blk % 2]
            P.copy(o, po[0:65, :])
            P.dma(OT[kv][:, n * 512:(n + 1) * 512], o)
            blk += 1
    return P

import time, os, sys, numpy as np, ml_dtypes

bf = ml_dtypes.bfloat16
B, N, D, NCTX = 2, 16384, 1024, 256
T = 33
def bc(v): return np.ascontiguousarray(np.broadcast_to(np.asarray(v, np.float32), (128, len(v))))
def wl(w, k): return np.ascontiguousarray(w.reshape(k, 128, -1).transpose(1, 0, 2))
def pack(i, lat, ctx):
    b, q = i // 4, i % 4
    return np.ascontiguousarray(np.concatenate([ctx[b][(q % 2) * 128:(q % 2 + 1) * 128], lat[b][q * 4096:(q + 1) * 4096]], 0))
def unpack(outs, name):
    lat = np.stack([np.concatenate([outs[b * 4 + q][name][128:] for q in range(4)], 0) for b in range(B)])
    ctx = np.stack([np.concatenate([outs[b * 4 + q][name][:128] for q in range(2)], 0) for b in range(B)])
    return lat, ctx
def run(P, in_maps, tag=""):
    t = time.time(); nc = P.build(); tb = time.time() - t
    t = time.time(); res = run_bass_kernel_spmd(nc, in_maps, core_ids=list(range(8))); print(f"[{tag}] build {tb:.1f}s run {time.time()-t:.1f}s exec_ns={getattr(res, 'exec_time_ns', None)}", flush=True)
    return res.results
def rope_tabs_mla(tpos):
    n = len(tpos); C = np.ones((n, 32), np.float32); S = np.zeros((n, 32), np.float32)
    half = 8
    inv = (10000.0 ** (-np.arange(half, dtype=np.float32) / half)).astype(np.float32)
    for blk, pos in enumerate((tpos // 64, tpos % 64)):
        ang = pos.astype(np.float32)[:, None] * inv[None, :]
        c = np.cos(ang).astype(np.float32); s = np.sin(ang).astype(np.float32)
        C[:, blk*16:blk*16+8] = c; C[:, blk*16+8:blk*16+16] = c
        S[:, blk*16:blk*16+8] = -s; S[:, blk*16+8:blk*16+16] = s
    return C, S
def rope_tabs_ret(pos):
    half = 64
    inv = (10000.0 ** (-np.arange(half, dtype=np.float32) / half)).astype(np.float32)
    ang = pos.astype(np.float32)[:, None] * inv[None, :]
    return np.cos(ang).astype(np.float32), np.sin(ang).astype(np.float32)
IDB = np.eye(128, dtype=np.float32).astype(bf); IDF = np.eye(128, dtype=np.float32)

def stage_mod(I):
    pass

def modulation_dev(I):

    cv = np.concatenate([I['c'], I['c_ctx'][None]], 0)
    cT = np.ascontiguousarray(cv.reshape(3, 8, 128).transpose(2, 1, 0))
    ims = []
    for i in range(8):
        l = i // 4; cs = (i % 4) * 1536
        ims.append({"cT": cT, "w": wl(np.ascontiguousarray(I['ada_w'][l][:, cs:cs+1536]), 8), "b": np.ascontiguousarray(np.broadcast_to(I['ada_b'][l][cs:cs+1536], (3, 1536)))})
    r = run(build_mod(), ims, "mod")
    out = np.zeros((2, 3, 6144), np.float32)
    for i in range(8): out[i//4][:, (i%4)*1536:(i%4+1)*1536] = r[i]["o"]
    return out

def layer0_pre(I, mod, x_lat, x_ctx):

    ims = []
    for i in range(8):
        b, q = i // 4, i % 4
        tl = q * 4096 + np.arange(4096); tc = (q % 2) * 128 + np.arange(128)
        Cl, Sl = rope_tabs_mla(tl); Cc, Sc = np.ones((128, 32), np.float32), np.zeros((128, 32), np.float32)
        Crl, Srl = rope_tabs_ret(256 + tl); Crc, Src = rope_tabs_ret(tc)
        Cr = np.concatenate([Crc, Crl]); Sr = np.concatenate([Src, Srl]); ks = np.float32(128 ** -0.5)
        m = mod[0]
        ims.append({"x": pack(i, x_lat, x_ctx), "gB": bc(I['norm_mix_g'][0]),
                    "scB": np.stack([bc(m[b][1024:2048]), bc(m[2][1024:2048])]), "shB": np.stack([bc(m[b][0:1024]), bc(m[2][0:1024])]),
                    "w_in": wl(I['ev_w_in'][0], 8), "w_uq": wl(I['mla_w_uq'][0], 2), "w_ukv": wl(I['mla_w_ukv'][0], 1),
                    "gq": bc(I['mla_q_norm_g'][0]), "gkv": bc(I['mla_kv_norm_g'][0]),
                    "Cq": np.concatenate([Cc, Cl]), "Sq": np.concatenate([Sc, Sl]), "Cr": Cr, "Sr": Sr, "Crk": Cr * ks, "Srk": Sr * ks, "ident": IDB})
    r = run(k1_build(T), ims, "k1")
    return {n: unpack(r, n) for n in ("Qa", "Ka", "Va", "RQ", "RK", "RV", "RG")}

def layer0_mla(o1):

    Qa_l, Qa_c = o1["Qa"]; Ka_l, Ka_c = o1["Ka"]; Va_l, Va_c = o1["Va"]
    Ka = np.concatenate([Ka_c, Ka_l], 1).reshape(B, 16640, 8, 98); Va = np.concatenate([Va_c, Va_l], 1).reshape(B, 16640, 8, 65)
    Ql = Qa_l.reshape(B, N, 8, 98); Qc = Qa_c.reshape(B, NCTX, 8, 98)
    ims = []
    for h in range(8):
        ims.append({"QT": np.ascontiguousarray(Ql[:, :, h, :].transpose(0, 2, 1)), "QTc": np.ascontiguousarray(Qc[:, :, h, :].transpose(0, 2, 1)),
                    "KT": np.ascontiguousarray(Ka[:, :, h, :].transpose(0, 2, 1)),
                    "Vv": np.ascontiguousarray(Va[:, :, h, :].reshape(B, 130, 128, 65).transpose(0, 2, 1, 3))})
    r = run(k2_build(), ims, "k2")
    Ol = np.stack([r[h]["OT"].transpose(0, 2, 1) for h in range(8)], 2)
    Oc = np.stack([r[h]["OTc"].transpose(0, 2, 1) for h in range(8)], 2)
    return np.ascontiguousarray(Ol), np.ascontiguousarray(Oc)

def layer0_ret(o1):

    RQ = np.concatenate([o1["RQ"][1], o1["RQ"][0]], 1); RK = np.concatenate([o1["RK"][1], o1["RK"][0]], 1); RV = np.concatenate([o1["RV"][1], o1["RV"][0]], 1)
    ims = []
    for i in range(8):
        b, h = i // 4, i % 4
        sl = slice(h * 128, (h + 1) * 128)
        d = {"qT": np.ascontiguousarray(RQ[b][:, sl].T), "kT": np.ascontiguousarray(RK[b][:, sl].T),
             "kt": np.ascontiguousarray(RK[b][:, sl].reshape(130, 128, 128).transpose(1, 0, 2)),
             "vt": np.ascontiguousarray(RV[b][:, sl].reshape(130, 128, 128).transpose(1, 0, 2))}
        d.update(ret_consts(h)); ims.append(d)
    r = run(k3_build(), ims, "k3")
    O = np.zeros((B, 16640, 2, 512), np.float32)
    for i in range(8):
        b, h = i // 4, i % 4
        O[b][:, :, h * 128:(h + 1) * 128] = r[i]["o"].reshape(2, 16640, 128).transpose(1, 0, 2)
    return np.ascontiguousarray(O[:, 256:]), np.ascontiguousarray(O[:, :256])

def mid_common(I, mod, layer, b):
    m = mod[layer]
    return {"g1B": np.stack([bc(m[b][2048:3072]), bc(m[2][2048:3072])]), "gfB": bc(I['norm_ffn_g'][layer]),
            "sc2B": np.stack([bc(m[b][4096:5120]), bc(m[2][4096:5120])]), "sh2B": np.stack([bc(m[b][3072:4096]), bc(m[2][3072:4096])]),
            "w_r": wl(I['moe_router'][layer], 8), "ident": IDB, "identf": IDF}

def layer0_mid(I, mod, x_lat, x_ctx, o1, Oatt, Oret):

    ims = []
    for i in range(8):
        b = i // 4
        d = {"x": pack(i, x_lat, x_ctx), "Oatt": pack(i, Oatt[0], Oatt[1]), "Oret": pack(i, Oret[0], Oret[1]), "RG": pack(i, o1["RG"][0], o1["RG"][1]),
             "gn": bc(I['ret_norm_g'][0]), "w_out": wl(I['ev_w_out'][0], 8)}
        d.update(mid_common(I, mod, 0, b)); ims.append(d)
    r = run(k4_build(T, "even"), ims, "k4")
    return {n: unpack(r, n) for n in ("xmid", "h", "aff")}

def relerr(a, b): return float(np.sqrt(((a.astype(np.float64) - b) ** 2).mean() / (b.astype(np.float64) ** 2).mean()))

def moe_dev(I, layer, h_lat, h_ctx, aff_lat, aff_ctx, with_ctx=True):
    probs = ((128, 2048), (2, 32)) if with_ctx else ((128, 2048),)
    tri = np.triu(np.ones((128, 128), np.float32), 1)
    ims = []
    for i in range(8):
        b, eg = i // 4, i % 4
        es = slice(eg * 4, eg * 4 + 4)
        d = {"aff0": np.ascontiguousarray(aff_lat[b][:, es].reshape(128, 128, 4)), "h0": np.ascontiguousarray(h_lat[b].reshape(128, 128, 1024)),
             "wg": np.ascontiguousarray(I['moe_w_gate'][layer][es].reshape(4, 8, 128, 1536).transpose(0, 2, 1, 3)),
             "wu": np.ascontiguousarray(I['moe_w_up'][layer][es].reshape(4, 8, 128, 1536).transpose(0, 2, 1, 3)),
             "wd": np.ascontiguousarray(I['moe_w_down'][layer][es].reshape(4, 12, 128, 1024).transpose(0, 2, 1, 3)),
             "tri": tri, "ones": np.ones((128, 128), np.float32), "ident": IDB}
        if with_ctx:
            d["aff1"] = np.ascontiguousarray(aff_ctx[b][:, es].reshape(128, 2, 4)); d["h1"] = np.ascontiguousarray(h_ctx[b].reshape(128, 2, 1024))
        ims.append(d)
    r = run(k5_build(probs), ims, "k5")
    out = {}
    for pi, (J, cap) in enumerate(probs):
        n = 128 * J
        out[pi] = {"ys": [np.concatenate([r[b * 4 + eg][f"ys{pi}"] for eg in range(4)], 0) for b in range(B)],
                   "dest": np.stack([np.concatenate([r[b * 4 + eg][f"dest{pi}"].reshape(n, 4) for eg in range(4)], 1) for b in range(B)]),
                   "gsel": np.stack([np.concatenate([r[b * 4 + eg][f"gsel{pi}"].reshape(n, 4) for eg in range(4)], 1) for b in range(B)])}
    return out

def combine_dev(I, mod, layer, xmid_lat, xmid_ctx, mo, final=False):

    with_ctx = 1 in mo
    ims = []
    for i in range(8):
        b = i // 4
        m = mod[layer]
        if with_ctx:
            d = {"xmid": pack(i, xmid_lat, xmid_ctx), "gsel": pack(i, mo[0]["gsel"], mo[1]["gsel"]), "dest": pack(i, mo[0]["dest"], mo[1]["dest"]),
                 }
            for e in range(16):
                d[f"ys0_{e}"] = np.ascontiguousarray(mo[0]["ys"][b][e]); d[f"ys1_{e}"] = np.ascontiguousarray(mo[1]["ys"][b][e])
        else:
            q = i % 4; sl = slice(q * 4096, (q + 1) * 4096)
            d = {"xmid": np.ascontiguousarray(xmid_lat[b][sl]), "gsel": np.ascontiguousarray(mo[0]["gsel"][b][sl]), "dest": np.ascontiguousarray(mo[0]["dest"][b][sl])}
            for e in range(16):
                d[f"ys0_{e}"] = np.ascontiguousarray(mo[0]["ys"][b][e])
        d["g2B"] = np.stack([bc(m[b][5120:6144]), bc(m[2][5120:6144])])
        if final: d["fgB"] = bc(I['final_g'])
        ims.append(d)
    Tn = T if with_ctx else 32
    r = run(k6_build(Tn, (2048, 32) if with_ctx else (2048,), final=final), ims, "k6")
    if with_ctx:
        return unpack(r, "xo")
    xo = np.stack([np.concatenate([r[b * 4 + q]["xo"] for q in range(4)], 0) for b in range(B)])
    fo = np.stack([np.concatenate([r[b * 4 + q]["fo"] for q in range(4)], 0) for b in range(B)]) if final else None
    return xo, fo

def rope_tabs_win(tpos, is_ctx):
    n = len(tpos); C = np.ones((n, 64), np.float32); S = np.zeros((n, 64), np.float32)
    if not is_ctx:
        half = 16
        inv = (10000.0 ** (-np.arange(half, dtype=np.float32) / half)).astype(np.float32)
        for blk, pos in enumerate((tpos // 64, tpos % 64)):
            ang = pos.astype(np.float32)[:, None] * inv[None, :]
            c = np.cos(ang).astype(np.float32); s = np.sin(ang).astype(np.float32)
            C[:, blk*32:blk*32+16] = c; C[:, blk*32+16:blk*32+32] = c
            S[:, blk*32:blk*32+16] = -s; S[:, blk*32+16:blk*32+32] = s
    return C, S

def shifted_streams(xs):
    z = np.zeros_like(xs[:, :1])
    xp = np.concatenate([z, xs[:, :-1]], 1); xn = np.concatenate([xs[:, 1:], z], 1)
    fl = np.ones(xs.shape[:2] + (2,), np.float32); fl[:, 0, 0] = 0; fl[:, -1, 1] = 0
    return xp, xn, fl

def layer1_pre(I, mod, x_lat, x_ctx):

    xpl, xnl, fll = shifted_streams(x_lat); xpc, xnc, flc = shifted_streams(x_ctx)
    m = mod[1]; o = 0
    ims = []
    for i in range(8):
        b, q = i // 4, i % 4
        Cl, Sl = rope_tabs_win(q * 4096 + np.arange(4096), False); Cc, Sc = rope_tabs_win(np.arange(128), True)
        ims.append({"x": pack(i, x_lat, x_ctx), "xp": pack(i, xpl, xpc), "xn": pack(i, xnl, xnc), "fl": pack(i, fll, flc), "gB": bc(I['norm_mix_g'][1]),
            "scB": np.stack([bc(m[b][1024:2048]), bc(m[2][1024:2048])]), "shB": np.stack([bc(m[b][0:1024]), bc(m[2][0:1024])]),
            "w_in": wl(I['od_w_in'][o], 8), "muB": bc(I['rwkv_mu'][o]), "kkB": bc(I['rwkv_k_k'][o]), "kaB": bc(I['rwkv_k_a'][o]), "rkB": bc(I['rwkv_r_k'][o].reshape(-1)),
            "w0B": np.stack([bc(I['rwkv_w0'][o][d]) for d in range(2)]), "a0B": np.stack([bc(I['rwkv_a0'][o][d]) for d in range(2)]),
            "w2s": np.ascontiguousarray(I['rwkv_w2'][o].reshape(128, 1, 512)), "a2s": np.ascontiguousarray(I['rwkv_a2'][o].reshape(128, 1, 512)),
            "g2s": np.ascontiguousarray(I['rwkv_g2'][o].reshape(128, 1, 512)), "Cw": np.concatenate([Cc, Cl]), "Sw": np.concatenate([Sc, Sl]), "ident": IDB})
    r = run(k7_build(T), ims, "k7")
    names = ["R", "Vv", "KK", "LWf", "LWb", "Af", "Ab", "Kf", "Kb", "G", "BG", "NQ", "Qw", "Kw", "Vw"]
    return {n: unpack(r, n) for n in names}

def layer1_rwkv(o7):

    cst = k8_consts()
    ims = []
    def seq(name, b, dirn, ch):
        lat, ctx = o7[name]
        a = np.concatenate([ctx[b][::-1] if dirn else ctx[b], lat[b][::-1] if dirn else lat[b]], 0)[:, ch]
        return np.ascontiguousarray(a.reshape(260, 64, 256))
    for i in range(8):
        b, dirn, hg = i // 4, (i % 4) // 2, i % 2
        ch = slice(hg * 256, (hg + 1) * 256)
        d = {"R": seq("R", b, dirn, ch), "K": seq("Kb" if dirn else "Kf", b, dirn, ch), "Vv": seq("Vv", b, dirn, ch), "KK": seq("KK", b, dirn, ch),
             "A": seq("Ab" if dirn else "Af", b, dirn, ch), "LW": seq("LWb" if dirn else "LWf", b, dirn, ch)}
        d.update(cst); ims.append(d)
    r = run(k8_build(260, 4), ims, "k8")
    Y = np.zeros((B, N, 2, 512), np.float32)
    for i in range(8):
        b, dirn, hg = i // 4, (i % 4) // 2, i % 2
        y = r[i]["y"].reshape(N, 256)
        Y[b][:, dirn, hg * 256:(hg + 1) * 256] = y[::-1] if dirn else y
    return Y

def layer1_win(o7):

    Ql = o7["Qw"][0].reshape(B, N, 2, 4, 66)
    Kl = o7["Kw"][0].reshape(B, N, 2, 66); Vl = o7["Vw"][0].reshape(B, N, 2, 65)
    Kc = o7["Kw"][1].reshape(B, NCTX, 2, 66); Vc = o7["Vw"][1].reshape(B, NCTX, 2, 65)
    padK = np.zeros((128, 2, 66), bf); padK[:, :, 64] = 1.0; padK[:, :, 65] = -10000.0
    padV = np.zeros((128, 2, 65), bf)
    cst = k9_consts(); ims = []
    for i in range(8):
        b, q = i // 4, i % 4
        t0 = q * 4096
        lo = Kl[b][t0 - 128:t0] if q > 0 else padK; hi = Kl[b][t0 + 4096:t0 + 4224] if q < 3 else padK
        Kh = np.concatenate([lo, Kl[b][t0:t0 + 4096], hi], 0)
        lo = Vl[b][t0 - 128:t0] if q > 0 else padV; hi = Vl[b][t0 + 4096:t0 + 4224] if q < 3 else padV
        Vh = np.concatenate([lo, Vl[b][t0:t0 + 4096], hi], 0)
        Qb = Ql[b][t0:t0 + 4096].reshape(32, 128, 2, 4, 66)
        d = {"QT": np.ascontiguousarray(Qb.transpose(2, 4, 0, 3, 1).reshape(2, 66, 32 * 512)),
             "KT": np.ascontiguousarray(Kh.transpose(1, 2, 0)), "Vv": np.ascontiguousarray(Vh.reshape(34, 128, 2, 65).transpose(2, 1, 0, 3)),
             "KTc": np.ascontiguousarray(Kc[b].transpose(1, 2, 0)), "Vc": np.ascontiguousarray(Vc[b].reshape(2, 128, 2, 65).transpose(2, 1, 0, 3))}
        d.update(cst); ims.append(d)
    r = run(k9_build(32), ims, "k9")
    O = np.zeros((B, N, 8, 65), np.float32)
    for i in range(8):
        b, q = i // 4, i % 4
        ot = r[i]["OT"].reshape(2, 65, 32, 4, 128)
        O[b][q * 4096:(q + 1) * 4096] = ot.transpose(2, 4, 0, 3, 1).reshape(4096, 8, 65)
    return O

def layer1_mid(I, mod, x_lat, o7, Y, Ow):

    ims = []
    for i in range(8):
        b, q = i // 4, i % 4; sl = slice(q * 4096, (q + 1) * 4096)
        c = lambda a: np.ascontiguousarray(a[b][sl])
        d = {"x": c(x_lat), "Yr": c(Y), "G": c(o7["G"][0]), "BG": c(o7["BG"][0]), "Ow": c(Ow), "NQ": c(o7["NQ"][0]),
             "sinkB": bc(I['win_sink'][0]), "lngB": bc(I['rwkv_ln_g'][0]), "lnbB": bc(I['rwkv_ln_b'][0]), "w_out": wl(I['od_w_out'][0], 8)}
        d.update(mid_common(I, mod, 1, b)); ims.append(d)
    r = run(k4_build(32, "odd", has_ctx=False), ims, "k10")
    cat = lambda n: np.stack([np.concatenate([r[b * 4 + q][n] for q in range(4)], 0) for b in range(B)])
    return {n: cat(n) for n in ("xmid", "h", "aff")}


def kernel(**inputs):
    I = {k: np.asarray(v) for k, v in inputs.items()}
    mod = modulation_dev(I)
    x_lat, x_ctx = I['x'], I['ctx']
    o1 = layer0_pre(I, mod, x_lat, x_ctx)
    Oret = layer0_ret(o1)
    Oatt = layer0_mla(o1)
    o4 = layer0_mid(I, mod, x_lat, x_ctx, o1, Oatt, Oret)
    del o1, Oret, Oatt
    mo = moe_dev(I, 0, o4["h"][0], o4["h"][1], o4["aff"][0], o4["aff"][1], True)
    x_lat, x_ctx = combine_dev(I, mod, 0, o4["xmid"][0], o4["xmid"][1], mo)
    del o4, mo
    o7 = layer1_pre(I, mod, x_lat, x_ctx)
    Y = layer1_rwkv(o7)
    Ow = layer1_win(o7)
    o10 = layer1_mid(I, mod, x_lat, o7, Y, Ow)
    del o7, Y, Ow
    mo = moe_dev(I, 1, o10["h"], None, o10["aff"], None, False)
    xo2, fo = combine_dev(I, mod, 1, o10["xmid"], None, mo, final=True)
    return np.ascontiguousarray(fo.astype(np.float32))
```

```python
import numpy as np

from contextlib import ExitStack
import concourse.bass as bass
import concourse.mybir as mybir
from concourse.bass_utils import run_bass_kernel_spmd

F32 = mybir.dt.float32
BF16 = mybir.dt.bfloat16
I32 = mybir.dt.int32
U32 = mybir.dt.uint32
AF = mybir.ActivationFunctionType
ALU = mybir.AluOpType
AX = mybir.AxisListType


class V:
    __slots__ = ("key", "ap")

    def __init__(self, key, ap):
        self.key = key
        self.ap = ap

    def __getitem__(self, idx):
        return V(self.key, self.ap[idx])

    def sub(self, subkey, idx=None):
        return V((self.key, subkey), self.ap if idx is None else self.ap[idx])

    def re(self, pattern, **kw):
        return V(self.key, self.ap.rearrange(pattern, **kw))


class _Op:
    __slots__ = ("eng", "fn", "kind", "deps", "sig", "dsem", "dval", "idx", "selfwait")


class Prog:
    ENGS = ("pe", "act", "dve", "pool", "sp")
    NDMA = 8

    def __init__(self, name="k"):
        self.nc = bass.Bass("TRN2", target_bir_lowering=False)
        self.es = ExitStack()
        self.ops = {e: [] for e in self.ENGS}
        self.state = {}
        self.ndma = {e: 0 for e in self.ENGS}
        self.uid = 0
        self.outs = []

    def dram(self, name, shape, dt, kind):
        t = self.nc.dram_tensor(name, list(shape), dt, kind=kind)
        return V(("d", name), t.ap())

    def inp(self, name, shape, dt=F32):
        return self.dram(name, shape, dt, "ExternalInput")

    def out(self, name, shape, dt=F32):
        v = self.dram(name, shape, dt, "ExternalOutput")
        self.outs.append(name)
        return v

    def scratch(self, name, shape, dt=F32):
        return self.dram(name, shape, dt, "Internal")

    def sb(self, name, shape, dt=F32):
        t = self.es.enter_context(self.nc.sbuf_tensor(name, list(shape), dt))
        return V(("s", name), t.ap() if hasattr(t, "ap") and callable(t.ap) else t[:])

    def ps(self, name, shape, dt=F32):
        t = self.es.enter_context(self.nc.psum_tensor(name, list(shape), dt))
        return V(("p", name), t.ap() if hasattr(t, "ap") and callable(t.ap) else t[:])

    def op(self, eng, fn, reads=(), writes=(), kind="c"):
        o = _Op()
        o.eng, o.fn, o.kind = eng, fn, kind
        o.sig = None
        o.idx = len(self.ops[eng])
        deps = set()
        for v in reads:
            st = self.state.get(v.key)
            if st is not None and st[0] is not None:
                deps.add(st[0])
        for v in writes:
            st = self.state.get(v.key)
            if st is not None:
                if st[0] is not None:
                    deps.add(st[0])
                for r in st[1].values():
                    deps.add(r)
        o.deps = [d for d in deps if not (d.eng == eng and d.kind == "c" and kind == "c" and eng == "pe")]
        if kind == "d":
            j = self.ndma[eng]
            self.ndma[eng] += 1
            o.dsem = j % self.NDMA
            o.dval = 16 * (j // self.NDMA + 1)
        self.ops[eng].append(o)
        for v in reads:
            st = self.state.setdefault(v.key, [None, {}])
            if kind == "d":
                st[1][(eng, kind, o.idx)] = o
            else:
                st[1][(eng, kind)] = o
        for v in writes:
            self.state[v.key] = [o, {}]
        return o

    def dma(self, out, in_, eng="sp", **kw):
        return self.op(eng, lambda e: e.dma_start(out=out.ap, in_=in_.ap, **kw), [in_], [out], kind="d")

    def mm(self, out, lhsT, rhs, start=True, stop=True, extra_reads=()):
        return self.op("pe", lambda e: e.matmul(out.ap, lhsT.ap, rhs.ap, start=start, stop=stop),
                       [lhsT, rhs] + ([] if start else [out]) + list(extra_reads), [out])

    def tr(self, out, in_, ident):
        return self.op("pe", lambda e: e.transpose(out.ap, in_.ap, ident.ap), [in_, ident], [out])

    def act(self, out, in_, func, bias=None, scale=None, accum=None, eng="act"):
        kw = {}
        rd = [in_]
        wr = [out]
        if bias is not None:
            if isinstance(bias, V):
                kw["bias"] = bias.ap
                rd.append(bias)
            else:
                kw["bias"] = bias
        if scale is not None:
            if isinstance(scale, V):
                kw["scale"] = scale.ap
                rd.append(scale)
            else:
                kw["scale"] = scale
        if accum is not None:
            kw["accum_out"] = accum.ap
            wr.append(accum)
        return self.op(eng, lambda e: e.activation(out.ap, in_.ap, func, **kw), rd, wr)

    def tt(self, out, a, b, op, eng="dve"):
        return self.op(eng, lambda e: e.tensor_tensor(out=out.ap, in0=a.ap, in1=b.ap, op=op), [a, b], [out])

    def ts(self, out, a, s1, op0, s2=None, op1=None, accum=None, eng="dve"):
        rd = [a]
        wr = [out]
        kw = {}
        a1 = s1
        a2 = s2
        if isinstance(s1, V):
            rd.append(s1)
            a1 = s1.ap
        if isinstance(s2, V):
            rd.append(s2)
            a2 = s2.ap
        if op1 is not None:
            kw["op1"] = op1
        if accum is not None:
            kw["accum_out"] = accum.ap
            wr.append(accum)
        return self.op(eng, lambda e: e.tensor_scalar(out=out.ap, in0=a.ap, scalar1=a1, scalar2=a2, op0=op0, **kw), rd, wr)

    def stt(self, out, a, s, b, op0, op1, accum=None, eng="dve"):
        rd = [a, b]
        wr = [out]
        sa = s
        kw = {}
        if isinstance(s, V):
            rd.append(s)
            sa = s.ap
        if accum is not None:
            kw["accum_out"] = accum.ap
            wr.append(accum)
        return self.op(eng, lambda e: e.scalar_tensor_tensor(out=out.ap, in0=a.ap, scalar=sa, in1=b.ap, op0=op0, op1=op1, **kw), rd, wr)

    def copy(self, out, in_, eng="dve"):
        if eng == "act":
            return self.op("act", lambda e: e.copy(out.ap, in_.ap), [in_], [out])
        return self.op(eng, lambda e: e.tensor_copy(out=out.ap, in_=in_.ap), [in_], [out])

    def memset(self, out, val, eng="dve"):
        return self.op(eng, lambda e: e.memset(out.ap, val), [], [out])

    def recip(self, out, in_, eng="dve"):
        return self.op(eng, lambda e: e.reciprocal(out=out.ap, in_=in_.ap), [in_], [out])

    def reduce(self, out, in_, op, axis=AX.X, eng="dve"):
        return self.op(eng, lambda e: e.tensor_reduce(out=out.ap, in_=in_.ap, axis=axis, op=op), [in_], [out])

    def scan(self, out, d0, d1, init, op0, op1):
        rd = [d0, d1]
        ia = init
        if isinstance(init, V):
            rd.append(init)
            ia = init.ap
        return self.op("dve", lambda e: e.tensor_tensor_scan(out=out.ap, data0=d0.ap, data1=d1.ap, initial=ia, op0=op0, op1=op1), rd, [out])

    def bcreg(self, g, val):
        d = self.__dict__.setdefault("_bcregs", {})
        if val not in d:
            d[val] = g.alloc_register(f"bc{val}")
            g.reg_mov(d[val], val)
        return d[val]

    def build(self):
        nc = self.nc
        cnt = {e: 0 for e in self.ENGS}
        for e in self.ENGS:
            for o in self.ops[e]:
                for d in o.deps:
                    if d.kind == "c":
                        d.sig = True
        for e in self.ENGS:
            for o in self.ops[e]:
                if o.kind == "c" and o.sig:
                    cnt[e] += 1
                    o.sig = cnt[e]
        sems = {}
        for e in self.ENGS:
            sems[("c", e)] = self.es.enter_context(nc.semaphore(f"c_{e}"))
            if self.ndma[e]:
                for i in range(min(self.NDMA, self.ndma[e])):
                    sems[("d", e, i)] = self.es.enter_context(nc.semaphore(f"d_{e}_{i}"))
        final = {e: {} for e in self.ENGS}

        def emit(eng_name, eng):
            waited = {}
            for o in self.ops[eng_name]:
                ws = {}
                for d in o.deps:
                    if d.kind == "c":
                        k, v = ("c", d.eng), d.sig
                    else:
                        k, v = ("d", d.eng, d.dsem), d.dval
                    if ws.get(k, 0) < v:
                        ws[k] = v
                if o.kind == "d" and o.dval > 16:
                    k = ("d", eng_name, o.dsem)
                    if ws.get(k, 0) < o.dval - 16:
                        ws[k] = o.dval - 16
                for k, v in ws.items():
                    if waited.get(k, 0) < v:
                        eng.wait_ge(sems[k], v)
                        waited[k] = v
                inst = o.fn(eng)
                if o.kind == "d":
                    inst.then_inc(sems[("d", eng_name, o.dsem)], 16)
                    final[eng_name][("d", eng_name, o.dsem)] = o.dval
                elif o.sig:
                    inst.then_inc(sems[("c", eng_name)], 1)
            for k, v in final[eng_name].items():
                if waited.get(k, 0) < v:
                    eng.wait_ge(sems[k], v)

        with nc.Block() as block:
            if self.ops["sp"]:
                @block.sync
                def _(e):
                    emit("sp", e)
            if self.ops["pe"]:
                @block.tensor
                def _(e):
                    emit("pe", e)
            if self.ops["act"]:
                @block.scalar
                def _(e):
                    emit("act", e)
            if self.ops["dve"]:
                @block.vector
                def _(e):
                    emit("dve", e)
            if self.ops["pool"]:
                @block.gpsimd
                def _(e):
                    emit("pool", e)
        self.es.close()
        return nc

    def run(self, in_maps, n=8, trace=False):
        nc = self.build()
        res = run_bass_kernel_spmd(nc, in_maps, core_ids=list(range(n)), trace=trace)
        return res


def build_mod():
    P = Prog()
    cT = P.inp("cT", [128, 8, 3]); w = P.inp("w", [128, 8, 1536]); b = P.inp("b", [3, 1536]); o = P.out("o", [3, 1536])
    cs = P.sb("cs", [128, 8, 3]); ws = P.sb("ws", [128, 8, 1536]); bs = P.sb("bs", [3, 1536]); os_ = P.sb("os", [3, 1536]); sg = P.sb("sg", [128, 8, 3])
    P.dma(cs, cT); P.dma(ws, w); P.dma(bs, b)
    P.act(sg, cs, AF.Sigmoid)
    P.tt(cs, cs, sg, ALU.mult)
    pp = [P.ps(f"pp{i}", [128, 512]) for i in range(3)]
    for j in range(3):
        for k in range(8):
            P.mm(pp[j][0:3, :], cs[:, k, :], ws[:, k, j*512:(j+1)*512], start=(k==0), stop=(k==7))
        P.tt(os_[:, j*512:(j+1)*512], pp[j][0:3, :], bs[:, j*512:(j+1)*512], ALU.add)
    P.dma(o, os_)
    return P


EPS = 1e-6
SQ = 96 ** -0.25
IN_CH = [(0, 512), (512, 1024), (1024, 1536), (1536, 2048), (2048, 2464)]


def load_w_bf16(P, dst, src, kchunks, ncols, stage):
    for k in range(kchunks):
        P.dma(stage[:, :ncols], src[:, k, :])
        P.copy(dst[:, k, :], stage[:, :ncols], eng=("act" if k % 2 else "dve"))


def rms_rstd(P, rs, junk, xin, n, eps=EPS):
    P.tt(junk, xin, xin, ALU.mult)
    P.reduce(rs, junk, ALU.add)
    P.ts(rs, rs, 1.0 / n, ALU.mult, eps, ALU.add)
    P.act(rs, rs, AF.Sqrt)
    P.recip(rs, rs)


def k1_build(T):
    P = Prog()
    x = P.inp("x", [T * 128, 1024])
    gB = P.inp("gB", [128, 1024])
    scB = P.inp("scB", [2, 128, 1024])
    shB = P.inp("shB", [2, 128, 1024])
    w_in = P.inp("w_in", [128, 8, 2464])
    w_uq = P.inp("w_uq", [128, 2, 768])
    w_ukv = P.inp("w_ukv", [128, 1, 1024])
    gq = P.inp("gq", [128, 256])
    gkv = P.inp("gkv", [128, 128])
    Cq = P.inp("Cq", [T * 128, 32])
    Sq_ = P.inp("Sq", [T * 128, 32])
    Cr = P.inp("Cr", [T * 128, 64])
    Sr = P.inp("Sr", [T * 128, 64])
    Crk = P.inp("Crk", [T * 128, 64])
    Srk = P.inp("Srk", [T * 128, 64])
    identd = P.inp("ident", [128, 128], BF16)
    Qa_o = P.out("Qa", [T * 128, 8 * 98], BF16)
    Ka_o = P.out("Ka", [T * 128, 8 * 98], BF16)
    Va_o = P.out("Va", [T * 128, 8 * 65], BF16)
    RQ_o = P.out("RQ", [T * 128, 512], BF16)
    RK_o = P.out("RK", [T * 128, 512], BF16)
    RV_o = P.out("RV", [T * 128, 512], BF16)
    RG_o = P.out("RG", [T * 128, 512], F32)

    ident = P.sb("ident_s", [128, 128], BF16)
    P.dma(ident, identd)
    stage = P.sb("stage", [128, 2464])
    w_in_b = P.sb("w_in_b", [128, 8, 2464], BF16)
    w_uq_b = P.sb("w_uq_b", [128, 2, 768], BF16)
    w_ukv_b = P.sb("w_ukv_b", [128, 1, 1024], BF16)
    load_w_bf16(P, w_in_b, w_in, 8, 2464, stage)
    load_w_bf16(P, w_uq_b, w_uq, 2, 768, stage)
    load_w_bf16(P, w_ukv_b, w_ukv, 1, 1024, stage)
    gqs = P.sb("gqs", [128, 256])
    gkvs = P.sb("gkvs", [128, 128])
    P.dma(gqs, gq)
    P.dma(gkvs, gkv)
    gs = P.sb("gs", [128, 1024])
    P.dma(gs, gB)
    Gp = P.sb("Gp", [128, 2, 1024])
    SH = P.sb("SHt", [128, 2, 1024])
    for c in range(2):
        P.dma(stage[:, :1024], scB[c])
        P.stt(Gp[:, c, :], stage[:, :1024], 1.0, gs, ALU.add, ALU.mult)
        P.dma(SH[:, c, :], shB[c])

    xs = [P.sb(f"xs{i}", [128, 1024]) for i in range(2)]
    junk = P.sb("junk", [128, 1024])
    tmp = P.sb("tmp", [128, 1024])
    a_bf = P.sb("a_bf", [128, 1024], BF16)
    aT = P.sb("aT", [128, 1024], BF16)
    Pm = P.sb("Pm", [128, 2464])
    rs = P.sb("rs", [128, 1])
    rs2 = P.sb("rs2", [128, 1])
    rs3 = P.sb("rs3", [128, 1])
    cqn = P.sb("cqn", [128, 256], BF16)
    cqnT = P.sb("cqnT", [128, 256], BF16)
    ckvn = P.sb("ckvn", [128, 128], BF16)
    ckvnT = P.sb("ckvnT", [128, 128], BF16)
    qf = P.sb("qf", [128, 768])
    kvf = P.sb("kvf", [128, 1024])
    t1 = P.sb("t1", [128, 512])
    t2 = P.sb("t2", [128, 512])
    krr = P.sb("krr", [128, 32])
    nq = P.sb("nq", [128, 8])
    nk = P.sb("nk", [128, 8])
    ek = P.sb("ek", [128, 8])
    krs = P.sb("krs", [128, 1])
    tabs = [[P.sb(f"tab{i}_{j}", [128, 64]) for j in range(6)] for i in range(2)]
    Qa = [P.sb(f"Qa{i}", [128, 8, 98], BF16) for i in range(2)]
    Ka = [P.sb(f"Ka{i}", [128, 8, 98], BF16) for i in range(2)]
    Va = [P.sb(f"Va{i}", [128, 8, 65], BF16) for i in range(2)]
    RQ = [P.sb(f"RQ{i}", [128, 512], BF16) for i in range(2)]
    RK = [P.sb(f"RK{i}", [128, 512], BF16) for i in range(2)]
    RV = [P.sb(f"RV{i}", [128, 512], BF16) for i in range(2)]
    RG = [P.sb(f"RG{i}", [128, 512], F32) for i in range(2)]
    pT = P.ps("pT", [128, 1024], BF16)
    pp = [P.ps(f"pp{i}", [128, 512]) for i in range(6)]

    for t in range(T):
        cls = 1 if t == 0 else 0
        i2 = t % 2
        rows = slice(t * 128, (t + 1) * 128)
        xt = xs[i2]
        P.dma(xt, x[rows, :])
        tb = tabs[i2]
        P.dma(tb[0][:, :32], Cq[rows, :])
        P.dma(tb[1][:, :32], Sq_[rows, :])
        P.dma(tb[2], Cr[rows, :])
        P.dma(tb[3], Sr[rows, :])
        P.dma(tb[4], Crk[rows, :])
        P.dma(tb[5], Srk[rows, :])
        rms_rstd(P, rs, junk, xt, 1024)
        P.stt(tmp, xt, rs, Gp[:, cls, :], ALU.mult, ALU.mult)
        P.tt(a_bf, tmp, SH[:, cls, :], ALU.add)
        for k in range(8):
            P.tr(pT[:, k * 128:(k + 1) * 128], a_bf[:, k * 128:(k + 1) * 128], ident)
        P.copy(aT, pT)
        for j, (c0, c1) in enumerate(IN_CH):
            for k in range(8):
                P.mm(pp[j][:, :c1 - c0], aT[:, k * 128:(k + 1) * 128], w_in_b[:, k, c0:c1], start=(k == 0), stop=(k == 7))
            P.copy(Pm[:, c0:c1], pp[j][:, :c1 - c0], eng=("act" if j % 2 else "dve"))
        rms_rstd(P, rs2, junk[:, :256], Pm[:, 0:256], 256)
        P.stt(cqn, Pm[:, 0:256], rs2, gqs, ALU.mult, ALU.mult)
        for k in range(2):
            P.tr(pT[:, k * 128:(k + 1) * 128], cqn[:, k * 128:(k + 1) * 128], ident)
        P.copy(cqnT, pT[:, :256])
        for j, (c0, c1) in enumerate([(0, 512), (512, 768)]):
            for k in range(2):
                P.mm(pp[j][:, :c1 - c0], cqnT[:, k * 128:(k + 1) * 128], w_uq_b[:, k, c0:c1], start=(k == 0), stop=(k == 1))
            P.act(qf[:, c0:c1], pp[j][:, :c1 - c0], AF.Copy, scale=SQ)
        qv = qf.re("p (h d) -> p h d", d=96)
        Qt = Qa[i2]
        P.tt(junk[:, :768], qf, qf, ALU.mult)
        P.reduce(nq, junk[:, :768].re("p (h d) -> p h d", d=96), ALU.add)
        P.ts(Qt[:, :, 96], nq, -0.5, ALU.mult)
        P.memset(Qt[:, :, 97], 1.0)
        P.copy(Qt[:, :, 0:64], qv[:, :, 0:64], eng="act")
        Cb = V(tb[0].key, tb[0].ap[:, :32].unsqueeze(1).to_broadcast([128, 8, 32]))
        t1v = t1[:, :256].re("p (h d) -> p h d", d=32)
        t2v = t2[:, :256].re("p (h d) -> p h d", d=32)
        P.tt(t1v, qv[:, :, 64:96], Cb, ALU.mult)
        q5 = qv[:, :, 64:96].re("p h (b f e) -> p h b f e", b=2, f=2)
        t25 = t2v.re("p h (b f e) -> p h b f e", b=2, f=2)
        S5 = tb[1].ap[:, :32].rearrange("p (b f e) -> p b f e", b=2, f=2)
        for f in range(2):
            Sb = V(tb[1].key, S5[:, :, f, :].unsqueeze(1).to_broadcast([128, 8, 2, 8]))
            P.tt(t25[:, :, :, f, :], q5[:, :, :, 1 - f, :], Sb, ALU.mult)
        P.tt(Qt[:, :, 64:96], t1v, t2v, ALU.add)
        P.dma(Qa_o[rows, :], Qt.re("p h d -> p (h d)"))
        rms_rstd(P, rs3, junk[:, :128], Pm[:, 256:384], 128)
        P.stt(ckvn, Pm[:, 256:384], rs3, gkvs, ALU.mult, ALU.mult)
        P.tr(pT[:, 0:128], ckvn, ident)
        P.copy(ckvnT, pT[:, :128])
        for j in range(2):
            P.mm(pp[2 + j], ckvnT, w_ukv_b[:, 0, j * 512:(j + 1) * 512], start=True, stop=True)
            P.copy(kvf[:, j * 512:(j + 1) * 512], pp[2 + j], eng=("act" if j % 2 else "dve"))
        kvv = kvf.re("p (h d) -> p h d", d=128)
        Kt = Ka[i2]
        Vt = Va[i2]
        kr = Pm[:, 384:416]
        P.tt(t1[:, :32], kr, tb[0][:, :32], ALU.mult)
        kr4 = kr.re("p (b f e) -> p b f e", b=2, f=2)
        t24 = t2[:, :32].re("p (b f e) -> p b f e", b=2, f=2)
        S4 = V(tb[1].key, S5)
        for f in range(2):
            P.tt(t24[:, :, f, :], kr4[:, :, 1 - f, :], S4[:, :, f, :], ALU.mult)
        P.tt(t1[:, :32], t1[:, :32], t2[:, :32], ALU.add)
        P.ts(krr, t1[:, :32], SQ, ALU.mult)
        P.act(Kt[:, :, 0:64], kvv[:, :, 0:64], AF.Copy, scale=SQ)
        P.copy(Kt[:, :, 64:96], V(krr.key, krr.ap.unsqueeze(1).to_broadcast([128, 8, 32])))
        P.memset(Kt[:, :, 96], 1.0)
        kn2 = junk[:, :512].re("p (h d) -> p h d", d=64)
        P.tt(kn2, kvv[:, :, 0:64], kvv[:, :, 0:64], ALU.mult)
        P.reduce(nk, kn2, ALU.add)
        P.tt(junk[:, 512:544], krr, krr, ALU.mult)
        P.reduce(krs, junk[:, 512:544], ALU.add)
        P.ts(nk, nk, SQ * SQ, ALU.mult, krs, ALU.add)
        P.ts(Kt[:, :, 97], nk, -0.5, ALU.mult)
        P.act(ek, Kt[:, :, 97], AF.Exp, scale=-1.0)
        P.tt(Vt[:, :, 0:64], kvv[:, :, 64:128], V(ek.key, ek.ap.unsqueeze(2).to_broadcast([128, 8, 64])), ALU.mult)
        P.copy(Vt[:, :, 64], ek)
        P.dma(Ka_o[rows, :], Kt.re("p h d -> p (h d)"))
        P.dma(Va_o[rows, :], Vt.re("p h d -> p (h d)"))
        for (c0, ci, si, dst, dst_o) in ((416, 2, 3, RQ[i2], RQ_o), (928, 4, 5, RK[i2], RK_o)):
            xv = Pm[:, c0:c0 + 512].re("p (h f e) -> p h f e", h=4, f=2)
            t1r = t1.re("p (h f e) -> p h f e", h=4, f=2)
            t2r = t2.re("p (h f e) -> p h f e", h=4, f=2)
            Cb2 = V(tb[ci].key, tb[ci].ap.unsqueeze(1).unsqueeze(1).to_broadcast([128, 4, 2, 64]))
            P.tt(t1r, xv, Cb2, ALU.mult)
            Sb2 = V(tb[si].key, tb[si].ap.unsqueeze(1).to_broadcast([128, 4, 64]))
            P.tt(t2r[:, :, 1, :], xv[:, :, 0, :], Sb2, ALU.mult)
            P.stt(t2r[:, :, 0, :], xv[:, :, 1, :], -1.0, Sb2, ALU.mult, ALU.mult)
            P.tt(dst, t1, t2, ALU.add)
            P.dma(dst_o[rows, :], dst)
        P.copy(RV[i2], Pm[:, 1440:1952], eng="act")
        P.dma(RV_o[rows, :], RV[i2])
        P.copy(RG[i2], Pm[:, 1952:2464], eng="act")
        P.dma(RG_o[rows, :], RG[i2])
    return P


def k2_build(NQ=16384, NK=16640, NC=256, nb=2):
    P = Prog()
    QT = P.inp("QT", [nb, 98, NQ], BF16)
    QTc = P.inp("QTc", [nb, 98, NC], BF16)
    KT = P.inp("KT", [nb, 98, NK], BF16)
    Vv = P.inp("Vv", [nb, 128, NK // 128, 65], BF16)
    OT = P.out("OT", [nb, 65, NQ])
    OTc = P.out("OTc", [nb, 65, NC])
    nkt = NK // 128
    kT = P.sb("kT", [98, NK], BF16)
    Vs = P.sb("Vs", [128, nkt, 65], BF16)
    qs = [P.sb(f"qs{i}", [98, 512], BF16) for i in range(2)]
    pTs = [P.sb(f"pT{i}", [128, 512], BF16) for i in range(6)]
    osb = [P.sb(f"osb{i}", [65, 512]) for i in range(2)]
    psS = [P.ps(f"psS{i}", [128, 512]) for i in range(6)]
    psO = [P.ps(f"psO{i}", [128, 512]) for i in range(2)]
    LA = 3
    for b in range(nb):
        P.dma(kT, KT[b])
        P.dma(Vs, Vv[b])
        jobs = [(QTc[b], OTc[b], NC, NC // 128)] + [(QT[b][:, q0:q0 + 512], OT[b][:, q0:q0 + 512], 512, nkt) for q0 in range(0, NQ, 512)]
        items = []
        for j, (qsrc, odst, w, nk) in enumerate(jobs):
            for kt in range(nk):
                items.append((j, kt, nk, qsrc, odst, w))
        for idx in range(len(items) + LA):
            if idx < len(items):
                j, kt, nk, qsrc, odst, w = items[idx]
                q = qs[j % 2]
                if kt == 0:
                    P.dma(q[:, :w], qsrc)
                s_ = psS[idx % 6]; pt = pTs[idx % 6]
                P.mm(s_[:, :w], kT[:, kt * 128:(kt + 1) * 128], q[:, :w])
                P.act(pt[:, :w], s_[:, :w], AF.Exp)
            if idx - LA >= 0:
                j, kt, nk, qsrc, odst, w = items[idx - LA]
                po = psO[j % 2]; pt = pTs[(idx - LA) % 6]
                P.mm(po[0:65, :w], Vs[:, kt, :], pt[:, :w], start=(kt == 0), stop=(kt == nk - 1))
                if kt == nk - 1:
                    o = osb[j % 2]
                    P.copy(o[:, :w], po[0:65, :w])
                    P.dma(odst, o[:, :w])
    return P


def ret_consts(h):
    L = 128
    pos = np.arange(L, dtype=np.float64)
    out = {}
    DinT = np.zeros((2, L, L), np.float32); qdec = np.zeros((2, 128, L), np.float32); kdec = np.zeros((128, 2), np.float32); cdec = np.zeros((128, 2), np.float32)
    for d, exp0 in enumerate((5.0, 5.5)):
        lg = np.log1p(-2.0 ** (-exp0 - h))
        j = pos[:, None]; i = pos[None, :]
        if d == 0:
            DinT[d] = np.where(i >= j, np.exp(lg * np.maximum(i - j, 0)), 0.0)
            qdec[d] = np.exp(lg * (pos + 1.0))[None, :]
            kdec[:, d] = np.exp(lg * (L - 1.0 - pos))
        else:
            DinT[d] = np.where(j >= i, np.exp(lg * np.maximum(j - i, 0)), 0.0)
            qdec[d] = np.exp(lg * (L - pos))[None, :]
            kdec[:, d] = np.exp(lg * pos)
        cdec[:, d] = np.exp(lg * L)
    return {"DinT": DinT, "qdec": qdec, "kdec": kdec, "cdec": cdec}

def k3_build(NCH=130, NCTX=2):
    P = Prog()
    N = NCH * 128
    qT = P.inp("qT", [128, N], BF16)
    kT = P.inp("kT", [128, N], BF16)
    kt = P.inp("kt", [128, NCH, 128], BF16)
    vt = P.inp("vt", [128, NCH, 128], BF16)
    DinT = P.inp("DinT", [2, 128, 128]); qdec = P.inp("qdec", [2, 128, 128]); kdec = P.inp("kdec", [128, 2]); cdec = P.inp("cdec", [128, 2])
    o = P.out("o", [2, NCH, 128, 128])
    qTs = P.sb("qTs", [128, N], BF16); kTs = P.sb("kTs", [128, N], BF16)
    kts = P.sb("kts", [128, NCH, 128], BF16); vts = P.sb("vts", [128, NCH, 128], BF16)
    P.dma(qTs, qT); P.dma(kTs, kT); P.dma(kts, kt); P.dma(vts, vt)
    Dm = P.sb("Dm", [128, 2, 128]); qd_ = P.sb("qd_", [128, 2, 128]); kd_ = P.sb("kd_", [128, 2]); cd_ = P.sb("cd_", [128, 2])
    for d in range(2):
        P.dma(Dm[:, d, :], DinT[d]); P.dma(qd_[:, d, :], qdec[d])
    P.dma(kd_, kdec); P.dma(cd_, cdec)
    S = P.sb("S", [128, 128]); Sb = P.sb("Sb", [128, 128], BF16)
    sTm = [P.sb(f"sTm{i}", [128, 128], BF16) for i in range(2)]
    qdv = [P.sb(f"qdv{i}", [128, 128], BF16) for i in range(2)]
    kdv = [P.sb(f"kdv{i}", [128, 128], BF16) for i in range(2)]
    ob = [P.sb(f"ob{i}", [128, 128]) for i in range(2)]
    psA = [P.ps(f"psA{i}", [128, 128]) for i in range(2)]
    psB = [P.ps(f"psB{i}", [128, 128]) for i in range(2)]
    psC = [P.ps(f"psC{i}", [128, 128]) for i in range(2)]
    n = 0
    for d in range(2):
        order = list(range(NCTX)) + list(range(NCTX, NCH))
        if d == 1:
            order = list(range(NCTX))[::-1] + list(range(NCTX, NCH))[::-1]
        P.memset(S, 0.0); P.memset(Sb, 0.0)
        for c in order:
            i2 = n % 2; n += 1
            cols = slice(c * 128, (c + 1) * 128)
            P.mm(psA[i2], kTs[:, cols], qTs[:, cols])
            P.tt(sTm[i2], psA[i2], Dm[:, d, :], ALU.mult)
            P.tt(qdv[i2], qTs[:, cols], qd_[:, d, :], ALU.mult, eng="pool")
            P.mm(psB[i2], sTm[i2], vts[:, c, :], start=True, stop=False)
            P.mm(psB[i2], qdv[i2], Sb, start=False, stop=True)
            P.copy(ob[i2], psB[i2], eng="act")
            P.dma(o[d, c], ob[i2])
            P.ts(kdv[i2], kts[:, c, :], kd_[:, d:d + 1], ALU.mult, eng="pool")
            P.mm(psC[i2], kdv[i2], vts[:, c, :])
            P.stt(S, S, cd_[:, d:d + 1], psC[i2], ALU.mult, ALU.add)
            P.copy(Sb, S)
    return P


def k4_build(T, layer_kind="even", has_ctx=True):
    P = Prog()
    x = P.inp("x", [T * 128, 1024])
    if layer_kind == "even":
        Oatt = P.inp("Oatt", [T * 128, 8, 65])
        Oret = P.inp("Oret", [T * 128, 2, 512])
        RG = P.inp("RG", [T * 128, 512])
        gn = P.inp("gn", [128, 512])
    else:
        Yr = P.inp("Yr", [T * 128, 2, 512]); Gi = P.inp("G", [T * 128, 512]); BGi = P.inp("BG", [T * 128, 512])
        Ow = P.inp("Ow", [T * 128, 8, 65]); NQi = P.inp("NQ", [T * 128, 8])
        sinkB = P.inp("sinkB", [128, 8]); lngB = P.inp("lngB", [128, 512]); lnbB = P.inp("lnbB", [128, 512])
    w_out = P.inp("w_out", [128, 8, 1024])
    g1B = P.inp("g1B", [2, 128, 1024])
    gfB = P.inp("gfB", [128, 1024]); sc2B = P.inp("sc2B", [2, 128, 1024]); sh2B = P.inp("sh2B", [2, 128, 1024])
    w_r = P.inp("w_r", [128, 8, 16])
    identd = P.inp("ident", [128, 128], BF16); identfd = P.inp("identf", [128, 128])
    xmid_o = P.out("xmid", [T * 128, 1024])
    h_o = P.out("h", [T * 128, 1024], BF16)
    aff_o = P.out("aff", [T * 128, 16])
    ident = P.sb("ident_s", [128, 128], BF16); P.dma(ident, identd)
    identf = P.sb("identf_s", [128, 128]); P.dma(identf, identfd)
    stage = P.sb("stage", [128, 1024])
    w_out_b = P.sb("w_out_b", [128, 8, 1024], BF16)
    load_w_bf16(P, w_out_b, w_out, 8, 1024, stage)
    wr = P.sb("wr", [128, 8, 16]); P.dma(wr, w_r)
    G1 = P.sb("G1", [128, 2, 1024]); Gp = P.sb("Gp", [128, 2, 1024]); SH = P.sb("SHt", [128, 2, 1024]); gs = P.sb("gs", [128, 1024])
    P.dma(gs, gfB)
    for c in range(2):
        P.dma(G1[:, c, :], g1B[c])
        P.dma(stage, sc2B[c])
        P.stt(Gp[:, c, :], stage, 1.0, gs, ALU.add, ALU.mult)
        P.dma(SH[:, c, :], sh2B[c])
    if layer_kind == "even":
        gns = P.sb("gns", [128, 512]); P.dma(gns, gn)
    else:
        sks = P.sb("sks", [128, 8]); P.dma(sks, sinkB); lngs = P.sb("lngs", [128, 512]); P.dma(lngs, lngB); lnbs = P.sb("lnbs", [128, 512]); P.dma(lnbs, lnbB)
        gis = [P.sb(f"gis{i}", [128, 512]) for i in range(2)]; bgs = [P.sb(f"bgs{i}", [128, 512]) for i in range(2)]
        nqs = [P.sb(f"nqs{i}", [128, 8]) for i in range(2)]; mu8 = P.sb("mu8", [128, 8]); var8 = P.sb("var8", [128, 8]); den = P.sb("den", [128, 8])
    xs = [P.sb(f"xs{i}", [128, 1024]) for i in range(2)]
    oa = [P.sb(f"oa{i}", [128, 8, 65]) for i in range(2)]
    orr = [P.sb(f"orr{i}", [128, 2, 512]) for i in range(2)]
    rgs = [P.sb(f"rgs{i}", [128, 512]) for i in range(2)]
    cat = P.sb("cat", [128, 1024], BF16); catT = P.sb("catT", [128, 1024], BF16)
    rc = P.sb("rc", [128, 8]); osum = P.sb("osum", [128, 512]); mu = P.sb("mu", [128, 4]); var = P.sb("var", [128, 4])
    junk = P.sb("junk", [128, 1024]); tmp = P.sb("tmp", [128, 1024]); sg = P.sb("sg", [128, 512])
    xm = [P.sb(f"xm{i}", [128, 1024]) for i in range(2)]
    hf = P.sb("hf", [128, 1024]); hb = [P.sb(f"hb{i}", [128, 1024], BF16) for i in range(2)]
    hT = P.sb("hT", [128, 1024]); rs = P.sb("rs", [128, 1])
    lg = P.sb("lg", [128, 16]); mx = P.sb("mx", [128, 1]); sm = P.sb("sm", [128, 1]); af = [P.sb(f"af{i}", [128, 16]) for i in range(2)]
    pT = P.ps("pT", [128, 1024], BF16)
    pTf = [P.ps(f"pTf{i}", [128, 512]) for i in range(2)]
    pp = [P.ps(f"pp{i}", [128, 512]) for i in range(2)]
    pl = P.ps("pl", [128, 16])
    for t in range(T):
        cls = 1 if (t == 0 and has_ctx) else 0
        i2 = t % 2
        rows = slice(t * 128, (t + 1) * 128)
        P.dma(xs[i2], x[rows, :])
        if layer_kind == "even":
            P.dma(oa[i2], Oatt[rows]); P.dma(orr[i2], Oret[rows]); P.dma(rgs[i2], RG[rows, :])
            P.recip(rc, oa[i2][:, :, 64])
            P.tt(cat[:, 0:512].re("p (h d) -> p h d", d=64), oa[i2][:, :, 0:64], V(rc.key, rc.ap.unsqueeze(2).to_broadcast([128, 8, 64])), ALU.mult)
            P.tt(osum, orr[i2][:, 0, :], orr[i2][:, 1, :], ALU.add)
            ov = osum.re("p (h d) -> p h d", d=128)
            P.reduce(mu, ov, ALU.add)
            P.ts(mu, mu, 1.0 / 128, ALU.mult)
            P.tt(ov, ov, V(mu.key, mu.ap.unsqueeze(2).to_broadcast([128, 4, 128])), ALU.subtract)
            P.tt(junk[:, :512], osum, osum, ALU.mult)
            P.reduce(var, junk[:, :512].re("p (h d) -> p h d", d=128), ALU.add)
            P.ts(var, var, 1.0 / 128, ALU.mult, 1e-5, ALU.add)
            P.act(var, var, AF.Sqrt)
            P.recip(var, var)
            P.tt(ov, ov, V(var.key, var.ap.unsqueeze(2).to_broadcast([128, 4, 128])), ALU.mult)
            P.tt(osum, osum, gns, ALU.mult)
            P.act(sg, rgs[i2], AF.Sigmoid)
            P.tt(sg, sg, rgs[i2], ALU.mult)
            P.tt(cat[:, 512:1024], osum, sg, ALU.mult)
        else:
            P.dma(orr[i2], Yr[rows]); P.dma(gis[i2], Gi[rows, :]); P.dma(bgs[i2], BGi[rows, :]); P.dma(oa[i2], Ow[rows]); P.dma(nqs[i2], NQi[rows, :])
            P.tt(osum, orr[i2][:, 0, :], orr[i2][:, 1, :], ALU.add)
            ov = osum.re("p (h d) -> p h d", d=64)
            P.reduce(mu8, ov, ALU.add)
            P.ts(mu8, mu8, 1.0 / 64, ALU.mult)
            P.tt(ov, ov, V(mu8.key, mu8.ap.unsqueeze(2).to_broadcast([128, 8, 64])), ALU.subtract)
            P.tt(junk[:, :512], osum, osum, ALU.mult)
            P.reduce(var8, junk[:, :512].re("p (h d) -> p h d", d=64), ALU.add)
            P.ts(var8, var8, 1.0 / 64, ALU.mult, 64e-5, ALU.add)
            P.act(var8, var8, AF.Sqrt)
            P.recip(var8, var8)
            P.tt(ov, ov, V(var8.key, var8.ap.unsqueeze(2).to_broadcast([128, 8, 64])), ALU.mult)
            P.tt(osum, osum, lngs, ALU.mult)
            P.tt(osum, osum, lnbs, ALU.add)
            P.tt(osum, osum, gis[i2], ALU.mult)
            P.tt(cat[:, 0:512], osum, bgs[i2], ALU.add)
            P.tt(den, nqs[i2], sks, ALU.add)
            P.act(den, den, AF.Exp)
            P.tt(den, den, oa[i2][:, :, 64], ALU.add)
            P.recip(rc, den)
            P.tt(cat[:, 512:1024].re("p (h d) -> p h d", d=64), oa[i2][:, :, 0:64], V(rc.key, rc.ap.unsqueeze(2).to_broadcast([128, 8, 64])), ALU.mult)
        for k in range(8):
            P.tr(pT[:, k * 128:(k + 1) * 128], cat[:, k * 128:(k + 1) * 128], ident)
        P.copy(catT, pT)
        for j in range(2):
            for k in range(8):
                P.mm(pp[j], catT[:, k * 128:(k + 1) * 128], w_out_b[:, k, j * 512:(j + 1) * 512], start=(k == 0), stop=(k == 7))
            P.tt(tmp[:, j * 512:(j + 1) * 512], pp[j], G1[:, cls, j * 512:(j + 1) * 512], ALU.mult)
        P.tt(xm[i2], tmp, xs[i2], ALU.add)
        P.dma(xmid_o[rows, :], xm[i2])
        rms_rstd(P, rs, junk, xm[i2], 1024)
        P.stt(tmp, xm[i2], rs, Gp[:, cls, :], ALU.mult, ALU.mult)
        P.tt(hf, tmp, SH[:, cls, :], ALU.add)
        P.copy(hb[i2], hf, eng="act")
        P.dma(h_o[rows, :], hb[i2])
        for k in range(8):
            P.tr(pTf[k // 4][:, (k % 4) * 128:(k % 4 + 1) * 128], hf[:, k * 128:(k + 1) * 128], identf)
        for j in range(2):
            P.copy(hT[:, j * 512:(j + 1) * 512], pTf[j], eng=("act" if j else "dve"))
        for k in range(8):
            P.mm(pl, hT[:, k * 128:(k + 1) * 128], wr[:, k, :], start=(k == 0), stop=(k == 7))
        P.copy(lg, pl)
        P.reduce(mx, lg, ALU.max)
        P.ts(lg, lg, mx, ALU.subtract)
        P.act(lg, lg, AF.Exp)
        P.reduce(sm, lg, ALU.add)
        P.recip(sm, sm)
        P.ts(af[i2], lg, sm, ALU.mult)
        P.dma(aff_o[rows, :], af[i2])
    return P


BIG = 4000000.0

def k5_build(probs=((128, 2048), (2, 32)), NE=4, NIT=34):
    P = Prog()
    nc = P.nc
    ins = []
    for pi, (J, cap) in enumerate(probs):
        ins.append((P.inp(f"aff{pi}", [128, J, NE]), P.inp(f"h{pi}", [128, J, 1024], BF16),
                    P.out(f"ys{pi}", [NE, cap, 1024], BF16), P.out(f"dest{pi}", [128, J, NE], I32), P.out(f"gsel{pi}", [128, J, NE]),
                    [P.scratch(f"xs{pi}_{e}", [cap, 1024], BF16) for e in range(NE)]))
    wg = P.inp("wg", [NE, 128, 8, 1536]); wu = P.inp("wu", [NE, 128, 8, 1536]); wd = P.inp("wd", [NE, 128, 12, 1024])
    trid = P.inp("tri", [128, 128]); onesd = P.inp("ones", [128, 128]); identd = P.inp("ident", [128, 128], BF16)
    tri = P.sb("tri_s", [128, 128]); ones = P.sb("ones_s", [128, 128]); ident = P.sb("ident_s", [128, 128], BF16)
    P.dma(tri, trid); P.dma(ones, onesd); P.dma(ident, identd)
    JM = max(j for j, _ in probs)
    onesJ = P.sb("onesJ", [128, JM]); P.memset(onesJ, 1.0)
    af = P.sb("af", [128, JM, NE]); cmpb = P.sb("cmpb", [128, JM, NE]); cs = P.sb("cs", [128, JM, NE])
    lo = P.sb("lo", [128, NE]); hi = P.sb("hi", [128, NE]); mid = P.sb("mid", [128, NE]); cntp = P.sb("cntp", [128, NE])
    pred = P.sb("pred", [128, NE]); d1 = P.sb("d1", [128, NE]); d2 = P.sb("d2", [128, NE]); offs = P.sb("offs", [128, NE])
    desti = [P.sb(f"desti{pi}", [128, J, NE], I32) for pi, (J, cap) in enumerate(probs)]
    pt = P.ps("pt", [128, NE])
    hrow = [P.sb(f"hrow{i}", [128, 1024], BF16) for i in range(3)]
    for pi, (J, cap) in enumerate(probs):
        aff_i, h_i, ys_o, dest_o, gsel_o, xs = ins[pi]
        a = af[:, :J, :]; cb = cmpb[:, :J, :]; c_ = cs[:, :J, :]
        P.dma(a, aff_i)
        P.memset(lo, 0.0); P.memset(hi, 1.5)
        def bcj(v): return V(v.key, v.ap.unsqueeze(1).to_broadcast([128, J, NE]))
        for it in range(NIT):
            P.tt(mid, lo, hi, ALU.add)
            P.ts(mid, mid, 0.5, ALU.mult)
            P.tt(cb, a, bcj(mid), ALU.is_ge)
            P.reduce(cntp, cb.re("p j e -> p e j"), ALU.add)
            P.mm(pt, ones, cntp)
            P.ts(pred, pt, cap - 0.5, ALU.is_ge)
            P.tt(d1, mid, lo, ALU.subtract)
            P.tt(d1, d1, pred, ALU.mult)
            P.tt(d2, hi, mid, ALU.subtract)
            P.tt(d2, d2, pred, ALU.mult)
            P.tt(lo, lo, d1, ALU.add)
            P.tt(hi, mid, d2, ALU.add)
        P.tt(cb, a, bcj(lo), ALU.is_ge)
        P.reduce(cntp, cb.re("p j e -> p e j"), ALU.add)
        P.mm(pt, tri, cntp)
        P.ts(offs, pt, -(1.0 + BIG), ALU.add)
        for e in range(NE):
            P.scan(c_[:, :, e], onesJ[:, :J], cb[:, :, e], 0.0, ALU.mult, ALU.add)
        P.tt(c_, c_, bcj(offs), ALU.add)
        P.tt(c_, c_, cb, ALU.mult)
        P.ts(c_, c_, BIG, ALU.add)
        P.copy(desti[pi], c_)
        P.dma(dest_o, desti[pi])
        P.tt(cb, cb, a, ALU.mult)
        P.dma(gsel_o, cb)
        for j in range(J):
            hr = hrow[j % 3]
            P.dma(hr, h_i[:, j, :])
            for e in range(NE):
                idx = desti[pi][:, j, e:e + 1]
                P.op("pool", (lambda g, e=e, idx=idx, hr=hr, xs=xs, cap=cap: g.indirect_dma_start(
                    out=xs[e].ap, out_offset=bass.IndirectOffsetOnAxis(ap=idx.ap, axis=0), in_=hr.ap, in_offset=None,
                    bounds_check=P.bcreg(g, cap - 1), oob_is_err=False)), [hr, idx], [xs[e]], kind="d")
    stage2 = [P.sb(f"stage{i}", [128, 1536]) for i in range(2)]; wcnt = [0]
    wgb = P.sb("wgb", [128, 8, 1536], BF16); wub = P.sb("wub", [128, 8, 1536], BF16); wdb = P.sb("wdb", [128, 12, 1024], BF16)
    xr = [P.sb(f"xr{i}", [128, 1024], BF16) for i in range(2)]
    xT = P.sb("xT", [128, 8, 512], BF16)
    uT = P.sb("uT", [128, 12, 512], BF16)
    sgt = [P.sb(f"sgt{i}", [128, 512]) for i in range(2)]
    yb = [P.sb(f"yb{i}", [128, 1024], BF16) for i in range(2)]
    pT = P.ps("pT", [128, 1024], BF16)
    pg = [P.ps(f"pg{i}", [128, 512]) for i in range(2)]
    pu = [P.ps(f"pu{i}", [128, 512]) for i in range(2)]
    py = [P.ps(f"py{i}", [128, 512]) for i in range(2)]
    n = 0
    for e in range(NE):
        for (dst_, src_, kc_, nc_) in ((wgb, wg[e], 8, 1536), (wub, wu[e], 8, 1536), (wdb, wd[e], 12, 1024)):
            for k in range(kc_):
                st_ = stage2[wcnt[0] % 2]; wcnt[0] += 1
                P.dma(st_[:, :nc_], src_[:, k, :])
                P.copy(dst_[:, k, :], st_[:, :nc_], eng=("act" if k % 2 else "dve"))
        for pi, (J, cap) in enumerate(probs):
            aff_i, h_i, ys_o, dest_o, gsel_o, xs = ins[pi]
            for g0 in range(0, cap, 512):
                gw = min(512, cap - g0)
                nblk = (gw + 127) // 128
                for bi in range(nblk):
                    r0 = g0 + bi * 128; rw = min(128, cap - r0)
                    x_ = xr[bi % 2]
                    P.dma(x_[:rw, :], xs[e][r0:r0 + rw, :])
                    for k in range(8):
                        P.tr(pT[:, k * 128:k * 128 + rw], x_[:rw, k * 128:(k + 1) * 128], ident[:rw, :rw])
                    P.copy(xT[:, :, bi * 128:bi * 128 + rw], pT.re("p (k t) -> p k t", k=8)[:, :, :rw], eng=("act" if bi % 2 else "dve"))
                for fc in range(12):
                    i2 = n % 2; n += 1
                    for k in range(8):
                        P.mm(pg[i2][:, :gw], wgb[:, k, fc * 128:(fc + 1) * 128], xT[:, k, :gw], start=(k == 0), stop=(k == 7))
                    for k in range(8):
                        P.mm(pu[i2][:, :gw], wub[:, k, fc * 128:(fc + 1) * 128], xT[:, k, :gw], start=(k == 0), stop=(k == 7))
                    P.act(sgt[i2][:, :gw], pg[i2][:, :gw], AF.Sigmoid)
                    P.tt(sgt[i2][:, :gw], sgt[i2][:, :gw], pg[i2][:, :gw], ALU.mult)
                    P.tt(uT[:, fc, :gw], sgt[i2][:, :gw], pu[i2][:, :gw], ALU.mult)
                for bi in range(nblk):
                    r0 = g0 + bi * 128; rw = min(128, cap - r0)
                    y_ = yb[bi % 2]
                    for j in range(2):
                        for fc in range(12):
                            P.mm(py[j][:rw, :], uT[:, fc, bi * 128:bi * 128 + rw], wdb[:, fc, j * 512:(j + 1) * 512], start=(fc == 0), stop=(fc == 11))
                        P.copy(y_[:rw, j * 512:(j + 1) * 512], py[j][:rw, :], eng=("act" if j else "dve"))
                    P.dma(ys_o[e, r0:r0 + rw, :], y_[:rw, :])
    return P


def k6_build(T, caps=(2048, 32), final=False, NE=16):
    P = Prog()
    xmid = P.inp("xmid", [T * 128, 1024])
    gsel = P.inp("gsel", [T * 128, NE])
    dest = P.inp("dest", [T * 128, NE], I32)
    ys = [[P.inp(f"ys{c}_{e}", [caps[c], 1024], BF16) for e in range(NE)] for c in range(len(caps))]
    g2B = P.inp("g2B", [2, 128, 1024])
    xo = P.out("xo", [T * 128, 1024])
    G2 = P.sb("G2", [128, 2, 1024])
    for c in range(2):
        P.dma(G2[:, c, :], g2B[c])
    if final:
        fgB = P.inp("fgB", [128, 1024]); fg = P.sb("fg", [128, 1024]); P.dma(fg, fgB)
        fo = P.out("fo", [T * 128, 1024])
        junk = P.sb("junk", [128, 1024]); rs = P.sb("rs", [128, 1])
    xs = [P.sb(f"xs{i}", [128, 1024]) for i in range(2)]
    gs = [P.sb(f"gs{i}", [128, NE]) for i in range(2)]
    ds = [P.sb(f"ds{i}", [128, NE], I32) for i in range(2)]
    gt = [P.sb(f"gt{i}", [128, 1024], BF16) for i in range(4)]
    for g in gt:
        P.memset(g, 0.0)
    acc = P.sb("acc", [128, 1024])
    ob = [P.sb(f"ob{i}", [128, 1024]) for i in range(2)]
    fb = [P.sb(f"fb{i}", [128, 1024]) for i in range(2)]
    n = 0
    for t in range(T):
        cls = 1 if (t == 0 and len(caps) > 1) else 0
        i2 = t % 2
        rows = slice(t * 128, (t + 1) * 128)
        P.dma(xs[i2], xmid[rows, :]); P.dma(gs[i2], gsel[rows, :]); P.dma(ds[i2], dest[rows, :])
        P.memset(acc, 0.0)
        for e in range(NE):
            g = gt[n % 4]; n += 1
            idx = ds[i2][:, e:e + 1]
            src = ys[cls][e]
            P.op("pool", (lambda q, e=e, idx=idx, g=g, src=src, cap=caps[cls]: q.indirect_dma_start(
                out=g.ap, out_offset=None, in_=src.ap, in_offset=bass.IndirectOffsetOnAxis(ap=idx.ap, axis=0),
                bounds_check=P.bcreg(q, cap - 1), oob_is_err=False)), [src, idx], [g], kind="d")
            P.stt(acc, g, gs[i2][:, e:e + 1], acc, ALU.mult, ALU.add)
        P.tt(acc, acc, G2[:, cls, :], ALU.mult)
        P.tt(ob[i2], acc, xs[i2], ALU.add)
        P.dma(xo[rows, :], ob[i2])
        if final:
            rms_rstd(P, rs, junk, ob[i2], 1024)
            P.stt(fb[i2], ob[i2], rs, fg, ALU.mult, ALU.mult)
            P.dma(fo[rows, :], fb[i2])
    return P


SQW = 64 ** -0.25
RW_CH = [(0, 512), (512, 1024), (1024, 1536), (1536, 1920)]
WIN_CH = [(1920, 2432), (2432, 2688)]

def k7_build(T):
    P = Prog()
    x = P.inp("x", [T * 128, 1024]); xp = P.inp("xp", [T * 128, 1024]); xn = P.inp("xn", [T * 128, 1024]); fl = P.inp("fl", [T * 128, 2])
    gB = P.inp("gB", [128, 1024]); scB = P.inp("scB", [2, 128, 1024]); shB = P.inp("shB", [2, 128, 1024])
    w_in = P.inp("w_in", [128, 8, 2688]); muB = P.inp("muB", [128, 1920])
    kkB = P.inp("kkB", [128, 512]); kaB = P.inp("kaB", [128, 512]); rkB = P.inp("rkB", [128, 512])
    w0B = P.inp("w0B", [2, 128, 512]); a0B = P.inp("a0B", [2, 128, 512])
    w2s = P.inp("w2s", [128, 1, 512]); a2s = P.inp("a2s", [128, 1, 512]); g2s = P.inp("g2s", [128, 1, 512])
    Cw = P.inp("Cw", [T * 128, 64]); Sw = P.inp("Sw", [T * 128, 64])
    identd = P.inp("ident", [128, 128], BF16)
    onames = ["R", "Vv", "KK", "LWf", "LWb", "Af", "Ab", "Kf", "Kb", "G", "BG"]
    outs = {n: P.out(n, [T * 128, 512]) for n in onames}
    NQ_o = P.out("NQ", [T * 128, 8])
    Qw_o = P.out("Qw", [T * 128, 8 * 66], BF16); Kw_o = P.out("Kw", [T * 128, 2 * 66], BF16); Vw_o = P.out("Vw", [T * 128, 2 * 65], BF16)
    ident = P.sb("ident_s", [128, 128], BF16); P.dma(ident, identd)
    stage = P.sb("stage", [128, 2688])
    Pm = P.sb("Pm", [128, 2688]); tmpw = Pm[:, :1920]
    mus = P.sb("mus", [128, 1920]); P.dma(mus, muB)
    W1 = P.sb("W1", [128, 8, 1920], BF16); W2 = P.sb("W2", [128, 8, 1920], BF16); Ww = P.sb("Ww", [128, 8, 768], BF16)
    for k in range(8):
        P.dma(stage, w_in[:, k, :])
        P.tt(tmpw, stage[:, :1920], mus, ALU.mult)
        P.copy(W2[:, k, :], tmpw, eng="act")
        P.tt(W1[:, k, :], stage[:, :1920], tmpw, ALU.subtract)
        P.copy(Ww[:, k, :], stage[:, 1920:2688], eng="act")
    w2b = P.sb("w2b", [128, 1, 512], BF16); a2b = P.sb("a2b", [128, 1, 512], BF16); g2b = P.sb("g2b", [128, 1, 512], BF16)
    load_w_bf16(P, w2b, w2s, 1, 512, stage); load_w_bf16(P, a2b, a2s, 1, 512, stage); load_w_bf16(P, g2b, g2s, 1, 512, stage)
    cB = {}
    for n, d in (("kk", kkB), ("ka", kaB), ("rk", rkB)):
        cB[n] = P.sb(n + "_s", [128, 512]); P.dma(cB[n], d)
    w0s = P.sb("w0s", [128, 2, 512]); a0s = P.sb("a0s", [128, 2, 512])
    for d in range(2):
        P.dma(w0s[:, d, :], w0B[d]); P.dma(a0s[:, d, :], a0B[d])
    gs = P.sb("gs", [128, 1024]); P.dma(gs, gB)
    Gp = P.sb("Gp", [128, 2, 1024]); SH = P.sb("SHt", [128, 2, 1024])
    for c in range(2):
        P.dma(stage[:, :1024], scB[c])
        P.stt(Gp[:, c, :], stage[:, :1024], 1.0, gs, ALU.add, ALU.mult)
        P.dma(SH[:, c, :], shB[c])
    xs = [[P.sb(f"xs{i}_{j}", [128, 1024]) for j in range(3)] for i in range(1)] * 2
    fls = [P.sb(f"fls{i}", [128, 2]) for i in range(2)]
    tabs = [[P.sb(f"tab{i}_{j}", [128, 64]) for j in range(2)] for i in range(2)]
    junk = P.sb("junk", [128, 1024]); tmp = P.sb("tmp", [128, 1024]); rs = P.sb("rs", [128, 1])
    a_bf = P.sb("a_bf", [128, 1024], BF16); af32 = [P.sb(f"af32_{j}", [128, 1024]) for j in range(2)]
    ash = P.sb("ash", [128, 1024], BF16)
    aT = P.sb("aT", [128, 1024], BF16); ashT = P.sb("ashT", [128, 1024], BF16)
    sm = {n: P.sb("sm_" + n, [128, 8]) for n in ("ss", "srk", "nq")}
    nk2 = P.sb("nk2", [128, 2]); ek2 = P.sb("ek2", [128, 2])
    bft = {n: P.sb("bf_" + n, [128, 128], BF16) for n in ("th", "al", "sg")}
    bfT = {n: P.sb("bfT_" + n, [128, 128], BF16) for n in ("th", "al", "sg")}
    ob = {n: [P.sb(f"ob_{n}{i}", [128, 512]) for i in range(1)] * 2 for n in onames if n not in ("R", "Vv")}
    t5 = P.sb("t5", [128, 512]); t6 = P.sb("t6", [128, 512]); qf = P.sb("qf", [128, 512]); kf = P.sb("kf", [128, 128])
    NQb = [P.sb(f"NQb{i}", [128, 8]) for i in range(2)]
    Qw = [P.sb(f"Qw{i}", [128, 8, 66], BF16) for i in range(2)]
    Kw = [P.sb(f"Kw{i}", [128, 2, 66], BF16) for i in range(2)]
    Vw = [P.sb(f"Vw{i}", [128, 2, 65], BF16) for i in range(2)]
    pT = P.ps("pT", [128, 1024], BF16)
    pp = [P.ps(f"pp{i}", [128, 512]) for i in range(6)]
    pcnt = [0]
    def npp():
        p = pp[pcnt[0] % 6]; pcnt[0] += 1
        return p

    def norm_mod(dst, xt, cls):
        rms_rstd(P, rs, junk, xt, 1024)
        P.stt(tmp, xt, rs, Gp[:, cls, :], ALU.mult, ALU.mult)
        P.tt(dst, tmp, SH[:, cls, :], ALU.add)

    def rope(dst, src, nh, tb, scale):
        w = nh * 64
        Cb = V(tb[0].key, tb[0].ap.unsqueeze(1).to_broadcast([128, nh, 64]))
        t1v = t5[:, :w].re("p (h d) -> p h d", d=64); t2v = t6[:, :w].re("p (h d) -> p h d", d=64)
        P.tt(t1v, src, Cb, ALU.mult)
        s5 = src.re("p h (b f e) -> p h b f e", b=2, f=2); t25 = t2v.re("p h (b f e) -> p h b f e", b=2, f=2)
        S5 = tb[1].ap.rearrange("p (b f e) -> p b f e", b=2, f=2)
        for f in range(2):
            Sb = V(tb[1].key, S5[:, :, f, :].unsqueeze(1).to_broadcast([128, nh, 2, 16]))
            P.tt(t25[:, :, :, f, :], s5[:, :, :, 1 - f, :], Sb, ALU.mult)
        P.tt(t1v, t1v, t2v, ALU.add)
        P.ts(dst, t1v, scale, ALU.mult)

    for t in range(T):
        cls = 1 if t == 0 else 0
        i2 = t % 2
        rows = slice(t * 128, (t + 1) * 128)
        xt, xpt, xnt = xs[i2]
        P.dma(xt, x[rows, :]); P.dma(xpt, xp[rows, :]); P.dma(xnt, xn[rows, :]); P.dma(fls[i2], fl[rows, :])
        tb = tabs[i2]
        P.dma(tb[0], Cw[rows, :]); P.dma(tb[1], Sw[rows, :])
        norm_mod(a_bf, xt, cls)
        norm_mod(af32[0], xpt, cls)
        norm_mod(af32[1], xnt, cls)
        P.ts(af32[0], af32[0], fls[i2][:, 0:1], ALU.mult, 0.5, ALU.mult)
        P.ts(af32[1], af32[1], fls[i2][:, 1:2], ALU.mult, 0.5, ALU.mult)
        P.tt(ash, af32[0], af32[1], ALU.add)
        for k in range(8):
            P.tr(pT[:, k * 128:(k + 1) * 128], a_bf[:, k * 128:(k + 1) * 128], ident)
        P.copy(aT, pT)
        for k in range(8):
            P.tr(pT[:, k * 128:(k + 1) * 128], ash[:, k * 128:(k + 1) * 128], ident)
        P.copy(ashT, pT)
        for j, (c0, c1) in enumerate(RW_CH):
            p = npp()
            for k in range(8):
                P.mm(p[:, :c1 - c0], aT[:, k * 128:(k + 1) * 128], W1[:, k, c0:c1], start=(k == 0), stop=False)
            for k in range(8):
                P.mm(p[:, :c1 - c0], ashT[:, k * 128:(k + 1) * 128], W2[:, k, c0:c1], start=False, stop=(k == 7))
            P.copy(Pm[:, c0:c1], p[:, :c1 - c0], eng=("act" if j % 2 else "dve"))
        for j, (c0, c1) in enumerate(WIN_CH):
            p = npp()
            for k in range(8):
                P.mm(p[:, :c1 - c0], aT[:, k * 128:(k + 1) * 128], Ww[:, k, c0 - 1920:c1 - 1920], start=(k == 0), stop=(k == 7))
            P.copy(Pm[:, c0:c1], p[:, :c1 - c0], eng=("act" if j % 2 else "dve"))
        O = {n: ob[n][i2] for n in onames if n not in ("R", "Vv")}
        r_, k_, v_ = Pm[:, 0:512], Pm[:, 512:1024], Pm[:, 1024:1536]
        O["R"] = r_; O["Vv"] = v_
        P.tt(t5, k_, cB["kk"], ALU.mult)
        P.tt(t6, t5, t5, ALU.mult)
        P.reduce(sm["ss"], t6.re("p (h d) -> p h d", d=64), ALU.add)
        P.ts(sm["ss"], sm["ss"], 1e-12, ALU.add)
        P.act(sm["ss"], sm["ss"], AF.Sqrt)
        P.recip(sm["ss"], sm["ss"])
        P.tt(O["KK"].re("p (h d) -> p h d", d=64), t5.re("p (h d) -> p h d", d=64), V(sm["ss"].key, sm["ss"].ap.unsqueeze(2).to_broadcast([128, 8, 64])), ALU.mult)
        P.act(bft["th"], Pm[:, 1536:1664], AF.Tanh)
        P.copy(bft["al"], Pm[:, 1664:1792])
        P.act(bft["sg"], Pm[:, 1792:1920], AF.Sigmoid)
        for i, n in enumerate(("th", "al", "sg")):
            P.tr(pT[:, i * 128:(i + 1) * 128], bft[n], ident)
            P.copy(bfT[n], pT[:, i * 128:(i + 1) * 128], eng=("act" if i % 2 else "dve"))
        for d, (lw_n, a_n, k_n) in enumerate((("LWf", "Af", "Kf"), ("LWb", "Ab", "Kb"))):
            ps_ = slice(d * 64, (d + 1) * 64)
            p = npp(); P.mm(p, bfT["th"][ps_, :], w2b[ps_, 0, :])
            P.tt(t5, p, w0s[:, d, :], ALU.add)
            P.act(t5, t5, AF.Sigmoid)
            P.ts(O[lw_n], t5, -0.6065306597126334, ALU.mult)
            p = npp(); P.mm(p, bfT["al"][ps_, :], a2b[ps_, 0, :])
            P.tt(t5, p, a0s[:, d, :], ALU.add)
            P.act(O[a_n], t5, AF.Sigmoid)
            P.stt(t6, O[a_n], -1.0, cB["ka"], ALU.add, ALU.mult)
            P.stt(O[k_n], t6, 1.0, k_, ALU.add, ALU.mult)
        p = npp(); P.mm(p, bfT["sg"], g2b[:, 0, :])
        P.copy(O["G"], p, eng="act")
        P.tt(t5, r_, k_, ALU.mult)
        P.tt(t5, t5, cB["rk"], ALU.mult)
        P.reduce(sm["srk"], t5.re("p (h d) -> p h d", d=64), ALU.add)
        P.tt(t6.re("p (h d) -> p h d", d=64), v_.re("p (h d) -> p h d", d=64), V(sm["srk"].key, sm["srk"].ap.unsqueeze(2).to_broadcast([128, 8, 64])), ALU.mult)
        P.tt(O["BG"], t6, O["G"], ALU.mult)
        for n in onames:
            P.dma(outs[n][rows, :], O[n])
        qv = qf.re("p (h d) -> p h d", d=64)
        rope(qv, Pm[:, 1920:2432].re("p (h d) -> p h d", d=64), 8, tb, SQW)
        Qt = Qw[i2]
        P.copy(Qt[:, :, 0:64], qv, eng="act")
        P.tt(t5, qf, qf, ALU.mult)
        P.reduce(sm["nq"], t5.re("p (h d) -> p h d", d=64), ALU.add)
        P.ts(NQb[i2], sm["nq"], -0.5, ALU.mult)
        P.copy(Qt[:, :, 64], NQb[i2])
        P.memset(Qt[:, :, 65], 1.0)
        P.dma(NQ_o[rows, :], NQb[i2])
        P.dma(Qw_o[rows, :], Qt.re("p h d -> p (h d)"))
        kv = kf.re("p (h d) -> p h d", d=64)
        rope(kv, Pm[:, 2432:2560].re("p (h d) -> p h d", d=64), 2, tb, SQW)
        Kt = Kw[i2]; Vt = Vw[i2]
        P.copy(Kt[:, :, 0:64], kv, eng="act")
        P.memset(Kt[:, :, 64], 1.0)
        P.tt(t5[:, :128], kf, kf, ALU.mult)
        P.reduce(nk2, t5[:, :128].re("p (h d) -> p h d", d=64), ALU.add)
        P.ts(Kt[:, :, 65], nk2, -0.5, ALU.mult)
        P.act(ek2, Kt[:, :, 65], AF.Exp, scale=-1.0)
        P.tt(Vt[:, :, 0:64], Pm[:, 2560:2688].re("p (h d) -> p h d", d=64), V(ek2.key, ek2.ap.unsqueeze(2).to_broadcast([128, 2, 64])), ALU.mult)
        P.copy(Vt[:, :, 64], ek2)
        P.dma(Kw_o[rows, :], Kt.re("p h d -> p (h d)"))
        P.dma(Vw_o[rows, :], Vt.re("p h d -> p (h d)"))
    return P


L = 64

def k8_consts():
    s = np.arange(L)[:, None]; t = np.arange(L)[None, :]
    t4 = lambda m: np.ascontiguousarray(np.tile(m.astype(np.float32), (1, 4)))
    return {"triI": (s <= t).astype(np.float32), "ones": np.ones((L, L), np.float32), "mS": t4(s < t), "mST": t4(s > t), "mI": t4(s <= t),
            "I4": t4(np.eye(L)), "identf": np.eye(L, dtype=np.float32)}

def k8_build(NCH, NCTX, W=3):
    P = Prog()
    names = ["R", "K", "Vv", "KK", "A", "LW"]
    din = {n: P.inp(n, [NCH, L, 256]) for n in names}
    yo = P.out("y", [NCH - NCTX, L, 256])
    cst = {}
    for n, w in (("triI", 64), ("ones", 64), ("mS", 256), ("mST", 256), ("mI", 256), ("I4", 256), ("identf", 64)):
        d = P.inp(n, [L, w]); cst[n] = P.sb(n + "_s", [L, w]); P.dma(cst[n], d)
    tl = {}
    cur = [0]
    def T_(n, w=256, nb=1):
        k = (n, cur[0])
        if k not in tl:
            tl[k] = [P.sb(f"{n}_s{cur[0]}", [L, w])] * 2
        return tl[k]
    pss = [P.ps(f"ps{i}", [L, 512]) for i in range(8)]
    pc = [0]
    def nps():
        p = pss[pc[0] % 8]; pc[0] += 1
        return p[:, :256]
    H = [slice(h * 64, (h + 1) * 64) for h in range(4)]
    def mm4(ps, A_, B_, start=True, stop=True):
        for h in range(4):
            P.mm(ps[:, H[h]], A_[:, H[h]], B_[:, H[h]], start=start, stop=stop)
    ST = P.sb("ST", [L, 256]); P.memset(ST, 0.0)
    def chunk(c):
        i2 = 0
        X = {}
        for n in names:
            X[n] = T_("in_" + n, nb=2)[i2]
            P.dma(X[n], din[n][c])
        R, K, Vv, KK, A, LW = (X[n] for n in names)
        pA = nps(); P.mm(pA, cst["triI"], LW)
        pB = nps(); P.mm(pB, cst["ones"], LW)
        yield
        cum = T_("cum")[0]; P.copy(cum, pA)
        eP = T_("eP")[0]; P.act(eP, cum, AF.Exp)
        eN = T_("eN")[0]; P.act(eN, cum, AF.Exp, scale=-1.0)
        yield
        t1 = T_("t1")[0]; P.tt(t1, cum, LW, ALU.subtract)
        ePx = T_("ePx")[0]; P.act(ePx, t1, AF.Exp)
        t2 = T_("t2")[0]; P.tt(t2, pB, cum, ALU.subtract)
        eT = T_("eT")[0]; P.act(eT, t2, AF.Exp)
        yield
        al = T_("al")[0]; P.stt(al, KK, -1.0, ePx, ALU.mult, ALU.mult)
        be = T_("be")[0]; P.tt(be, KK, A, ALU.mult)
        bcn = T_("bcn")[0]; P.tt(bcn, be, eN, ALU.mult)
        kc = T_("kc")[0]; P.tt(kc, K, eN, ALU.mult)
        rt = T_("rt")[0]; P.tt(rt, R, eP, ALU.mult)
        bh = T_("bh")[0]; P.tt(bh, be, eT, ALU.mult)
        kh = T_("kh")[0]; P.tt(kh, K, eT, ALU.mult)
        yield
        pPL = nps()
        for h in range(4):
            P.mm(pPL[:, h:h + 1], LW[:, H[h]], cst["ones"][:, 0:1])
        PL = T_("PL", 4)[0]; P.act(PL, pPL[:, 0:4], AF.Exp)
        yield
        TT = {}
        for n, src in (("alT", al), ("bcT", bcn), ("kcT", kc), ("rtT", rt)):
            p = nps()
            for h in range(4):
                P.tr(p[:, H[h]], src[:, H[h]], cst["identf"])
            TT[n] = T_(n)[0]; P.copy(TT[n], p, eng="act")
            yield
        def gram(name, a, b, mask):
            p = nps(); mm4(p, TT[a], TT[b])
            o = T_(name)[0]; P.tt(o, p, cst[mask], ALU.mult)
            return o
        M = gram("M0", "bcT", "alT", "mS")
        N_ = gram("N0", "alT", "bcT", "mST")
        AakT = gram("AakT", "kcT", "alT", "mS")
        ArbT = gram("ArbT", "bcT", "rtT", "mI")
        ArkT = gram("ArkT", "kcT", "rtT", "mI")
        yield
        Tt = T_("Tt")[0]; P.tt(Tt, M, cst["I4"], ALU.add)
        yield
        for i in range(5):
            pM = nps(); mm4(pM, N_, M)
            pN = nps(); mm4(pN, M, N_)
            Mn = T_(f"Mn{i % 2}")[0]; Nn = T_(f"Nn{i % 2}")[0]
            P.copy(Mn, pM, eng="act"); P.copy(Nn, pN)
            yield
            pX = nps(); mm4(pX, Nn, Tt)
            P.tt(Tt, Tt, pX, ALU.add)
            yield
            M, N_ = Mn, Nn
        p = nps(); mm4(p, AakT, Vv); AV = T_("AV")[0]; P.copy(AV, p, eng="act")
        yield
        p = nps(); mm4(p, Tt, AV); U0 = T_("U0")[0]; P.copy(U0, p)
        yield
        p = nps(); mm4(p, Tt, al); Ah = T_("Ah")[0]; P.copy(Ah, p, eng="act")
        yield
        p = nps(); mm4(p, Ah, bh); GT = T_("GT")[0]; P.copy(GT, p)
        p = nps(); mm4(p, Ah, ArbT); RhT = T_("RhT")[0]; P.tt(RhT, p, TT["rtT"], ALU.add)
        yield
        if c >= NCTX:
            p = nps()
            for h in range(4):
                P.mm(p[:, H[h]], ArbT[:, H[h]], U0[:, H[h]], start=True, stop=False)
                P.mm(p[:, H[h]], ArkT[:, H[h]], Vv[:, H[h]], start=False, stop=False)
                P.mm(p[:, H[h]], RhT[:, H[h]], ST[:, H[h]], start=False, stop=True)
            yb = T_("yb", nb=2)[i2]; P.copy(yb, p, eng="act")
            P.dma(yo[c - NCTX], yb)
        p = nps()
        for h in range(4):
            P.mm(p[:, H[h]], bh[:, H[h]], U0[:, H[h]], start=True, stop=False)
            P.mm(p[:, H[h]], kh[:, H[h]], Vv[:, H[h]], start=False, stop=False)
            P.mm(p[:, H[h]], GT[:, H[h]], ST[:, H[h]], start=False, stop=True)
        for h in range(4):
            P.stt(ST[:, H[h]], ST[:, H[h]], PL[:, h:h + 1], p[:, H[h]], ALU.mult, ALU.add)

    active = []
    nxt = 0
    while nxt < NCH or active:
        if nxt < NCH and len(active) < W:
            active.append((nxt % W, chunk(nxt))); nxt += 1
        for item in list(active):
            cur[0] = item[0]
            try:
                next(item[1])
            except StopIteration:
                active.remove(item)
    return P


def k9_consts():
    import numpy as np
    kj = np.arange(128)[:, None]; qi = np.arange(128)[None, :]
    mL = (kj >= qi).astype(np.float32)
    mR = (kj <= qi).astype(np.float32)
    t4 = lambda m: np.ascontiguousarray(np.tile(m, (1, 4)))
    return {"mL": t4(mL), "mR": t4(mR)}

def k9_build(NB=32):
    P = Prog()
    NKB = NB + 2
    QT = P.inp("QT", [2, 66, NB * 512], BF16)
    KT = P.inp("KT", [2, 66, NKB * 128], BF16)
    Vv = P.inp("Vv", [2, 128, NKB, 65], BF16)
    KTc = P.inp("KTc", [2, 66, 256], BF16); Vc = P.inp("Vc", [2, 128, 2, 65], BF16)
    mLd = P.inp("mL", [128, 512]); mRd = P.inp("mR", [128, 512])
    OT = P.out("OT", [2, 65, NB * 512])
    mL = P.sb("mL_s", [128, 512]); mR = P.sb("mR_s", [128, 512]); P.dma(mL, mLd); P.dma(mR, mRd)
    kT = P.sb("kT", [66, NKB * 128], BF16); vs = P.sb("vs", [128, NKB, 65], BF16)
    kTc = P.sb("kTc", [66, 256], BF16); vc = P.sb("vc", [128, 2, 65], BF16)
    qs = [P.sb(f"qs{i}", [66, 512], BF16) for i in range(2)]
    pTs = [P.sb(f"pT{i}", [128, 512], BF16) for i in range(4)]
    ef = [P.sb(f"ef{i}", [128, 512]) for i in range(2)]
    osb = [P.sb(f"osb{i}", [65, 512]) for i in range(2)]
    psS = [P.ps(f"psS{i}", [128, 512]) for i in range(4)]
    psO = [P.ps(f"psO{i}", [128, 512]) for i in range(2)]
    cnt = 0; blk = 0
    for kv in range(2):
        P.dma(kT, KT[kv]); P.dma(vs, Vv[kv]); P.dma(kTc, KTc[kv]); P.dma(vc, Vc[kv])
        for n in range(NB):
            q = qs[blk % 2]
            P.dma(q, QT[kv][:, n * 512:(n + 1) * 512])
            po = psO[blk % 2]
            tiles = [("c", 0), ("c", 1), ("L", n), ("C", n + 1), ("R", n + 2)]
            for ti, (kind, kb) in enumerate(tiles):
                s = psS[cnt % 4]; pt = pTs[cnt % 4]; cnt += 1
                if kind == "c":
                    P.mm(s, kTc[:, kb * 128:(kb + 1) * 128], q)
                else:
                    P.mm(s, kT[:, kb * 128:(kb + 1) * 128], q)
                if kind in ("L", "R"):
                    e = ef[ti % 2]
                    P.act(e, s, AF.Exp)
                    P.tt(pt, e, mL if kind == "L" else mR, ALU.mult)
                else:
                    P.act(pt, s, AF.Exp)
                vv = vc[:, kb, :] if kind == "c" else vs[:, kb, :]
                P.mm(po[0:65, :], vv, pt, start=(ti == 0), stop=(ti == len(tiles) - 1))
            o = osb[blk % 2]
            P.copy(o, po[0:65, :])
            P.dma(OT[kv][:, n * 512:(n + 1) * 512], o)
            blk += 1
    return P

import time, os, sys, numpy as np, ml_dtypes

bf = ml_dtypes.bfloat16
B, N, D, NCTX = 2, 16384, 1024, 256
T = 33
def bc(v): return np.ascontiguousarray(np.broadcast_to(np.asarray(v, np.float32), (128, len(v))))
def wl(w, k): return np.ascontiguousarray(w.reshape(k, 128, -1).transpose(1, 0, 2))
def pack(i, lat, ctx):
    b, q = i // 4, i % 4
    return np.ascontiguousarray(np.concatenate([ctx[b][(q % 2) * 128:(q % 2 + 1) * 128], lat[b][q * 4096:(q + 1) * 4096]], 0))
def unpack(outs, name):
    lat = np.stack([np.concatenate([outs[b * 4 + q][name][128:] for q in range(4)], 0) for b in range(B)])
    ctx = np.stack([np.concatenate([outs[b * 4 + q][name][:128] for q in range(2)], 0) for b in range(B)])
    return lat, ctx
def run(P, in_maps, tag=""):
    t = time.time(); nc = P.build(); tb = time.time() - t
    t = time.time(); res = run_bass_kernel_spmd(nc, in_maps, core_ids=list(range(8))); print(f"[{tag}] build {tb:.1f}s run {time.time()-t:.1f}s exec_ns={getattr(res, 'exec_time_ns', None)}", flush=True)
    return res.results
def rope_tabs_mla(tpos):
    n = len(tpos); C = np.ones((n, 32), np.float32); S = np.zeros((n, 32), np.float32)
    half = 8
    inv = (10000.0 ** (-np.arange(half, dtype=np.float32) / half)).astype(np.float32)
    for blk, pos in enumerate((tpos // 64, tpos % 64)):
        ang = pos.astype(np.float32)[:, None] * inv[None, :]
        c = np.cos(ang).astype(np.float32); s = np.sin(ang).astype(np.float32)
        C[:, blk*16:blk*16+8] = c; C[:, blk*16+8:blk*16+16] = c
        S[:, blk*16:blk*16+8] = -s; S[:, blk*16+8:blk*16+16] = s
    return C, S
def rope_tabs_ret(pos):
    half = 64
    inv = (10000.0 ** (-np.arange(half, dtype=np.float32) / half)).astype(np.float32)
    ang = pos.astype(np.float32)[:, None] * inv[None, :]
    return np.cos(ang).astype(np.float32), np.sin(ang).astype(np.float32)
IDB = np.eye(128, dtype=np.float32).astype(bf); IDF = np.eye(128, dtype=np.float32)

def stage_mod(I):
    pass

def modulation_dev(I):

    cv = np.concatenate([I['c'], I['c_ctx'][None]], 0)
    cT = np.ascontiguousarray(cv.reshape(3, 8, 128).transpose(2, 1, 0))
    ims = []
    for i in range(8):
        l = i // 4; cs = (i % 4) * 1536
        ims.append({"cT": cT, "w": wl(np.ascontiguousarray(I['ada_w'][l][:, cs:cs+1536]), 8), "b": np.ascontiguousarray(np.broadcast_to(I['ada_b'][l][cs:cs+1536], (3, 1536)))})
    r = run(build_mod(), ims, "mod")
    out = np.zeros((2, 3, 6144), np.float32)
    for i in range(8): out[i//4][:, (i%4)*1536:(i%4+1)*1536] = r[i]["o"]
    return out

def layer0_pre(I, mod, x_lat, x_ctx):

    ims = []
    for i in range(8):
        b, q = i // 4, i % 4
        tl = q * 4096 + np.arange(4096); tc = (q % 2) * 128 + np.arange(128)
        Cl, Sl = rope_tabs_mla(tl); Cc, Sc = np.ones((128, 32), np.float32), np.zeros((128, 32), np.float32)
        Crl, Srl = rope_tabs_ret(256 + tl); Crc, Src = rope_tabs_ret(tc)
        Cr = np.concatenate([Crc, Crl]); Sr = np.concatenate([Src, Srl]); ks = np.float32(128 ** -0.5)
        m = mod[0]
        ims.append({"x": pack(i, x_lat, x_ctx), "gB": bc(I['norm_mix_g'][0]),
                    "scB": np.stack([bc(m[b][1024:2048]), bc(m[2][1024:2048])]), "shB": np.stack([bc(m[b][0:1024]), bc(m[2][0:1024])]),
                    "w_in": wl(I['ev_w_in'][0], 8), "w_uq": wl(I['mla_w_uq'][0], 2), "w_ukv": wl(I['mla_w_ukv'][0], 1),
                    "gq": bc(I['mla_q_norm_g'][0]), "gkv": bc(I['mla_kv_norm_g'][0]),
                    "Cq": np.concatenate([Cc, Cl]), "Sq": np.concatenate([Sc, Sl]), "Cr": Cr, "Sr": Sr, "Crk": Cr * ks, "Srk": Sr * ks, "ident": IDB})
    r = run(k1_build(T), ims, "k1")
    return {n: unpack(r, n) for n in ("Qa", "Ka", "Va", "RQ", "RK", "RV", "RG")}

def layer0_mla(o1):

    Qa_l, Qa_c = o1["Qa"]; Ka_l, Ka_c = o1["Ka"]; Va_l, Va_c = o1["Va"]
    Ka = np.concatenate([Ka_c, Ka_l], 1).reshape(B, 16640, 8, 98); Va = np.concatenate([Va_c, Va_l], 1).reshape(B, 16640, 8, 65)
    Ql = Qa_l.reshape(B, N, 8, 98); Qc = Qa_c.reshape(B, NCTX, 8, 98)
    ims = []
    for h in range(8):
        ims.append({"QT": np.ascontiguousarray(Ql[:, :, h, :].transpose(0, 2, 1)), "QTc": np.ascontiguousarray(Qc[:, :, h, :].transpose(0, 2, 1)),
                    "KT": np.ascontiguousarray(Ka[:, :, h, :].transpose(0, 2, 1)),
                    "Vv": np.ascontiguousarray(Va[:, :, h, :].reshape(B, 130, 128, 65).transpose(0, 2, 1, 3))})
    r = run(k2_build(), ims, "k2")
    Ol = np.stack([r[h]["OT"].transpose(0, 2, 1) for h in range(8)], 2)
    Oc = np.stack([r[h]["OTc"].transpose(0, 2, 1) for h in range(8)], 2)
    return np.ascontiguousarray(Ol), np.ascontiguousarray(Oc)

def layer0_ret(o1):

    RQ = np.concatenate([o1["RQ"][1], o1["RQ"][0]], 1); RK = np.concatenate([o1["RK"][1], o1["RK"][0]], 1); RV = np.concatenate([o1["RV"][1], o1["RV"][0]], 1)
    ims = []
    for i in range(8):
        b, h = i // 4, i % 4
        sl = slice(h * 128, (h + 1) * 128)
        d = {"qT": np.ascontiguousarray(RQ[b][:, sl].T), "kT": np.ascontiguousarray(RK[b][:, sl].T),
             "kt": np.ascontiguousarray(RK[b][:, sl].reshape(130, 128, 128).transpose(1, 0, 2)),
             "vt": np.ascontiguousarray(RV[b][:, sl].reshape(130, 128, 128).transpose(1, 0, 2))}
        d.update(ret_consts(h)); ims.append(d)
    r = run(k3_build(), ims, "k3")
    O = np.zeros((B, 16640, 2, 512), np.float32)
    for i in range(8):
        b, h = i // 4, i % 4
        O[b][:, :, h * 128:(h + 1) * 128] = r[i]["o"].reshape(2, 16640, 128).transpose(1, 0, 2)
    return np.ascontiguousarray(O[:, 256:]), np.ascontiguousarray(O[:, :256])

def mid_common(I, mod, layer, b):
    m = mod[layer]
    return {"g1B": np.stack([bc(m[b][2048:3072]), bc(m[2][2048:3072])]), "gfB": bc(I['norm_ffn_g'][layer]),
            "sc2B": np.stack([bc(m[b][4096:5120]), bc(m[2][4096:5120])]), "sh2B": np.stack([bc(m[b][3072:4096]), bc(m[2][3072:4096])]),
            "w_r": wl(I['moe_router'][layer], 8), "ident": IDB, "identf": IDF}

def layer0_mid(I, mod, x_lat, x_ctx, o1, Oatt, Oret):

    ims = []
    for i in range(8):
        b = i // 4
        d = {"x": pack(i, x_lat, x_ctx), "Oatt": pack(i, Oatt[0], Oatt[1]), "Oret": pack(i, Oret[0], Oret[1]), "RG": pack(i, o1["RG"][0], o1["RG"][1]),
             "gn": bc(I['ret_norm_g'][0]), "w_out": wl(I['ev_w_out'][0], 8)}
        d.update(mid_common(I, mod, 0, b)); ims.append(d)
    r = run(k4_build(T, "even"), ims, "k4")
    return {n: unpack(r, n) for n in ("xmid", "h", "aff")}

def relerr(a, b): return float(np.sqrt(((a.astype(np.float64) - b) ** 2).mean() / (b.astype(np.float64) ** 2).mean()))

def moe_dev(I, layer, h_lat, h_ctx, aff_lat, aff_ctx, with_ctx=True):
    probs = ((128, 2048), (2, 32)) if with_ctx else ((128, 2048),)
    tri = np.triu(np.ones((128, 128), np.float32), 1)
    ims = []
    for i in range(8):
        b, eg = i // 4, i % 4
        es = slice(eg * 4, eg * 4 + 4)
        d = {"aff0": np.ascontiguousarray(aff_lat[b][:, es].reshape(128, 128, 4)), "h0": np.ascontiguousarray(h_lat[b].reshape(128, 128, 1024)),
             "wg": np.ascontiguousarray(I['moe_w_gate'][layer][es].reshape(4, 8, 128, 1536).transpose(0, 2, 1, 3)),
             "wu": np.ascontiguousarray(I['moe_w_up'][layer][es].reshape(4, 8, 128, 1536).transpose(0, 2, 1, 3)),
             "wd": np.ascontiguousarray(I['moe_w_down'][layer][es].reshape(4, 12, 128, 1024).transpose(0, 2, 1, 3)),
             "tri": tri, "ones": np.ones((128, 128), np.float32), "ident": IDB}
        if with_ctx:
            d["aff1"] = np.ascontiguousarray(aff_ctx[b][:, es].reshape(128, 2, 4)); d["h1"] = np.ascontiguousarray(h_ctx[b].reshape(128, 2, 1024))
        ims.append(d)
    r = run(k5_build(probs), ims, "k5")
    out = {}
    for pi, (J, cap) in enumerate(probs):
        n = 128 * J
        out[pi] = {"ys": [np.concatenate([r[b * 4 + eg][f"ys{pi}"] for eg in range(4)], 0) for b in range(B)],
                   "dest": np.stack([np.concatenate([r[b * 4 + eg][f"dest{pi}"].reshape(n, 4) for eg in range(4)], 1) for b in range(B)]),
                   "gsel": np.stack([np.concatenate([r[b * 4 + eg][f"gsel{pi}"].reshape(n, 4) for eg in range(4)], 1) for b in range(B)])}
    return out

def combine_dev(I, mod, layer, xmid_lat, xmid_ctx, mo, final=False):

    with_ctx = 1 in mo
    ims = []
    for i in range(8):
        b = i // 4
        m = mod[layer]
        if with_ctx:
            d = {"xmid": pack(i, xmid_lat, xmid_ctx), "gsel": pack(i, mo[0]["gsel"], mo[1]["gsel"]), "dest": pack(i, mo[0]["dest"], mo[1]["dest"]),
                 }
            for e in range(16):
                d[f"ys0_{e}"] = np.ascontiguousarray(mo[0]["ys"][b][e]); d[f"ys1_{e}"] = np.ascontiguousarray(mo[1]["ys"][b][e])
        else:
            q = i % 4; sl = slice(q * 4096, (q + 1) * 4096)
            d = {"xmid": np.ascontiguousarray(xmid_lat[b][sl]), "gsel": np.ascontiguousarray(mo[0]["gsel"][b][sl]), "dest": np.ascontiguousarray(mo[0]["dest"][b][sl])}
            for e in range(16):
                d[f"ys0_{e}"] = np.ascontiguousarray(mo[0]["ys"][b][e])
        d["g2B"] = np.stack([bc(m[b][5120:6144]), bc(m[2][5120:6144])])
        if final: d["fgB"] = bc(I['final_g'])
        ims.append(d)
    Tn = T if with_ctx else 32
    r = run(k6_build(Tn, (2048, 32) if with_ctx else (2048,), final=final), ims, "k6")
    if with_ctx:
        return unpack(r, "xo")
    xo = np.stack([np.concatenate([r[b * 4 + q]["xo"] for q in range(4)], 0) for b in range(B)])
    fo = np.stack([np.concatenate([r[b * 4 + q]["fo"] for q in range(4)], 0) for b in range(B)]) if final else None
    return xo, fo

def rope_tabs_win(tpos, is_ctx):
    n = len(tpos); C = np.ones((n, 64), np.float32); S = np.zeros((n, 64), np.float32)
    if not is_ctx:
        half = 16
        inv = (10000.0 ** (-np.arange(half, dtype=np.float32) / half)).astype(np.float32)
        for blk, pos in enumerate((tpos // 64, tpos % 64)):
            ang = pos.astype(np.float32)[:, None] * inv[None, :]
            c = np.cos(ang).astype(np.float32); s = np.sin(ang).astype(np.float32)
            C[:, blk*32:blk*32+16] = c; C[:, blk*32+16:blk*32+32] = c
            S[:, blk*32:blk*32+16] = -s; S[:, blk*32+16:blk*32+32] = s
    return C, S

def shifted_streams(xs):
    z = np.zeros_like(xs[:, :1])
    xp = np.concatenate([z, xs[:, :-1]], 1); xn = np.concatenate([xs[:, 1:], z], 1)
    fl = np.ones(xs.shape[:2] + (2,), np.float32); fl[:, 0, 0] = 0; fl[:, -1, 1] = 0
    return xp, xn, fl

def layer1_pre(I, mod, x_lat, x_ctx):

    xpl, xnl, fll = shifted_streams(x_lat); xpc, xnc, flc = shifted_streams(x_ctx)
    m = mod[1]; o = 0
    ims = []
    for i in range(8):
        b, q = i // 4, i % 4
        Cl, Sl = rope_tabs_win(q * 4096 + np.arange(4096), False); Cc, Sc = rope_tabs_win(np.arange(128), True)
        ims.append({"x": pack(i, x_lat, x_ctx), "xp": pack(i, xpl, xpc), "xn": pack(i, xnl, xnc), "fl": pack(i, fll, flc), "gB": bc(I['norm_mix_g'][1]),
            "scB": np.stack([bc(m[b][1024:2048]), bc(m[2][1024:2048])]), "shB": np.stack([bc(m[b][0:1024]), bc(m[2][0:1024])]),
            "w_in": wl(I['od_w_in'][o], 8), "muB": bc(I['rwkv_mu'][o]), "kkB": bc(I['rwkv_k_k'][o]), "kaB": bc(I['rwkv_k_a'][o]), "rkB": bc(I['rwkv_r_k'][o].reshape(-1)),
            "w0B": np.stack([bc(I['rwkv_w0'][o][d]) for d in range(2)]), "a0B": np.stack([bc(I['rwkv_a0'][o][d]) for d in range(2)]),
            "w2s": np.ascontiguousarray(I['rwkv_w2'][o].reshape(128, 1, 512)), "a2s": np.ascontiguousarray(I['rwkv_a2'][o].reshape(128, 1, 512)),
            "g2s": np.ascontiguousarray(I['rwkv_g2'][o].reshape(128, 1, 512)), "Cw": np.concatenate([Cc, Cl]), "Sw": np.concatenate([Sc, Sl]), "ident": IDB})
    r = run(k7_build(T), ims, "k7")
    names = ["R", "Vv", "KK", "LWf", "LWb", "Af", "Ab", "Kf", "Kb", "G", "BG", "NQ", "Qw", "Kw", "Vw"]
    return {n: unpack(r, n) for n in names}

def layer1_rwkv(o7):

    cst = k8_consts()
    ims = []
    def seq(name, b, dirn, ch):
        lat, ctx = o7[name]
        a = np.concatenate([ctx[b][::-1] if dirn else ctx[b], lat[b][::-1] if dirn else lat[b]], 0)[:, ch]
        return np.ascontiguousarray(a.reshape(260, 64, 256))
    for i in range(8):
        b, dirn, hg = i // 4, (i % 4) // 2, i % 2
        ch = slice(hg * 256, (hg + 1) * 256)
        d = {"R": seq("R", b, dirn, ch), "K": seq("Kb" if dirn else "Kf", b, dirn, ch), "Vv": seq("Vv", b, dirn, ch), "KK": seq("KK", b, dirn, ch),
             "A": seq("Ab" if dirn else "Af", b, dirn, ch), "LW": seq("LWb" if dirn else "LWf", b, dirn, ch)}
        d.update(cst); ims.append(d)
    r = run(k8_build(260, 4), ims, "k8")
    Y = np.zeros((B, N, 2, 512), np.float32)
    for i in range(8):
        b, dirn, hg = i // 4, (i % 4) // 2, i % 2
        y = r[i]["y"].reshape(N, 256)
        Y[b][:, dirn, hg * 256:(hg + 1) * 256] = y[::-1] if dirn else y
    return Y

def layer1_win(o7):

    Ql = o7["Qw"][0].reshape(B, N, 2, 4, 66)
    Kl = o7["Kw"][0].reshape(B, N, 2, 66); Vl = o7["Vw"][0].reshape(B, N, 2, 65)
    Kc = o7["Kw"][1].reshape(B, NCTX, 2, 66); Vc = o7["Vw"][1].reshape(B, NCTX, 2, 65)
    padK = np.zeros((128, 2, 66), bf); padK[:, :, 64] = 1.0; padK[:, :, 65] = -10000.0
    padV = np.zeros((128, 2, 65), bf)
    cst = k9_consts(); ims = []
    for i in range(8):
        b, q = i // 4, i % 4
        t0 = q * 4096
        lo = Kl[b][t0 - 128:t0] if q > 0 else padK; hi = Kl[b][t0 + 4096:t0 + 4224] if q < 3 else padK
        Kh = np.concatenate([lo, Kl[b][t0:t0 + 4096], hi], 0)
        lo = Vl[b][t0 - 128:t0] if q > 0 else padV; hi = Vl[b][t0 + 4096:t0 + 4224] if q < 3 else padV
        Vh = np.concatenate([lo, Vl[b][t0:t0 + 4096], hi], 0)
        Qb = Ql[b][t0:t0 + 4096].reshape(32, 128, 2, 4, 66)
        d = {"QT": np.ascontiguousarray(Qb.transpose(2, 4, 0, 3, 1).reshape(2, 66, 32 * 512)),
             "KT": np.ascontiguousarray(Kh.transpose(1, 2, 0)), "Vv": np.ascontiguousarray(Vh.reshape(34, 128, 2, 65).transpose(2, 1, 0, 3)),
             "KTc": np.ascontiguousarray(Kc[b].transpose(1, 2, 0)), "Vc": np.ascontiguousarray(Vc[b].reshape(2, 128, 2, 65).transpose(2, 1, 0, 3))}
        d.update(cst); ims.append(d)
    r = run(k9_build(32), ims, "k9")
    O = np.zeros((B, N, 8, 65), np.float32)
    for i in range(8):
        b, q = i // 4, i % 4
        ot = r[i]["OT"].reshape(2, 65, 32, 4, 128)
        O[b][q * 4096:(q + 1) * 4096] = ot.transpose(2, 4, 0, 3, 1).reshape(4096, 8, 65)
    return O

def layer1_mid(I, mod, x_lat, o7, Y, Ow):

    ims = []
    for i in range(8):
        b, q = i // 4, i % 4; sl = slice(q * 4096, (q + 1) * 4096)
        c = lambda a: np.ascontiguousarray(a[b][sl])
        d = {"x": c(x_lat), "Yr": c(Y), "G": c(o7["G"][0]), "BG": c(o7["BG"][0]), "Ow": c(Ow), "NQ": c(o7["NQ"][0]),
             "sinkB": bc(I['win_sink'][0]), "lngB": bc(I['rwkv_ln_g'][0]), "lnbB": bc(I['rwkv_ln_b'][0]), "w_out": wl(I['od_w_out'][0], 8)}
        d.update(mid_common(I, mod, 1, b)); ims.append(d)
    r = run(k4_build(32, "odd", has_ctx=False), ims, "k10")
    cat = lambda n: np.stack([np.concatenate([r[b * 4 + q][n] for q in range(4)], 0) for b in range(B)])
    return {n: cat(n) for n in ("xmid", "h", "aff")}


def kernel(**inputs):
    I = {k: np.asarray(v) for k, v in inputs.items()}
    mod = modulation_dev(I)
    x_lat, x_ctx = I['x'], I['ctx']
    o1 = layer0_pre(I, mod, x_lat, x_ctx)
    Oret = layer0_ret(o1)
    Oatt = layer0_mla(o1)
    o4 = layer0_mid(I, mod, x_lat, x_ctx, o1, Oatt, Oret)
    del o1, Oret, Oatt
    mo = moe_dev(I, 0, o4["h"][0], o4["h"][1], o4["aff"][0], o4["aff"][1], True)
    x_lat, x_ctx = combine_dev(I, mod, 0, o4["xmid"][0], o4["xmid"][1], mo)
    del o4, mo
    o7 = layer1_pre(I, mod, x_lat, x_ctx)
    Y = layer1_rwkv(o7)
    Ow = layer1_win(o7)
    o10 = layer1_mid(I, mod, x_lat, o7, Y, Ow)
    del o7, Y, Ow
    mo = moe_dev(I, 1, o10["h"], None, o10["aff"], None, False)
    xo2, fo = combine_dev(I, mod, 1, o10["xmid"], None, mo, final=True)
    return np.ascontiguousarray(fo.astype(np.float32))
```
